# Optimizing a Trainium2 kernel written in Bass

```python
import math
import jax, jax.numpy as jnp
from jax import lax
import numpy as np

D_MODEL = 2048
BATCH = 2
SEQ = 4096
DEPTH = 1

ATTN_HEADS = 8
ATTN_HEAD_DIM = 128
ATTN_WIDTH = ATTN_HEADS * ATTN_HEAD_DIM
MOBA_BLOCK = 256
MOBA_TOPK = 3
MOBA_Q_CHUNK = 32

RWKV_HEAD_DIM = 64
RWKV_WIDTH = D_MODEL - ATTN_WIDTH
RWKV_HEADS = RWKV_WIDTH // RWKV_HEAD_DIM
DECAY_LORA = 96
AAA_LORA = 96
GATE_LORA = 256
RWKV_GN_EPS = RWKV_HEAD_DIM * 1e-5

PEER_HEADS = 8
PEER_NKEYS = 128
PEER_EXPERTS = PEER_NKEYS * PEER_NKEYS
PEER_DKEY = 256
PEER_HALF = PEER_DKEY // 2
PEER_TOPK = 16
PEER_TOK_CHUNK = 128

RMS_EPS = 1e-6
NEG = -1e30

RWKV_COLS = 3 * RWKV_WIDTH + DECAY_LORA + AAA_LORA + GATE_LORA
IN_COLS = 3 * ATTN_WIDTH + RWKV_COLS

kernel_name = "hymba_moba_rwkv7_peer_block"


def rmsnorm(x, g):
    xf = x.astype(jnp.float32)
    y = xf * lax.rsqrt(jnp.mean(xf * xf, axis=-1, keepdims=True) + RMS_EPS)
    return (y * g.astype(jnp.float32)).astype(x.dtype)


def alibi_slopes(n):
    return 2.0 ** (-8.0 * (jnp.arange(n, dtype=jnp.float32) + 1.0) / n)


def moba_attention(q, k, v):
    B, S, _ = q.shape
    H, hd, L = ATTN_HEADS, ATTN_HEAD_DIM, MOBA_BLOCK
    nb = -(-S // L)
    s_pad = nb * L
    to_heads = lambda t: t.reshape(B, S, H, hd).transpose(0, 2, 1, 3)
    q, k, v = to_heads(q), to_heads(k), to_heads(v)
    pad = ((0, 0), (0, 0), (0, s_pad - S), (0, 0))
    k = jnp.pad(k, pad)
    v = jnp.pad(v, pad)
    kb = k.reshape(B, H, nb, L, hd)
    vb = v.reshape(B, H, nb, L, hd)
    kmean = jnp.mean(kb.astype(jnp.float32), axis=3)
    gate = jnp.einsum('bhsd,bhnd->bhsn', q.astype(jnp.float32), kmean)
    qblk = jnp.arange(S, dtype=jnp.int32) // L
    past = jnp.arange(nb, dtype=jnp.int32)[None, :] < qblk[:, None]
    gate = jnp.where(past[None, None], gate, NEG)
    n_sel = min(MOBA_TOPK, nb)
    _, sel = lax.top_k(gate, n_sel)
    sel = sel.astype(jnp.int32)
    sel_valid = sel < qblk[None, None, :, None]
    scale = hd ** -0.5
    slopes = alibi_slopes(H)
    offs = jnp.arange(L, dtype=jnp.int32)
    gather = jax.vmap(jax.vmap(lambda blocks, idx: blocks[idx]))
    Q = MOBA_Q_CHUNK

    def chunk(c0):
        qc = lax.dynamic_slice_in_dim(q, c0, Q, axis=2)
        ic = lax.dynamic_slice_in_dim(sel, c0, Q, axis=2)
        mc = lax.dynamic_slice_in_dim(sel_valid, c0, Q, axis=2)
        qpos = c0 + jnp.arange(Q, dtype=jnp.int32)
        ksel = gather(kb, ic)
        vsel = gather(vb, ic)
        s_sel = jnp.einsum('bhqd,bhqnld->bhqnl', qc, ksel).astype(jnp.float32) * scale
        kpos_sel = ic[..., None] * L + offs
        dist_sel = (qpos[None, None, :, None, None] - kpos_sel).astype(jnp.float32)
        s_sel = s_sel - slopes[None, :, None, None, None] * dist_sel
        s_sel = jnp.where(mc[..., None], s_sel, NEG).reshape(B, H, Q, n_sel * L)
        own0 = (c0 // L) * L
        kown = lax.dynamic_slice_in_dim(k, own0, L, axis=2)
        vown = lax.dynamic_slice_in_dim(v, own0, L, axis=2)
        s_own = jnp.einsum('bhqd,bhld->bhql', qc, kown).astype(jnp.float32) * scale
        dist_own = qpos[:, None] - (own0 + offs)[None, :]
        s_own = jnp.where(dist_own[None, None] >= 0,
                          s_own - slopes[None, :, None, None] * dist_own.astype(jnp.float32)[None, None],
                          NEG)
        p = jax.nn.softmax(jnp.concatenate([s_sel, s_own], axis=-1), axis=-1)
        p_sel = p[..., :n_sel * L].reshape(B, H, Q, n_sel, L).astype(v.dtype)
        p_own = p[..., n_sel * L:].astype(v.dtype)
        return (jnp.einsum('bhqnl,bhqnld->bhqd', p_sel, vsel)
                + jnp.einsum('bhql,bhld->bhqd', p_own, vown))

    starts = jnp.arange(0, S, Q, dtype=jnp.int32)
    out = lax.map(chunk, starts)
    return out.transpose(1, 0, 3, 2, 4).reshape(B, S, H * hd)


def token_shift_lerp(p, mu):
    prev = jnp.pad(p, ((0, 0), (1, 0), (0, 0)))[:, :-1]
    return p + (prev - p) * mu


def rwkv7_time_mix(p, w0, w2, a0, a2, g2, k_k, k_a, r_k, ln_g, ln_b):
    B, S, _ = p.shape
    H, N, W = RWKV_HEADS, RWKV_HEAD_DIM, RWKV_WIDTH
    f32 = jnp.float32
    r, k, v = p[..., :W], p[..., W:2 * W], p[..., 2 * W:3 * W]
    o = 3 * W
    xw = p[..., o:o + DECAY_LORA]
    o += DECAY_LORA
    xa = p[..., o:o + AAA_LORA]
    o += AAA_LORA
    xg = p[..., o:o + GATE_LORA]
    w_log = -jax.nn.softplus(-(w0 + jnp.tanh(xw) @ w2)) - 0.5
    decay = jnp.exp(-jnp.exp(w_log.astype(f32)))
    a = jax.nn.sigmoid(a0 + xa @ a2)
    g = jax.nn.sigmoid(xg) @ g2
    heads = lambda t: t.reshape(B, S, H, N)
    kk = heads(k * k_k).astype(f32)
    kk = kk / jnp.maximum(jnp.sqrt(jnp.sum(kk * kk, axis=-1, keepdims=True)), 1e-12)
    k = k * (1.0 + (a - 1.0) * k_a)
    r_h, k_h, v_h = heads(r), heads(k), heads(v)
    a_h = heads(a).astype(f32)
    b = kk * a_h
    xs = tuple(t.astype(f32).transpose(1, 0, 2, 3)
               for t in (r_h, heads(decay), k_h, v_h, -kk, b))

    def step(state, inp):
        rt, wt, kt, vt, at, bt = inp
        sa = jnp.einsum('bhij,bhj->bhi', state, at)
        state = (state * wt[:, :, None, :] + sa[..., None] * bt[:, :, None, :]
                 + vt[..., :, None] * kt[..., None, :])
        y = jnp.einsum('bhij,bhj->bhi', state, rt)
        return state, y

    s0 = jnp.zeros((B, H, N, N), f32)
    _, y = lax.scan(step, s0, xs)
    y = y.transpose(1, 0, 2, 3)
    mu = jnp.mean(y, axis=-1, keepdims=True)
    var = jnp.mean(jnp.square(y - mu), axis=-1, keepdims=True)
    y = ((y - mu) * lax.rsqrt(var + RWKV_GN_EPS)).reshape(B, S, W)
    y = y * ln_g.astype(f32) + ln_b.astype(f32)
    bonus = (jnp.sum(r_h * k_h * r_k, axis=-1, keepdims=True) * v_h).reshape(B, S, W)
    return ((y + bonus.astype(f32)) * g.astype(f32)).astype(p.dtype)


def peer_ffn(x, wq, sub_keys, u, v):
    B, S, D = x.shape
    T = B * S
    H, K, C = PEER_HEADS, PEER_TOPK, PEER_TOK_CHUNK
    xt = x.reshape(T, D)
    q = (xt @ wq).reshape(T, H, 2, PEER_HALF)
    s = jnp.einsum('thpc,hpnc->thpn', q, sub_keys).astype(jnp.float32)
    sv, si = lax.top_k(s, K)
    cand = (sv[:, :, 0, :, None] + sv[:, :, 1, None, :]).reshape(T, H, K * K)
    best, bi = lax.top_k(cand, K)
    i1 = jnp.take_along_axis(si[:, :, 0], bi // K, axis=-1)
    i2 = jnp.take_along_axis(si[:, :, 1], bi % K, axis=-1)
    expert = (i1 * PEER_NKEYS + i2).astype(jnp.int32)
    gates = jax.nn.softmax(best, axis=-1)
    nc = T // C

    def chunk(args):
        xc, ec, gc = args
        hpre = jnp.einsum('cd,chkd->chk', xc, u[ec]).astype(jnp.float32)
        act = (jax.nn.gelu(hpre, approximate=False) * gc).astype(x.dtype)
        return jnp.einsum('chk,chkd->cd', act, v[ec])

    y = lax.map(chunk, (xt.reshape(nc, C, D), expert.reshape(nc, C, H, K),
                        gates.reshape(nc, C, H, K)))
    return y.reshape(B, S, D)


def setup_inputs(seed: int = 0) -> dict:
    key = jax.random.key(seed)
    ks = jax.random.split(key, 22)
    f32 = jnp.float32
    nrm = lambda kk, shape, sc: jax.random.normal(kk, shape, f32) * sc
    L = DEPTH
    return {
        'x': nrm(ks[0], (BATCH, SEQ, D_MODEL), 1.0),
        'norm1_g': 1.0 + nrm(ks[1], (L, D_MODEL), 0.02),
        'w_in': nrm(ks[2], (L, D_MODEL, IN_COLS), D_MODEL ** -0.5),
        'rwkv_mu': jax.random.uniform(ks[3], (L, RWKV_COLS), f32),
        'rwkv_w0': jnp.tile(jnp.linspace(-6.0, -1.0, RWKV_HEAD_DIM, dtype=f32), RWKV_HEADS)[None]
                   + nrm(ks[4], (L, RWKV_WIDTH), 0.1),
        'rwkv_w2': nrm(ks[5], (L, DECAY_LORA, RWKV_WIDTH), 0.1 * DECAY_LORA ** -0.5),
        'rwkv_a0': nrm(ks[6], (L, RWKV_WIDTH), 0.5),
        'rwkv_a2': nrm(ks[7], (L, AAA_LORA, RWKV_WIDTH), AAA_LORA ** -0.5),
        'rwkv_g2': nrm(ks[8], (L, GATE_LORA, RWKV_WIDTH), GATE_LORA ** -0.5),
        'rwkv_k_k': 0.85 + nrm(ks[9], (L, RWKV_WIDTH), 0.02),
        'rwkv_k_a': 1.0 + nrm(ks[10], (L, RWKV_WIDTH), 0.02),
        'rwkv_r_k': nrm(ks[11], (L, RWKV_HEADS, RWKV_HEAD_DIM), 0.1),
        'rwkv_ln_g': 1.0 + nrm(ks[12], (L, RWKV_WIDTH), 0.02),
        'rwkv_ln_b': nrm(ks[13], (L, RWKV_WIDTH), 0.02),
        'w_out': nrm(ks[14], (L, ATTN_WIDTH + RWKV_WIDTH, D_MODEL), D_MODEL ** -0.5),
        'norm2_g': 1.0 + nrm(ks[15], (L, D_MODEL), 0.02),
        'peer_wq': nrm(ks[16], (L, D_MODEL, PEER_HEADS * PEER_DKEY), D_MODEL ** -0.5),
        'peer_sub_keys': nrm(ks[17], (L, PEER_HEADS, 2, PEER_NKEYS, PEER_HALF), PEER_HALF ** -0.5),
        'peer_u': nrm(ks[18], (L, PEER_EXPERTS, D_MODEL), D_MODEL ** -0.5),
        'peer_v': nrm(ks[19], (L, PEER_EXPERTS, D_MODEL), PEER_HEADS ** -0.5),
        'final_g': 1.0 + nrm(ks[20], (D_MODEL,), 0.02),
    }


def reference(x, norm1_g, w_in, rwkv_mu, rwkv_w0, rwkv_w2, rwkv_a0, rwkv_a2, rwkv_g2,
              rwkv_k_k, rwkv_k_a, rwkv_r_k, rwkv_ln_g, rwkv_ln_b, w_out, norm2_g,
              peer_wq, peer_sub_keys, peer_u, peer_v, final_g):
    A = ATTN_WIDTH
    h = x
    for l in range(DEPTH):
        xn = rmsnorm(h, norm1_g[l])
        proj = xn @ w_in[l]
        attn = moba_attention(proj[..., :A], proj[..., A:2 * A], proj[..., 2 * A:3 * A])
        rw_in = token_shift_lerp(proj[..., 3 * A:], rwkv_mu[l])
        rw = rwkv7_time_mix(rw_in, rwkv_w0[l], rwkv_w2[l], rwkv_a0[l], rwkv_a2[l], rwkv_g2[l],
                            rwkv_k_k[l], rwkv_k_a[l], rwkv_r_k[l], rwkv_ln_g[l], rwkv_ln_b[l])
        h = h + jnp.concatenate([attn, rw], axis=-1) @ w_out[l]
        h = h + peer_ffn(rmsnorm(h, norm2_g[l]), peer_wq[l], peer_sub_keys[l], peer_u[l], peer_v[l])
    return rmsnorm(h, final_g)
```

```python
import os
import time
from contextlib import ExitStack
import numpy as np
import concourse.bass as bass
import concourse.mybir as mybir
from concourse.bass_utils import run_bass_kernel_spmd

F32 = mybir.dt.float32
BF16 = mybir.dt.bfloat16
I32 = mybir.dt.int32
U32 = mybir.dt.uint32
AF = mybir.ActivationFunctionType
ALU = mybir.AluOpType
AX = mybir.AxisListType

D = 2048
S_ = int(os.environ.get("MK_S", "4096"))
NT = S_ // 128
TOWN = S_ // 4
STG = min(2048, S_)
NTO = TOWN // 128
A_W = 1024
R_W = 1024
IN_COLS = 6592
RW_COLS = 3520
UPTO = int(os.environ.get("MK_UPTO", "99"))
DBG = os.environ.get("MK_DBG", "")
CGS = [int(v) for v in os.environ.get("MK_CGS", "").split(",") if v]


class Sched:
    def __init__(self, nc, n_dma_sems=32):
        self.nc = nc
        self.engs = {"pe": nc.tensor, "act": nc.scalar, "dve": nc.vector,
                     "pool": nc.gpsimd, "sp": nc.sync}
        self.sem = {k: nc.alloc_semaphore(name=f"s_{k}") for k in self.engs}
        self.cnt = {k: 0 for k in self.engs}
        self.seen = {k: {} for k in self.engs}
        self.dma_sems = [nc.alloc_semaphore(name=f"s_dma{i}") for i in range(n_dma_sems)]
        self.dma_cnt = [0] * n_dma_sems
        self.dma_next = 0
        self.res = {}
        self.ninst = 0
        self.nwait = 0

    def _semh(self, sk):
        return self.sem[sk] if isinstance(sk, str) else self.dma_sems[sk[1]]

    def _need(self, e, sk, val):
        if val <= 0 or self.seen[e].get(sk, 0) >= val:
            return
        self.engs[e].wait_ge(self._semh(sk), val)
        self.seen[e][sk] = val
        self.nwait += 1

    @staticmethod
    def _key(x):
        return x if isinstance(x, str) else x.tensor.name

    def _r(self, key):
        return self.res.setdefault(key, {"w": {}, "r": {}})

    def _deps(self, e, reads, writes, me):
        for k in reads:
            for sk, v in self._r(k)["w"].items():
                self._need(e, sk, v)
        for k in writes:
            rr = self._r(k)
            for sk, v in rr["w"].items():
                if sk != me:
                    self._need(e, sk, v)
            for sk, v in rr["r"].items():
                if sk != me:
                    self._need(e, sk, v)

    def _commit(self, reads, writes, sk, val):
        for k in reads:
            self._r(k)["r"][sk] = val
        for k in writes:
            self._r(k)["w"][sk] = val

    def I(self, e, method, *args, w=("out", "accum_out"), xr=(), xw=(), **kw):
        reads, writes = [], []
        for name, v in kw.items():
            if isinstance(v, bass.AP):
                if v.tensor.name.startswith("in_"):
                    continue
                (writes if name in w else reads).append(v.tensor.name)
        for v in args:
            if isinstance(v, bass.AP):
                writes.append(v.tensor.name)
        reads += [self._key(x) for x in xr]
        writes += [self._key(x) for x in xw]
        self._deps(e, reads, writes, e)
        inst = getattr(self.engs[e], method)(*args, **kw)
        self.cnt[e] += 1
        inst.then_inc(self.sem[e], 1)
        self._commit(reads, writes, e, self.cnt[e])
        self.ninst += 1
        return inst

    def dma(self, q, out, in_, method="dma_start", xr=(), xw=(), **kw):
        reads, writes = [], []
        if not in_.tensor.name.startswith("in_"):
            reads.append(in_.tensor.name)
        writes.append(out.tensor.name)
        for name, v in kw.items():
            if isinstance(v, bass.AP):
                reads.append(v.tensor.name)
        reads += [self._key(x) for x in xr]
        writes += [self._key(x) for x in xw]
        i = self.dma_next
        self.dma_next = (self.dma_next + 1) % len(self.dma_sems)
        sk = ("dma", i)
        self._need(q, sk, self.dma_cnt[i])
        self._deps(q, reads, writes, sk)
        inst = getattr(self.engs[q], method)(out=out, in_=in_, **kw)
        self.dma_cnt[i] += 16
        inst.then_inc(self.dma_sems[i], 16)
        self._commit(reads, writes, sk, self.dma_cnt[i])
        self.ninst += 1
        return inst

    def barrier(self):
        for e in self.engs:
            self.wait_all(e)

    def wait_all(self, e):
        for k in self.engs:
            if k != e:
                self._need(e, k, self.cnt[k])
        for i in range(len(self.dma_sems)):
            self._need(e, ("dma", i), self.dma_cnt[i])


class Ctx:
    pass


def build():
    nc = bass.Bass("TRN2", target_bir_lowering=False)
    S = Sched(nc)
    K = Ctx()
    K.nc, K.S = nc, S
    K.uid = 0

    K.declared = []

    def din(name, shape, dt=F32):
        K.declared.append("in_" + name)
        return nc.dram_tensor("in_" + name, list(shape), dt, kind="ExternalInput").ap()

    def dscr(name, shape, dt):
        return nc.dram_tensor(name, list(shape), dt, kind="Internal").ap()

    K.din, K.dscr = din, dscr
    I = Ctx()
    K.I = I
    I.x = din("x", [S_, D])
    I.xo = din("xo", [TOWN, D])
    I.g1 = din("g1", [128, D])
    I.win = din("win", [D, IN_COLS])
    I.wout = din("wout", [D, D])
    I.g2 = din("g2", [128, D])
    I.gf = din("gf", [128, D])
    I.ident = din("ident", [128, 128])
    I.ridx = din("ridx", [128, 16], I32)
    I.pb = din("pb", [128, 16 * 16])
    I.pm = din("pm", [128, 16 * 16])
    I.own = din("own", [128, 16 * 16])
    I.dhi = din("dhi", [128, NT * 16])
    I.dlo = din("dlo", [128, NT * 16])
    I.esel = din("esel", [32, 16 * 128])
    I.kb = din("kb", [128, 2])
    I.cm = din("cm", [128, 4 * 512])
    I.ones = din("ones", [128, 128])
    I.w2 = din("w2", [96, 1024])
    I.a2 = din("a2", [96, 1024])
    I.rg2 = din("g2r", [256, 1024])
    I.mul = din("mul", [128, 4])
    I.w0 = din("w0", [128, 8])
    I.a0 = din("a0", [128, 8])
    I.bones = din("bones", [128, 128])
    I.mask2 = din("mask2", [128, 512])
    I.masklt = din("masklt", [128, 128])
    I.rst = din("rst", [128, min(2048, S_)])
    I.prm = din("prm", [128, 80])
    I.wq = din("wq", [D, 2048])
    I.subk = din("subk", [16, 128, 128])
    I.u = din("u", [16384, D])
    I.v = din("v", [16384, D])
    I.iota16 = din("iota16", [128, 16])
    out = nc.dram_tensor("out", [TOWN, D], F32, kind="ExternalOutput").ap()
    K.out = out
    Z = Ctx()
    K.Z = Z
    Z.qT = dscr("z_qT", [8, 128, S_], BF16)
    Z.kT = dscr("z_kT", [8, 128, S_], BF16)
    Z.v = dscr("z_v", [8, S_, 128], BF16)
    Z.rw = dscr("z_rw", [RW_COLS, S_], F32)
    Z.mix = dscr("z_mix", [D * 4, TOWN], BF16)
    Z.lw = dscr("z_lw", [1024, S_], F32)
    Z.a = dscr("z_a", [1024, S_], F32)
    Z.g = dscr("z_g", [1024, S_], F32)

    es_all = ExitStack()

    def sb(es, name, shape, dt):
        return es.enter_context(nc.sbuf_tensor(name, list(shape), dt))

    def ps(es, name, shape, dt=F32):
        return es.enter_context(nc.psum_tensor(name, list(shape), dt))

    K.sb, K.ps = sb, ps
    C = Ctx()
    K.C = C
    C.identf = sb(es_all, "identf", [128, 128], F32)
    C.identb = sb(es_all, "identb", [128, 128], BF16)
    S.dma("sp", C.identf[:], I.ident)
    S.I("dve", "tensor_copy", out=C.identb[:], in_=C.identf[:])

    if UPTO >= 1:
        phase_norm_proj(K)
        S.barrier()
    if UPTO >= 2:
        phase_zero_mix(K)
        S.barrier()
    if UPTO >= 3 and not os.environ.get("MK_NOATT"):
        phase_attn(K)
        S.barrier()
    if UPTO >= 4:
        phase_rwkv_lora(K)
        S.barrier()
        phase_rwkv_heads(K)
        S.barrier()
    if UPTO >= 5:
        phase_tail(K)
        S.barrier()
    if DBG:
        dbg_dump(K)
    S.wait_all("sp")
    es_all.close()
    print("ninst", S.ninst, "nwait", S.nwait)
    nc._mk_declared = list(K.declared)
    return nc


def dbg_dump(K):
    nc, S, Z = K.nc, K.S, K.Z
    items = {"qT0": Z.qT[0], "kT7": Z.kT[7], "v3": Z.v[3], "rw0": Z.rw[0:128, :], "rwl": Z.rw[RW_COLS - 64:RW_COLS, :],
             "mix": Z.mix, "mixa": Z.mix[0:4 * 256, :], "mixr": Z.mix[4 * 1024:4 * 1024 + 4 * 256, :],
             "lw": Z.lw[0:128, :], "za": Z.a[0:128, :], "zg": Z.g[0:128, :]}
    for name in DBG.split(","):
        if name not in items:
            continue
        src = items[name]
        dst = nc.dram_tensor("dbg_" + name, list(src.shape), src.dtype, kind="ExternalOutput").ap()
        S.dma("sp", dst, src)


def sumsq(K, junk, src, ss_col):
    S = K.S
    S.I("act", "activation", out=junk[:], in_=src, func=AF.Square)
    S.I("dve", "tensor_reduce", out=ss_col, in_=junk[:], axis=AX.X, op=ALU.add)


def rms_rstd(K, ss_col, tmp_col, out_col):
    S = K.S
    S.I("dve", "tensor_scalar", out=tmp_col, in0=ss_col, scalar1=1.0 / D, scalar2=1e-6,
        op0=ALU.mult, op1=ALU.add)
    S.I("act", "activation", out=tmp_col, in_=tmp_col, func=AF.Sqrt)
    S.I("dve", "reciprocal", out=out_col, in_=tmp_col)


def phase_norm_proj(K):
    nc, S, I, Z, C = K.nc, K.S, K.I, K.Z, K.C
    with ExitStack() as es:
        sb, ps = (lambda *a: K.sb(es, *a)), (lambda *a: K.ps(es, *a))
        xnT = sb("xnT", [128, 16, S_], BF16)
        ss = sb("ss", [128, NT], F32)
        ms = sb("ms", [128, NT], F32)
        rstd = sb("rstd", [128, NT], F32)
        xn = [sb(f"xn{j}", [128, D], BF16) for j in range(2)]
        with ExitStack() as es1:
            sb1, ps1 = (lambda *a: K.sb(es1, *a)), (lambda *a: K.ps(es1, *a))
            g1 = sb1("g1", [128, D], F32)
            S.dma("sp", g1[:], I.g1)
            xb = [sb1(f"xb{j}", [128, D], F32) for j in range(2)]
            junk = sb1("junk", [128, D], F32)
            pT = [ps1(f"pT{j}", [128, 16, 128], BF16) for j in range(2)]
            for i in range(NT):
                j = i % 2
                S.dma("sp", xb[j][:], I.x[i * 128:(i + 1) * 128, :])
                sumsq(K, junk, xb[j][:], ss[:, i:i + 1])
                rms_rstd(K, ss[:, i:i + 1], ms[:, i:i + 1], rstd[:, i:i + 1])
                S.I("dve", "scalar_tensor_tensor", out=xn[j][:], in0=xb[j][:], scalar=rstd[:, i:i + 1],
                    in1=g1[:], op0=ALU.mult, op1=ALU.mult)
                for kc in range(16):
                    S.I("pe", "transpose", out=pT[j][:, kc, :], in_=xn[j][:, kc * 128:(kc + 1) * 128],
                        identity=C.identb[:])
                S.I("act" if i % 2 == 0 else "dve", "copy" if i % 2 == 0 else "tensor_copy",
                    out=xnT[:, :, i * 128:(i + 1) * 128], in_=pT[j][:])
        S.barrier()
        if "stat" in DBG:
            for nm, t in (("ss", ss), ("ms", ms), ("rstd", rstd)):
                dst = nc.dram_tensor("dbg_" + nm, [128, NT], F32, kind="ExternalOutput").ap()
                S.dma("sp", dst, t[:])
            dst = nc.dram_tensor("dbg_xnl", [128, D], BF16, kind="ExternalOutput").ap()
            S.dma("sp", dst, xn[(NT - 1) % 2][:])
        if "xnT" in DBG:
            dst = nc.dram_tensor("dbg_xnT", [128, 16, S_], BF16, kind="ExternalOutput").ap()
            S.dma("sp", dst, xnT[:])
        wb = [sb(f"wb{j}", [128, 16, 512], BF16) for j in range(2)]
        stg_b = [sb(f"stgb{j}", [128, STG], BF16) for j in range(2)]
        stg_f = [sb(f"stgf{j}", [128, STG], F32) for j in range(2)]
        pp = [ps(f"pp{j}", [128, 512], F32) for j in range(4)]
        K.pcnt = 0
        K.ecnt = 0
        K.scnt = 0
        win_v = I.win.rearrange("(kc p) c -> p kc c", p=128)
        ngroups = (IN_COLS + 511) // 512
        for cg in range(ngroups):
            if CGS and cg not in CGS:
                continue
            c0 = cg * 512
            ncol = min(512, IN_COLS - c0)
            w = wb[cg % 2]
            S.dma("pool", w[:, :, 0:ncol], win_v[:, :, c0:c0 + ncol])
            is_v = 2048 <= c0 < 3072
            if is_v:
                h0 = (c0 - 2048) // 128
                for tt in range(NT):
                    p = pp[K.pcnt % 4]; K.pcnt += 1
                    for kc in range(16):
                        S.I("pe", "matmul", out=p[:, :], lhsT=xnT[:, kc, tt * 128:(tt + 1) * 128],
                            rhs=w[:, kc, :], start=(kc == 0), stop=(kc == 15))
                    st = stg_b[K.scnt % 2]; K.scnt += 1
                    evac(K, st[:, 0:512], p[:, :])
                    S.dma("sp", Z.v[h0:h0 + 4, tt * 128:(tt + 1) * 128, :].rearrange("h t d -> t h d"),
                          st[:, 0:512].rearrange("t (h d) -> t h d", h=4))
                continue
            for cc in range((ncol + 127) // 128):
                m = min(128, ncol - cc * 128)
                col = c0 + cc * 128
                f32_out = col >= 3072
                for half in range(S_ // STG):
                    st = (stg_f if f32_out else stg_b)[K.scnt % 2]; K.scnt += 1
                    for tg in range(STG // 512):
                        p = pp[K.pcnt % 4]; K.pcnt += 1
                        t0 = half * STG + tg * 512
                        for kc in range(16):
                            S.I("pe", "matmul", out=p[0:m, :], lhsT=w[:, kc, cc * 128:cc * 128 + m],
                                rhs=xnT[:, kc, t0:t0 + 512], start=(kc == 0), stop=(kc == 15))
                        evac(K, st[0:m, tg * 512:(tg + 1) * 512], p[0:m, :],
                             scale=(128 ** -0.5 if col < 1024 else None))
                    hs = slice(half * STG, (half + 1) * STG)
                    if col < 1024:
                        S.dma("sp", Z.qT[col // 128, :, hs], st[:, :])
                    elif col < 2048:
                        S.dma("sp", Z.kT[(col - 1024) // 128, :, hs], st[:, :])
                    else:
                        r0 = col - 3072
                        S.dma("sp", Z.rw[r0:r0 + m, hs], st[0:m, :])


def evac(K, dst, src, scale=None):
    S = K.S
    K.ecnt += 1
    if scale is not None:
        if K.ecnt % 2 == 0:
            S.I("act", "activation", out=dst, in_=src, func=AF.Copy, scale=float(scale))
        else:
            S.I("dve", "tensor_scalar", out=dst, in0=src, scalar1=float(scale), scalar2=None, op0=ALU.mult)
    else:
        if K.ecnt % 2 == 0:
            S.I("act", "copy", out=dst, in_=src)
        else:
            S.I("dve", "tensor_copy", out=dst, in_=src)


def phase_zero_mix(K):
    nc, S, Z = K.nc, K.S, K.Z
    with ExitStack() as es:
        zt = K.sb(es, "zt", [128, TOWN], BF16)
        S.I("dve", "memset", zt[:], 0.0)
        mv = Z.mix.rearrange("(a p) t -> a p t", p=128)
        for a in range(D * 4 // 128):
            S.dma("sp", mv[a], zt[:])
        S.barrier()


def phase_attn(K):
    nc, S, I, Z, C = K.nc, K.S, K.I, K.Z, K.C
    NB = S_ // 256
    NQ = S_ // 512
    with ExitStack() as es:
        sb, ps = (lambda *a: K.sb(es, *a)), (lambda *a: K.ps(es, *a))
        pb = sb("a_pb", [128, 16, 16], F32); S.dma("sp", pb[:], I.pb.rearrange("p (a b) -> p a b", a=16))
        pm = sb("a_pm", [128, 16, 16], F32); S.dma("sp", pm[:], I.pm.rearrange("p (a b) -> p a b", a=16))
        own = sb("a_own", [128, 16, 16], F32); S.dma("sp", own[:], I.own.rearrange("p (a b) -> p a b", a=16))
        dhi = sb("a_dhi", [128, NT, 16], F32); S.dma("sp", dhi[:], I.dhi.rearrange("p (a b) -> p a b", b=16))
        dlo = sb("a_dlo", [128, NT, 16], F32); S.dma("sp", dlo[:], I.dlo.rearrange("p (a b) -> p a b", b=16))
        esel = sb("a_esel", [32, 16, 128], BF16); S.dma("pool", esel[:], I.esel.rearrange("p (a b) -> p a b", a=16))
        kb = sb("a_kb", [128, 2], F32); S.dma("sp", kb[:], I.kb)
        cm = sb("a_cm", [128, 4, 512], BF16); S.dma("pool", cm[:], I.cm.rearrange("p (a b) -> p a b", a=4))
        ones = sb("a_ones", [128, 128], BF16); S.dma("pool", ones[:], I.ones)
        qT = sb("a_qT", [128, S_], BF16)
        kT = sb("a_kT", [128, S_], BF16)
        v = sb("a_v", [128, NT, 128], BF16)
        mbt = sb("a_mbt", [32, S_], BF16)
        km32 = sb("a_km32", [128, 16], F32)
        kmb = sb("a_kmb", [128, 16], BF16)
        gm = sb("a_gm", [128, 16], F32)
        m8 = sb("a_m8", [128, 8], F32)
        sel = sb("a_sel", [128, 16], F32)
        mbv = sb("a_mbv", [128, 16], F32)
        mb = sb("a_mb", [128, 32], F32)
        kbh = sb("a_kbh", [128, 2], F32)
        PT = [sb(f"a_PT{j}", [128, 512], BF16) for j in range(2)]
        rD = sb("a_rD", [128, 512], F32)
        Ost = [sb(f"a_O{j}", [128, 512], BF16) for j in range(2)]
        pg = ps("a_pg", [128, 16], F32)
        pt = ps("a_pt", [32, 128], F32)
        pS = [ps(f"a_pS{j}", [128, 512], F32) for j in range(2)]
        pO = ps("a_pO", [128, 512], F32)
        pD = ps("a_pD", [128, 512], F32)
        mixv = Z.mix.rearrange("(f q) t -> f q t", q=4)
        S.I("dve", "memset", km32[:], 0.0)
        for h in range(8):
            slope = 2.0 ** (-(h + 1))
            S.dma("sp", qT[:], Z.qT[h])
            S.dma("sp", kT[:], Z.kT[h])
            S.dma("sp", v[:], Z.v[h].rearrange("(j p) d -> p j d", p=128))
            S.I("dve", "tensor_reduce", out=km32[:, 0:NB], in_=kT[:].rearrange("p (n l) -> p n l", l=256),
                axis=AX.X, op=ALU.add)
            S.I("dve", "tensor_scalar", out=kmb[:], in0=km32[:], scalar1=1.0 / 256, scalar2=None, op0=ALU.mult)
            S.I("dve", "tensor_scalar", out=kbh[:], in0=kb[:], scalar1=slope, scalar2=None, op0=ALU.mult)
            for i in range(NT):
                qb = i // 2
                S.I("pe", "matmul", out=pg[:, :], lhsT=qT[:, i * 128:(i + 1) * 128], rhs=kmb[:, :],
                    start=True, stop=True)
                S.I("dve", "tensor_tensor", out=gm[:], in0=pg[:, :], in1=pb[:, qb, :], op=ALU.add)
                S.I("dve", "max", out=m8[:], in_=gm[:])
                S.I("dve", "scalar_tensor_tensor", out=sel[:], in0=gm[:], scalar=m8[:, 2:3], in1=pm[:, qb, :],
                    op0=ALU.is_ge, op1=ALU.mult)
                S.I("dve", "tensor_tensor", out=sel[:], in0=sel[:], in1=own[:, qb, :], op=ALU.add)
                S.I("dve", "tensor_scalar", out=mbv[:], in0=sel[:], scalar1=-1.0, scalar2=30000.0,
                    op0=ALU.add, op1=ALU.mult)
                S.I("dve", "scalar_tensor_tensor", out=mb[:, 0:16], in0=dhi[:, i, :], scalar=slope, in1=mbv[:],
                    op0=ALU.mult, op1=ALU.add)
                S.I("dve", "tensor_scalar", out=mb[:, 16:32], in0=dlo[:, i, :], scalar1=slope, scalar2=None,
                    op0=ALU.mult)
                S.I("pe", "transpose", out=pt[:, :], in_=mb[:, :], identity=C.identf[:])
                S.I("act", "copy", out=mbt[:, i * 128:(i + 1) * 128], in_=pt[:, :])
            for Q in range(NQ):
                nkt = 4 * (Q + 1)
                qs = slice(Q * 512, (Q + 1) * 512)

                def qk(kt):
                    p = pS[kt % 2]
                    S.I("pe", "matmul", out=p[:, :], lhsT=kT[:, kt * 128:(kt + 1) * 128], rhs=qT[:, qs],
                        start=True, stop=False)
                    diag = kt >= 4 * Q
                    S.I("pe", "matmul", out=p[:, :], lhsT=esel[:, kt // 2, :], rhs=mbt[:, qs],
                        start=False, stop=not diag)
                    if diag:
                        S.I("pe", "matmul", out=p[:, :], lhsT=C.identb[:], rhs=cm[:, kt - 4 * Q, :],
                            start=False, stop=True)

                qk(0)
                for kt in range(nkt):
                    if kt + 1 < nkt:
                        qk(kt + 1)
                    P = PT[kt % 2]
                    S.I("act", "activation", out=P[:], in_=pS[kt % 2][:, :], func=AF.Exp,
                        bias=kbh[:, (kt % 2):(kt % 2) + 1], scale=1.0)
                    S.I("pe", "matmul", out=pO[:, :], lhsT=v[:, kt, :], rhs=P[:], start=(kt == 0),
                        stop=(kt == nkt - 1))
                    S.I("pe", "matmul", out=pD[:, :], lhsT=ones[:], rhs=P[:], start=(kt == 0),
                        stop=(kt == nkt - 1))
                S.I("dve", "reciprocal", out=rD[:], in_=pD[:, :])
                O = Ost[Q % 2]
                S.I("dve", "tensor_tensor", out=O[:], in0=pO[:, :], in1=rD[:], op=ALU.mult)
                piece = min(512, TOWN)
                for pc in range(512 // piece):
                    t0 = Q * 512 + pc * piece
                    S.dma("sp", mixv[h * 128:(h + 1) * 128, t0 // TOWN, (t0 % TOWN):(t0 % TOWN) + piece],
                          O[:, pc * piece:(pc + 1) * piece])


SEG = min(2048, S_)
NCS = SEG // 128


def shifted_load(K, raw, halo, mu_col, rows, r0, t0, out, tmp, act=None):
    S, Z = K.S, K.Z
    if t0 == 0:
        S.I("dve", "memset", halo[0:rows, 0:1], 0.0)
        S.dma("sp", halo[0:rows, 1:SEG + 1], Z.rw[r0:r0 + rows, 0:SEG])
    else:
        S.dma("sp", halo[0:rows, 0:SEG + 1], Z.rw[r0:r0 + rows, t0 - 1:t0 + SEG])
    S.I("dve", "tensor_tensor", out=tmp[0:rows, :], in0=halo[0:rows, 0:SEG], in1=halo[0:rows, 1:SEG + 1],
        op=ALU.subtract)
    S.I("dve", "scalar_tensor_tensor", out=out, in0=tmp[0:rows, :], scalar=mu_col, in1=halo[0:rows, 1:SEG + 1],
        op0=ALU.mult, op1=ALU.add)


def phase_rwkv_lora(K):
    nc, S, I, Z, C = K.nc, K.S, K.I, K.Z, K.C
    with ExitStack() as es:
        sb, ps = (lambda *a: K.sb(es, *a)), (lambda *a: K.ps(es, *a))
        w2b = sb("l_w2", [96, 1024], BF16); S.dma("pool", w2b[:], I.w2)
        a2b = sb("l_a2", [96, 1024], BF16); S.dma("pool", a2b[:], I.a2)
        g2b = sb("l_g2", [128, 2, 1024], BF16); S.dma("pool", g2b[:], I.rg2.rearrange("(k p) c -> p k c", p=128))
        mul = sb("l_mu", [128, 4], F32); S.dma("sp", mul[:], I.mul)
        w0 = sb("l_w0", [128, 8], F32); S.dma("sp", w0[:], I.w0)
        a0 = sb("l_a0", [128, 8], F32); S.dma("sp", a0[:], I.a0)
        halo = sb("l_halo", [128, SEG + 1], F32)
        tmp = sb("l_tmp", [128, SEG], F32)
        xs = sb("l_xs", [128, SEG], F32)
        txw = sb("l_txw", [96, SEG], BF16)
        xab = sb("l_xab", [96, SEG], BF16)
        sxg = sb("l_sxg", [128, 2, SEG], BF16)
        stg = [sb(f"l_stg{j}", [128, SEG], F32) for j in range(2)]
        pp = [ps(f"l_pp{j}", [128, 512], F32) for j in range(4)]
        n = 0
        ns = 0
        for seg in range(S_ // SEG):
            t0 = seg * SEG
            shifted_load(K, Z.rw, halo, mul[0:96, 0:1], 96, 3072, t0, xs[0:96, :], tmp)
            S.I("act", "activation", out=txw[:], in_=xs[0:96, :], func=AF.Tanh)
            shifted_load(K, Z.rw, halo, mul[0:96, 1:2], 96, 3168, t0, xs[0:96, :], tmp)
            S.I("act", "copy", out=xab[:], in_=xs[0:96, :])
            for kc in range(2):
                shifted_load(K, Z.rw, halo, mul[:, 2 + kc:3 + kc], 128, 3264 + kc * 128, t0, xs[:, :], tmp)
                S.I("act", "activation", out=sxg[:, kc, :], in_=xs[:, :], func=AF.Sigmoid)
            for cc in range(8):
                cs = slice(cc * 128, (cc + 1) * 128)
                for which in range(3):
                    st = stg[ns % 2]; ns += 1
                    for tg in range(SEG // 512):
                        ts_ = slice(tg * 512, (tg + 1) * 512)
                        p = pp[n % 4]; n += 1
                        if which == 0:
                            S.I("pe", "matmul", out=p[:, :], lhsT=w2b[:, cs], rhs=txw[:, ts_], start=True, stop=True)
                            S.I("act", "activation", out=st[:, ts_], in_=p[:, :], func=AF.Sigmoid,
                                bias=w0[:, cc:cc + 1], scale=1.0)
                        elif which == 1:
                            S.I("pe", "matmul", out=p[:, :], lhsT=a2b[:, cs], rhs=xab[:, ts_], start=True, stop=True)
                            S.I("act", "activation", out=st[:, ts_], in_=p[:, :], func=AF.Sigmoid,
                                bias=a0[:, cc:cc + 1], scale=1.0)
                        else:
                            for kc in range(2):
                                S.I("pe", "matmul", out=p[:, :], lhsT=g2b[:, kc, cs], rhs=sxg[:, kc, ts_],
                                    start=(kc == 0), stop=(kc == 1))
                            S.I("dve", "tensor_copy", out=st[:, ts_], in_=p[:, :])
                    if which == 0:
                        S.I("dve", "tensor_scalar", out=st[:, :], in0=st[:, :], scalar1=-0.6065306597, scalar2=None,
                            op0=ALU.mult)
                    dst = (Z.lw, Z.a, Z.g)[which]
                    S.dma("sp", dst[cs, t0:t0 + SEG], st[:, :])


def phase_rwkv_heads(K):
    nc, S, I, Z, C = K.nc, K.S, K.I, K.Z, K.C
    NPAIR = int(os.environ.get("MK_NPAIR", "8"))
    with ExitStack() as es:
        sb, ps = (lambda *a: K.sb(es, *a)), (lambda *a: K.ps(es, *a))
        bones = sb("r_bones", [128, 128], BF16); S.dma("pool", bones[:], I.bones)
        mask2 = sb("r_mask2", [128, 2, 256], F32); S.dma("sp", mask2[:], I.mask2.rearrange("p (a b) -> p a b", a=2))
        masklt = sb("r_masklt", [128, 128], F32); S.dma("sp", masklt[:], I.masklt)
        rst = sb("r_rst", [128, SEG], F32); S.dma("sp", rst[:], I.rst)
        prm = sb("r_prm", [128, 10, 8], F32); S.dma("sp", prm[:], I.prm.rearrange("p (a b) -> p a b", a=10))
        omka = sb("r_omka", [128, 8], F32)
        S.I("dve", "tensor_scalar", out=omka[:], in0=prm[:, 5, :], scalar1=-1.0, scalar2=1.0, op0=ALU.mult, op1=ALU.add)
        halo = sb("r_halo", [128, SEG + 1], F32)
        F = [sb(f"r_F{j}", [128, SEG], F32) for j in range(9)]
        gkeep = sb("r_g", [128, SEG], F32)
        bvkeep = sb("r_bv", [128, SEG], F32)
        AR = sb("r_AR", [128, NCS, 2, 128], BF16)
        btb = sb("r_bt", [128, SEG], BF16)
        ktb = sb("r_kt", [128, SEG], BF16)
        bhb = sb("r_bh", [128, SEG], BF16)
        khb = sb("r_kh", [128, SEG], BF16)
        vbb = sb("r_vb", [128, SEG], BF16)
        sqb = sb("r_sqb", [128, SEG], BF16)
        gamC = sb("r_gamC", [128, NCS], F32)
        ostg = sb("r_ostg", [128, SEG], BF16)
        STh = [[sb(f"r_ST{h}{j}", [128, 64], F32) for j in range(2)] for h in range(2)]
        STbh = [sb(f"r_STb{h}", [128, 64], BF16) for h in range(2)]
        NU = 4
        TRg = [sb(f"r_TR{g}", [128, 3, 128], BF16) for g in range(2)]
        MM = [sb(f"r_MM{u}", [128, 2, 256], BF16) for u in range(NU)]
        Pm = [[sb(f"r_P{u}{j}", [128, 128], BF16) for j in range(2)] for u in range(NU)]
        PTm = [[sb(f"r_PT{u}{j}", [128, 128], BF16) for j in range(2)] for u in range(NU)]
        Tm = [[sb(f"r_T{u}{j}", [128, 128], BF16) for j in range(2)] for u in range(NU)]
        ZT = [sb(f"r_ZT{h}", [128, 64], BF16) for h in range(2)]
        UT = [sb(f"r_UT{h}", [128, 64], BF16) for h in range(2)]
        ysb = sb("r_ysb", [128, 2, 64], F32)
        ysq = sb("r_ysq", [128, 2, 64], F32)
        st1 = sb("r_st1", [128, 8], F32)
        ynb = sb("r_ynb", [128, 128], BF16)
        of1 = sb("r_of1", [128, 128], F32)
        pTb = ps("r_pTb", [128, 1024], BF16)
        pM = ps("r_pM", [128, 2, 256], F32)
        pDb = [ps(f"r_pD{u}", [128, 4, 128], F32) for u in range(NU)]
        pCh = [ps(f"r_pC{h}", [128, 8, 64], F32) for h in range(2)]
        pX = pM[:].rearrange("p a b -> p (a b)")
        mixv = Z.mix.rearrange("(f q) t -> f q t", q=4)

        for hp in range(NPAIR):
            cc = hp
            c0 = hp * 128
            col = lambda a: prm[:, a, cc:cc + 1]
            for hh in range(2):
                S.I("dve", "memset", STh[hh][0][:], 0.0)
                S.I("dve", "memset", STbh[hh][:], 0.0)
            cur = 0
            for seg in range(S_ // SEG):
                t0 = seg * SEG
                rs, ks, vs, lw, aa, t1, t2, t3, t4 = F
                shifted_load(K, Z.rw, halo, col(0), 128, c0, t0, rs[:, :], t1)
                shifted_load(K, Z.rw, halo, col(1), 128, 1024 + c0, t0, ks[:, :], t1)
                shifted_load(K, Z.rw, halo, col(2), 128, 2048 + c0, t0, vs[:, :], t1)
                S.dma("sp", lw[:], Z.lw[c0:c0 + 128, t0:t0 + SEG])
                S.dma("sp", aa[:], Z.a[c0:c0 + 128, t0:t0 + SEG])
                S.dma("sp", gkeep[:], Z.g[c0:c0 + 128, t0:t0 + SEG])
                S.I("dve", "tensor_scalar", out=t1[:], in0=ks[:], scalar1=col(4), scalar2=None, op0=ALU.mult)
                S.I("act", "activation", out=sqb[:], in_=t1[:], func=AF.Square)
                for tg in range(SEG // 512):
                    ts_ = slice(tg * 512, (tg + 1) * 512)
                    S.I("pe", "matmul", out=pX, lhsT=bones[:], rhs=sqb[:, ts_], start=True, stop=True)
                    S.I("dve", "tensor_scalar", out=t2[:, ts_], in0=pX, scalar1=1e-24, scalar2=None, op0=ALU.add)
                S.I("act", "activation", out=t2[:], in_=t2[:], func=AF.Sqrt)
                S.I("dve", "reciprocal", out=t2[:], in_=t2[:])
                S.I("dve", "tensor_tensor", out=t1[:], in0=t1[:], in1=t2[:], op=ALU.mult)
                S.I("dve", "tensor_scalar", out=t2[:], in0=aa[:], scalar1=col(5), scalar2=omka[:, cc:cc + 1],
                    op0=ALU.mult, op1=ALU.add)
                S.I("dve", "tensor_tensor", out=ks[:], in0=ks[:], in1=t2[:], op=ALU.mult)
                S.I("dve", "tensor_tensor", out=aa[:], in0=t1[:], in1=aa[:], op=ALU.mult)
                S.I("dve", "tensor_tensor", out=t2[:], in0=rs[:], in1=ks[:], op=ALU.mult)
                S.I("dve", "tensor_scalar", out=sqb[:], in0=t2[:], scalar1=col(6), scalar2=None, op0=ALU.mult)
                for tg in range(SEG // 512):
                    ts_ = slice(tg * 512, (tg + 1) * 512)
                    S.I("pe", "matmul", out=pX, lhsT=bones[:], rhs=sqb[:, ts_], start=True, stop=True)
                    S.I("dve", "tensor_tensor", out=bvkeep[:, ts_], in0=pX, in1=vs[:, ts_], op=ALU.mult)
                S.I("act", "copy", out=vbb[:], in_=vs[:])
                S.I("dve", "tensor_tensor_scan", out=t2[:], data0=rst[:], data1=lw[:], initial=0.0,
                    op0=ALU.mult, op1=ALU.add)
                S.I("act", "activation", out=t3[:], in_=t2[:], func=AF.Exp)
                S.I("dve", "tensor_tensor", out=AR[:, :, 1, :], in0=rs[:].rearrange("p (c t) -> p c t", t=128),
                    in1=t3[:].rearrange("p (c t) -> p c t", t=128), op=ALU.mult)
                S.I("dve", "tensor_tensor", out=t3[:], in0=t2[:], in1=lw[:], op=ALU.subtract)
                S.I("act", "activation", out=t3[:], in_=t3[:], func=AF.Exp)
                S.I("dve", "scalar_tensor_tensor", out=AR[:, :, 0, :], in0=t1[:].rearrange("p (c t) -> p c t", t=128),
                    scalar=-1.0, in1=t3[:].rearrange("p (c t) -> p c t", t=128), op0=ALU.mult, op1=ALU.mult)
                S.I("act", "activation", out=t3[:], in_=t2[:], func=AF.Exp, scale=-1.0)
                S.I("dve", "tensor_tensor", out=btb[:], in0=aa[:], in1=t3[:], op=ALU.mult)
                S.I("dve", "tensor_tensor", out=ktb[:], in0=ks[:], in1=t3[:], op=ALU.mult)
                for c in range(NCS):
                    cs = slice(c * 128, (c + 1) * 128)
                    S.I("dve", "tensor_scalar", out=t4[:, cs], in0=t2[:, cs], scalar1=-1.0,
                        scalar2=t2[:, c * 128 + 127:c * 128 + 128], op0=ALU.mult, op1=ALU.add)
                S.I("act", "activation", out=t4[:], in_=t4[:], func=AF.Exp)
                S.I("dve", "tensor_tensor", out=bhb[:], in0=aa[:], in1=t4[:], op=ALU.mult)
                S.I("dve", "tensor_tensor", out=khb[:], in0=ks[:], in1=t4[:], op=ALU.mult)
                S.I("act", "activation", out=gamC[:], in_=t2[:].rearrange("p (c t) -> p c t", t=128)[:, :, 127],
                    func=AF.Exp)
                def stageA(u, c, hh, TR):
                    cs = slice(c * 128, (c + 1) * 128)
                    rows = slice(hh * 64, hh * 64 + 64)
                    mm, pD = MM[u], pDb[u]
                    P, PT, T = Pm[u], PTm[u], Tm[u]
                    S.I("pe", "matmul", out=pM[:, 0, :], lhsT=btb[rows, cs], rhs=AR[rows, c, :, :], start=True, stop=True)
                    S.I("pe", "matmul", out=pM[:, 1, :], lhsT=ktb[rows, cs], rhs=AR[rows, c, :, :], start=True, stop=True)
                    S.I("pe", "matmul", out=pD[:, 0, :], lhsT=AR[rows, c, 0, :], rhs=btb[rows, cs], start=True, stop=True)
                    S.I("dve", "tensor_tensor", out=mm[:], in0=pM[:], in1=mask2[:], op=ALU.mult)
                    yield
                    S.I("dve", "tensor_tensor", out=PT[0][:], in0=pD[:, 0, :], in1=masklt[:], op=ALU.mult)
                    S.I("dve", "tensor_tensor", out=T[0][:], in0=mm[:, 0, 0:128], in1=C.identb[:], op=ALU.add)
                    yield
                    Pc, PTc, Tc = mm[:, 0, 0:128], PT[0][:], T[0][:]
                    for rd in range(6):
                        j = (rd + 1) % 2
                        S.I("pe", "matmul", out=pD[:, 1, :], lhsT=Pc, rhs=PTc, start=True, stop=True)
                        if rd < 5:
                            S.I("pe", "matmul", out=pD[:, 2, :], lhsT=PTc, rhs=Pc, start=True, stop=True)
                        yield
                        S.I("act", "copy", out=PT[j][:], in_=pD[:, 1, :])
                        if rd < 5:
                            S.I("act", "copy", out=P[j][:], in_=pD[:, 2, :])
                        yield
                        S.I("pe", "matmul", out=pD[:, 3, :], lhsT=PT[j][:], rhs=Tc, start=True, stop=True)
                        yield
                        S.I("dve", "tensor_tensor", out=T[j][:], in0=pD[:, 3, :], in1=Tc, op=ALU.add)
                        Pc, PTc, Tc = P[j][:], PT[j][:], T[j][:]
                    K.Tfinal[u] = Tc

                def chain(u, c, hh, TR, cur):
                    rows = slice(hh * 64, hh * 64 + 64)
                    hc = slice(hh * 64, hh * 64 + 64)
                    mm, pC, Tc = MM[u], pCh[hh], K.Tfinal[u]
                    Sc, Sn, STb = STh[hh][cur], STh[hh][1 - cur], STbh[hh]
                    S.I("pe", "matmul", out=pC[:, 0, :], lhsT=AR[rows, c, 0, :], rhs=STb[rows, :], start=True, stop=False)
                    S.I("pe", "matmul", out=pC[:, 0, :], lhsT=mm[:, 1, 0:128], rhs=TR[:, 2, hc], start=False, stop=True)
                    yield
                    S.I("act", "copy", out=ZT[hh][:], in_=pC[:, 0, :])
                    yield
                    S.I("pe", "matmul", out=pC[:, 1, :], lhsT=Tc, rhs=ZT[hh][:], start=True, stop=True)
                    yield
                    S.I("act", "copy", out=UT[hh][:], in_=pC[:, 1, :])
                    yield
                    pY = pC[:, 3, :]
                    S.I("pe", "matmul", out=pY, lhsT=AR[rows, c, 1, :], rhs=STb[rows, :], start=True, stop=False)
                    S.I("pe", "matmul", out=pY, lhsT=mm[:, 0, 128:256], rhs=UT[hh][:], start=False, stop=False)
                    S.I("pe", "matmul", out=pY, lhsT=mm[:, 1, 128:256], rhs=TR[:, 2, hc], start=False, stop=True)
                    S.I("pe", "matmul", out=pC[rows, 2, :], lhsT=TR[:, 0, hc], rhs=UT[hh][:], start=True, stop=False)
                    S.I("pe", "matmul", out=pC[rows, 2, :], lhsT=TR[:, 1, hc], rhs=TR[:, 2, hc], start=False, stop=True)
                    yield
                    S.I("dve", "scalar_tensor_tensor", out=Sn[rows, :], in0=Sc[rows, :], scalar=gamC[rows, c:c + 1],
                        in1=pC[rows, 2, :], op0=ALU.mult, op1=ALU.add)
                    S.I("act", "copy", out=STb[rows, :], in_=Sn[rows, :])
                    S.I("act", "copy", out=ysb[:, hh, :], in_=pY)

                def run_rr(gens):
                    gens = list(gens)
                    while gens:
                        for g_ in list(gens):
                            try:
                                next(g_)
                            except StopIteration:
                                gens.remove(g_)

                K.Tfinal = [None] * NU
                for c0_ in range(0, NCS, 2):
                    grp = [c0_ + g_ for g_ in range(2) if c0_ + g_ < NCS]
                    for g_, c in enumerate(grp):
                        cs = slice(c * 128, (c + 1) * 128)
                        TR = TRg[g_]
                        S.I("pe", "transpose", out=pTb[:, g_ * 384 + 0:g_ * 384 + 128], in_=bhb[:, cs], identity=C.identb[:])
                        S.I("pe", "transpose", out=pTb[:, g_ * 384 + 128:g_ * 384 + 256], in_=khb[:, cs], identity=C.identb[:])
                        S.I("pe", "transpose", out=pTb[:, g_ * 384 + 256:g_ * 384 + 384], in_=vbb[:, cs], identity=C.identb[:])
                        S.I("act", "copy", out=TR[:].rearrange("p a b -> p (a b)"), in_=pTb[:, g_ * 384:g_ * 384 + 384])
                    run_rr([stageA(g_ * 2 + hh, c, hh, TRg[g_]) for g_, c in enumerate(grp) for hh in range(2)])
                    for g_, c in enumerate(grp):
                        cs = slice(c * 128, (c + 1) * 128)
                        run_rr([chain(g_ * 2 + hh, c, hh, TRg[g_], cur) for hh in range(2)])
                        cur = 1 - cur
                        y3 = ysb[:]
                        S.I("dve", "tensor_reduce", out=st1[:, 0:2], in_=y3, axis=AX.X, op=ALU.add)
                        S.I("act", "activation", out=ysq[:], in_=y3, func=AF.Square)
                        S.I("dve", "tensor_reduce", out=st1[:, 2:4], in_=ysq[:], axis=AX.X, op=ALU.add)
                        S.I("dve", "tensor_scalar", out=st1[:, 0:2], in0=st1[:, 0:2], scalar1=1.0 / 64, scalar2=None, op0=ALU.mult)
                        S.I("dve", "tensor_tensor", out=st1[:, 4:6], in0=st1[:, 0:2], in1=st1[:, 0:2], op=ALU.mult)
                        S.I("dve", "tensor_scalar", out=st1[:, 2:4], in0=st1[:, 2:4], scalar1=1.0 / 64, scalar2=64e-5,
                            op0=ALU.mult, op1=ALU.add)
                        S.I("dve", "tensor_tensor", out=st1[:, 2:4], in0=st1[:, 2:4], in1=st1[:, 4:6], op=ALU.subtract)
                        S.I("act", "activation", out=st1[:, 2:4], in_=st1[:, 2:4], func=AF.Sqrt)
                        S.I("dve", "reciprocal", out=st1[:, 6:8], in_=st1[:, 2:4])
                        for hh in range(2):
                            S.I("dve", "tensor_scalar", out=ynb[:, hh * 64:(hh + 1) * 64], in0=ysb[:, hh, :],
                                scalar1=st1[:, hh:hh + 1], scalar2=st1[:, 6 + hh:7 + hh], op0=ALU.subtract, op1=ALU.mult)
                        S.I("pe", "transpose", out=pTb[:, 768:896], in_=ynb[:], identity=C.identb[:])
                        S.I("dve", "tensor_scalar", out=of1[:], in0=pTb[:, 768:896], scalar1=col(7), scalar2=col(8),
                            op0=ALU.mult, op1=ALU.add)
                        S.I("dve", "tensor_tensor", out=of1[:], in0=of1[:], in1=bvkeep[:, cs], op=ALU.add)
                        S.I("dve", "tensor_tensor", out=ostg[:, cs], in0=of1[:], in1=gkeep[:, cs], op=ALU.mult)
                piece = min(SEG, TOWN)
                for pc in range(SEG // piece):
                    tt0 = t0 + pc * piece
                    S.dma("sp", mixv[1024 + c0:1024 + c0 + 128, tt0 // TOWN, (tt0 % TOWN):(tt0 % TOWN) + piece],
                          ostg[:, pc * piece:(pc + 1) * piece])


def phase_peer(K, hres):
    nc, S, I, Z, C = K.nc, K.S, K.I, K.Z, K.C
    NEG = -1e30
    with ExitStack() as es:
        sb, ps = (lambda *a: K.sb(es, *a)), (lambda *a: K.ps(es, *a))
        idx_all = sb("p_idx", [128, NTO, 128], I32)
        gate_all = sb("p_gate", [128, NTO, 128], F32)
        ss2 = sb("p_ss2", [128, 8], F32)
        ms2 = sb("p_ms2", [128, 8], F32)
        rstd2 = sb("p_rstd2", [128, 8], F32)
        g2 = sb("p_g2", [128, D], F32); S.dma("sp", g2[:], I.g2)
        junk = sb("p_junk", [128, D], F32)
        with ExitStack() as es1:
            sb1, ps1 = (lambda *a: K.sb(es1, *a)), (lambda *a: K.ps(es1, *a))
            wqb = sb1("p_wq", [128, 16, 2048], BF16)
            wq_v = I.wq.rearrange("(kc p) c -> p kc c", p=128)
            for j in range(4):
                S.dma("pool", wqb[:, :, j * 512:(j + 1) * 512], wq_v[:, :, j * 512:(j + 1) * 512])
            skn = sb1("p_skn", [128, 16, 128], BF16)
            S.dma("pool", skn[:], I.subk.rearrange("h n c -> n h c"))
            skT = sb1("p_skT", [128, 16, 128], BF16)
            iota16 = sb1("p_iota", [128, 16], F32); S.dma("sp", iota16[:], I.iota16)
            xn2b = sb1("p_xn2b", [128, D], BF16)
            xn2T = sb1("p_xn2T", [128, 16, 128], BF16)
            q2T = sb1("p_q2T", [128, 16, 128], BF16)
            s_sb = sb1("p_s", [128, 16, 128], F32)
            s2 = sb1("p_s2", [128, 128], F32)
            sv = sb1("p_sv", [128, 16, 16], F32)
            si = sb1("p_si", [128, 16, 16], U32)
            sif = sb1("p_sif", [128, 16, 16], F32)
            cand = sb1("p_cand", [128, 8, 256], F32)
            c2 = sb1("p_c2", [128, 256], F32)
            best = sb1("p_best", [128, 8, 16], F32)
            bi = sb1("p_bi", [128, 8, 16], U32)
            ai = sb1("p_ai", [128, 8, 16], U32)
            bbi = sb1("p_bbi", [128, 8, 16], U32)
            af = sb1("p_af", [128, 8, 16], F32)
            bf = sb1("p_bf", [128, 8, 16], F32)
            oh = sb1("p_oh", [128, 8, 16, 16], F32)
            i1 = sb1("p_i1", [128, 8, 16], F32)
            i2 = sb1("p_i2", [128, 8, 16], F32)
            ef = sb1("p_ef", [128, 8, 16], F32)
            gs = sb1("p_gs", [128, 8], F32)
            pT = ps1("p_pT", [128, 16, 128], BF16)
            pq = [ps1(f"p_pq{j}", [128, 4, 128], F32) for j in range(2)]
            psc = [ps1(f"p_psc{j}", [128, 4, 128], F32) for j in range(2)]
            for half in range(2):
                for j in range(8):
                    hp = half * 8 + j
                    S.I("pe", "transpose", out=pT[:, j, :], in_=skn[:, hp, :], identity=C.identb[:])
                S.I("act", "copy", out=skT[:, half * 8:(half + 1) * 8, :], in_=pT[:, 0:8, :])
            B4 = [128, 8, 16, 16]
            for tt in range(NTO):
                sumsq(K, junk, hres[:, tt, :], ss2[:, tt:tt + 1])
                rms_rstd(K, ss2[:, tt:tt + 1], ms2[:, tt:tt + 1], rstd2[:, tt:tt + 1])
                S.I("dve", "scalar_tensor_tensor", out=xn2b[:], in0=hres[:, tt, :], scalar=rstd2[:, tt:tt + 1],
                    in1=g2[:], op0=ALU.mult, op1=ALU.mult)
                for kc in range(16):
                    S.I("pe", "transpose", out=pT[:, kc, :], in_=xn2b[:, kc * 128:(kc + 1) * 128], identity=C.identb[:])
                S.I("act", "copy", out=xn2T[:], in_=pT[:])
                for g4 in range(4):
                    p = pq[g4 % 2]
                    for j in range(4):
                        hp = g4 * 4 + j
                        for kc in range(16):
                            S.I("pe", "matmul", out=p[:, j, :], lhsT=wqb[:, kc, hp * 128:(hp + 1) * 128],
                                rhs=xn2T[:, kc, :], start=(kc == 0), stop=(kc == 15))
                    S.I("act", "copy", out=q2T[:, g4 * 4:(g4 + 1) * 4, :], in_=p[:])
                for g4 in range(4):
                    p = psc[g4 % 2]
                    for j in range(4):
                        hp = g4 * 4 + j
                        S.I("pe", "matmul", out=p[:, j, :], lhsT=q2T[:, hp, :], rhs=skT[:, hp, :], start=True, stop=True)
                    S.I("dve", "tensor_copy", out=s_sb[:, g4 * 4:(g4 + 1) * 4, :], in_=p[:])
                for hp in range(16):
                    S.I("dve", "max", out=sv[:, hp, 0:8], in_=s_sb[:, hp, :])
                    S.I("dve", "max_index", out=si[:, hp, 0:8], in_max=sv[:, hp, 0:8], in_values=s_sb[:, hp, :])
                    S.I("dve", "match_replace", out=s2[:], in_to_replace=sv[:, hp, 0:8], in_values=s_sb[:, hp, :],
                        imm_value=NEG)
                    S.I("dve", "max", out=sv[:, hp, 8:16], in_=s2[:])
                    S.I("dve", "max_index", out=si[:, hp, 8:16], in_max=sv[:, hp, 8:16], in_values=s2[:])
                S.I("dve", "tensor_copy", out=sif[:], in_=si[:])
                sv4 = sv[:].rearrange("p (h two) k -> p h two k", two=2)
                sif4 = sif[:].rearrange("p (h two) k -> p h two k", two=2)
                S.I("dve", "tensor_tensor", out=cand[:].rearrange("p h (a b) -> p h a b", a=16),
                    in0=sv4[:, :, 0, :].unsqueeze(3).broadcast_to(B4), in1=sv4[:, :, 1, :].unsqueeze(2).broadcast_to(B4),
                    op=ALU.add)
                for h in range(8):
                    S.I("dve", "max", out=best[:, h, 0:8], in_=cand[:, h, :])
                    S.I("dve", "max_index", out=bi[:, h, 0:8], in_max=best[:, h, 0:8], in_values=cand[:, h, :])
                    S.I("dve", "match_replace", out=c2[:], in_to_replace=best[:, h, 0:8], in_values=cand[:, h, :],
                        imm_value=NEG)
                    S.I("dve", "max", out=best[:, h, 8:16], in_=c2[:])
                    S.I("dve", "max_index", out=bi[:, h, 8:16], in_max=best[:, h, 8:16], in_values=c2[:])
                S.I("dve", "tensor_single_scalar", out=ai[:], in_=bi[:], scalar=4, op=ALU.logical_shift_right)
                S.I("dve", "tensor_single_scalar", out=bbi[:], in_=bi[:], scalar=15, op=ALU.bitwise_and)
                S.I("dve", "tensor_copy", out=af[:], in_=ai[:])
                S.I("dve", "tensor_copy", out=bf[:], in_=bbi[:])
                io4 = iota16[:].unsqueeze(1).unsqueeze(1).broadcast_to(B4)
                for (xf, which, dst) in ((af, 0, i1), (bf, 1, i2)):
                    S.I("dve", "tensor_tensor", out=oh[:], in0=xf[:].unsqueeze(3).broadcast_to(B4), in1=io4, op=ALU.is_equal)
                    S.I("dve", "tensor_tensor", out=oh[:], in0=oh[:], in1=sif4[:, :, which, :].unsqueeze(2).broadcast_to(B4),
                        op=ALU.mult)
                    S.I("dve", "tensor_reduce", out=dst[:], in_=oh[:], axis=AX.X, op=ALU.add)
                S.I("dve", "scalar_tensor_tensor", out=ef[:], in0=i1[:], scalar=128.0, in1=i2[:], op0=ALU.mult, op1=ALU.add)
                S.I("dve", "tensor_copy", out=idx_all[:, tt, :].rearrange("p (h k) -> p h k", h=8), in_=ef[:])
                S.I("dve", "tensor_tensor", out=ef[:], in0=best[:], in1=best[:, :, 0:1].broadcast_to([128, 8, 16]),
                    op=ALU.subtract)
                S.I("act", "activation", out=ef[:], in_=ef[:], func=AF.Exp)
                S.I("dve", "tensor_reduce", out=gs[:], in_=ef[:], axis=AX.X, op=ALU.add)
                S.I("dve", "reciprocal", out=gs[:], in_=gs[:])
                S.I("dve", "tensor_tensor", out=gate_all[:, tt, :].rearrange("p (h k) -> p h k", h=8), in0=ef[:],
                    in1=gs[:].unsqueeze(2).broadcast_to([128, 8, 16]), op=ALU.mult)
        S.barrier()
        if "peer" in DBG:
            for nm, t in (("pidx", idx_all), ("pgate", gate_all)):
                dst = nc.dram_tensor("dbg_" + nm, [128, NTO, 128], t.dtype, kind="ExternalOutput").ap()
                S.dma("sp", dst, t[:])
        with ExitStack() as es1:
            sb1, ps1 = (lambda *a: K.sb(es1, *a)), (lambda *a: K.ps(es1, *a))
            xn2p = [ps1(f"p_xn2p{j}", [128, 512], F32) for j in range(4)]
            accp = [ps1(f"p_accp{j}", [128, 512], F32) for j in range(4)]
            ring = [sb1(f"p_ring{j}", [128, D], F32) for j in range(6)]
            hpre = sb1("p_hpre", [128, 128], F32)
            hp4 = sb1("p_hp4", [128, 4, 128], F32)
            actv = sb1("p_act", [128, 128], F32)
            nr = 0
            for tt in range(NTO):
                for j in range(4):
                    js = slice(j * 512, (j + 1) * 512)
                    S.I("dve", "scalar_tensor_tensor", out=xn2p[j][:, :], in0=hres[:, tt, js], scalar=rstd2[:, tt:tt + 1],
                        in1=g2[:, js], op0=ALU.mult, op1=ALU.mult)
                for slot in range(128):
                    ug = ring[nr % 6]; nr += 1
                    S.dma("pool", ug[:], I.u, method="indirect_dma_start", out_offset=None,
                          in_offset=bass.IndirectOffsetOnAxis(ap=idx_all[:, tt, slot:slot + 1], axis=0), xr=[idx_all[:]])
                    for j in range(4):
                        js = slice(j * 512, (j + 1) * 512)
                        S.I("dve", "scalar_tensor_tensor", out=junk[:, js], in0=ug[:, js], scalar=1.0, in1=xn2p[j][:, :],
                            op0=ALU.mult, op1=ALU.mult, accum_out=hp4[:, j, slot:slot + 1])
                S.I("dve", "tensor_tensor", out=hp4[:, 0:2, :], in0=hp4[:, 0:2, :], in1=hp4[:, 2:4, :], op=ALU.add)
                S.I("dve", "tensor_tensor", out=hpre[:], in0=hp4[:, 0, :], in1=hp4[:, 1, :], op=ALU.add)
                if "peer" in DBG and tt == 0:
                    dst = nc.dram_tensor("dbg_hpre", [128, 128], F32, kind="ExternalOutput").ap()
                    S.dma("sp", dst, hpre[:])
                S.I("act", "activation", out=actv[:], in_=hpre[:], func=AF.Gelu)
                S.I("dve", "tensor_tensor", out=actv[:], in0=actv[:], in1=gate_all[:, tt, :], op=ALU.mult)
                for slot in range(128):
                    vg = ring[nr % 6]; nr += 1
                    S.dma("pool", vg[:], I.v, method="indirect_dma_start", out_offset=None,
                          in_offset=bass.IndirectOffsetOnAxis(ap=idx_all[:, tt, slot:slot + 1], axis=0), xr=[idx_all[:]])
                    for j in range(4):
                        js = slice(j * 512, (j + 1) * 512)
                        if slot == 0:
                            S.I("dve", "tensor_scalar", out=accp[j][:, :], in0=vg[:, js], scalar1=actv[:, 0:1], scalar2=None,
                                op0=ALU.mult)
                        else:
                            S.I("dve", "scalar_tensor_tensor", out=accp[j][:, :], in0=vg[:, js], scalar=actv[:, slot:slot + 1],
                                in1=accp[j][:, :], op0=ALU.mult, op1=ALU.add)
                for j in range(4):
                    js = slice(j * 512, (j + 1) * 512)
                    S.I("dve", "tensor_tensor", out=hres[:, tt, js], in0=accp[j][:, :], in1=hres[:, tt, js], op=ALU.add)
        S.barrier()


def phase_tail(K):
    nc, S, I, Z, C = K.nc, K.S, K.I, K.Z, K.C
    with ExitStack() as es:
        sb, ps = (lambda *a: K.sb(es, *a)), (lambda *a: K.ps(es, *a))
        hres = sb("hres", [128, NTO, D], F32)
        S.dma("sp", hres[:], I.xo.rearrange("(tt p) d -> p tt d", p=128))
        with ExitStack() as es1:
            sb1, ps1 = (lambda *a: K.sb(es1, *a)), (lambda *a: K.ps(es1, *a))
            ridx = sb1("ridx", [128, 16], I32)
            S.dma("sp", ridx[:], I.ridx)
            mixT = sb1("mixT", [128, 16, TOWN], BF16)
            for fc in range(16):
                if os.environ.get("MK_NOIND"):
                    S.dma("sp", mixT[:, fc, :], Z.mix[fc * 128:(fc + 1) * 128, :])
                    continue
                S.dma("pool", mixT[:, fc, :], Z.mix, method="indirect_dma_start", out_offset=None,
                      in_offset=bass.IndirectOffsetOnAxis(ap=ridx[:, fc:fc + 1], axis=0), xr=[ridx[:]])
            wb = [sb1(f"wo{j}", [128, 16, 512], BF16) for j in range(2)]
            pp = [ps1(f"po{j}", [128, 512], F32) for j in range(4)]
            wo_v = I.wout.rearrange("(kc p) c -> p kc c", p=128)
            n = 0
            for cg in range(4):
                w = wb[cg % 2]
                S.dma("pool", w[:], wo_v[:, :, cg * 512:(cg + 1) * 512])
                for tt in range(NTO):
                    p = pp[n % 4]; n += 1
                    for kc in range(16):
                        S.I("pe", "matmul", out=p[:, :], lhsT=mixT[:, kc, tt * 128:(tt + 1) * 128],
                            rhs=w[:, kc, :], start=(kc == 0), stop=(kc == 15))
                    S.I("dve", "tensor_tensor", out=hres[:, tt, cg * 512:(cg + 1) * 512], in0=p[:, :],
                        in1=hres[:, tt, cg * 512:(cg + 1) * 512], op=ALU.add)
        S.barrier()
        if UPTO >= 6:
            phase_peer(K, hres)
        if os.environ.get("MK_NOFIN"):
            for tt in range(NTO):
                S.dma("sp", K.out[tt * 128:(tt + 1) * 128, :], hres[:, tt, :])
            return
        with ExitStack() as es1:
            sb1 = (lambda *a: K.sb(es1, *a))
            gf = sb1("gf", [128, D], F32)
            S.dma("sp", gf[:], I.gf)
            junk = sb1("junk2", [128, D], F32)
            ss = sb1("ss2", [128, max(NTO, 8)], F32)
            ms = sb1("ms2", [128, max(NTO, 8)], F32)
            rstd = sb1("rstd2", [128, max(NTO, 8)], F32)
            ot = [sb1(f"ot{j}", [128, D], F32) for j in range(2)]
            for tt in range(NTO):
                sumsq(K, junk, hres[:, tt, :], ss[:, tt:tt + 1])
                rms_rstd(K, ss[:, tt:tt + 1], ms[:, tt:tt + 1], rstd[:, tt:tt + 1])
                S.I("dve", "scalar_tensor_tensor", out=ot[tt % 2][:], in0=hres[:, tt, :],
                    scalar=rstd[:, tt:tt + 1], in1=gf[:], op0=ALU.mult, op1=ALU.mult)
                S.dma("sp", K.out[tt * 128:(tt + 1) * 128, :], ot[tt % 2][:])


def rep(v, n=128):
    v = np.asarray(v, np.float32).reshape(1, -1)
    return np.ascontiguousarray(np.broadcast_to(v, (n, v.shape[1])))


def make_in_maps(inp):
    maps = []
    for c in range(8):
        b, tq = c // 4, c % 4
        m = {}
        m["in_x"] = np.ascontiguousarray(inp["x"][b, :S_])
        m["in_xo"] = np.ascontiguousarray(inp["x"][b, tq * TOWN:(tq + 1) * TOWN])
        m["in_g1"] = rep(inp["norm1_g"][0])
        m["in_win"] = np.ascontiguousarray(inp["w_in"][0])
        m["in_wout"] = np.ascontiguousarray(inp["w_out"][0])
        m["in_g2"] = rep(inp["norm2_g"][0])
        m["in_gf"] = rep(inp["final_g"])
        m["in_ident"] = np.eye(128, dtype=np.float32)
        m.update(attn_consts())
        m.update(rwkv_consts(inp))
        m["in_wq"] = np.ascontiguousarray(inp["peer_wq"][0])
        m["in_subk"] = np.ascontiguousarray(inp["peer_sub_keys"][0].reshape(16, 128, 128))
        m["in_u"] = np.ascontiguousarray(inp["peer_u"][0])
        m["in_v"] = np.ascontiguousarray(inp["peer_v"][0])
        m["in_iota16"] = rep(np.arange(16))
        f = np.arange(128)[:, None] + 128 * np.arange(16)[None, :]
        m["in_ridx"] = (f * 4 + tq).astype(np.int32)
        maps.append(m)
    return maps


_AC = None


def attn_consts():
    global _AC
    if _AC is not None:
        return _AC
    NB = S_ // 256
    n = np.arange(16)[None, :]
    qb = np.arange(16)[:, None]
    past = (n < qb) & (n < NB)
    pb = np.where(past, 0.0, -1e30).astype(np.float32)
    pm = past.astype(np.float32)
    own = (n == qb).astype(np.float32)
    p = np.arange(128)[:, None, None]
    i = np.arange(NT)[None, :, None]
    nn = np.arange(16)[None, None, :]
    Dm = (256 * nn - (128 * i + p)).astype(np.float64)
    dhi = 64.0 * np.floor(Dm / 64.0)
    dlo = Dm - dhi
    esel = np.zeros((32, 16, 128), np.float32)
    for nb in range(16):
        esel[nb, nb, :] = 1.0
        esel[16 + nb, nb, :] = 1.0
    kb = (np.arange(2)[None, :] * 128 + np.arange(128)[:, None]).astype(np.float32)
    k = np.arange(128)[:, None, None]
    r = np.arange(4)[None, :, None]
    q = np.arange(512)[None, None, :]
    cm = np.where((r * 128 + k) <= q, 0.0, -30000.0).astype(np.float32)
    _AC = {
        "in_pb": rep(pb.reshape(-1)), "in_pm": rep(pm.reshape(-1)), "in_own": rep(own.reshape(-1)),
        "in_dhi": np.ascontiguousarray(dhi.reshape(128, -1).astype(np.float32)),
        "in_dlo": np.ascontiguousarray(dlo.reshape(128, -1).astype(np.float32)),
        "in_esel": esel.reshape(32, -1), "in_kb": kb, "in_cm": np.ascontiguousarray(cm.reshape(128, -1)),
        "in_ones": np.ones((128, 128), np.float32),
    }
    return _AC


def rwkv_consts(inp):
    cols = lambda v: np.ascontiguousarray(np.asarray(v, np.float32).reshape(8, 128).T)
    mu = inp["rwkv_mu"][0]
    mul = np.zeros((128, 4), np.float32)
    mul[:96, 0] = mu[3072:3168]
    mul[:96, 1] = mu[3168:3264]
    mul[:, 2] = mu[3264:3392]
    mul[:, 3] = mu[3392:3520]
    prm = np.zeros((128, 10, 8), np.float32)
    prm[:, 0] = cols(mu[0:1024]); prm[:, 1] = cols(mu[1024:2048]); prm[:, 2] = cols(mu[2048:3072])
    prm[:, 4] = cols(inp["rwkv_k_k"][0]); prm[:, 5] = cols(inp["rwkv_k_a"][0]); prm[:, 6] = cols(inp["rwkv_r_k"][0].reshape(-1))
    prm[:, 7] = cols(inp["rwkv_ln_g"][0]); prm[:, 8] = cols(inp["rwkv_ln_b"][0])
    s_ = np.arange(128)[:, None]; t_ = np.arange(128)[None, :]
    strict = (s_ < t_).astype(np.float32); incl = (s_ <= t_).astype(np.float32)
    m2 = np.concatenate([strict, incl, strict, incl], axis=1)
    bones = np.kron(np.eye(2, dtype=np.float32), np.ones((64, 64), np.float32))
    seg = min(2048, S_)
    rst = np.ones((128, seg), np.float32); rst[:, ::128] = 0.0
    return {"in_w2": np.ascontiguousarray(inp["rwkv_w2"][0]), "in_a2": np.ascontiguousarray(inp["rwkv_a2"][0]),
            "in_g2r": np.ascontiguousarray(inp["rwkv_g2"][0]), "in_mul": mul, "in_w0": cols(inp["rwkv_w0"][0]),
            "in_a0": cols(inp["rwkv_a0"][0]), "in_bones": bones, "in_mask2": m2,
            "in_masklt": (s_ > t_).astype(np.float32), "in_rst": rst, "in_prm": prm.reshape(128, 80)}


_NC = None
K_last = {}


def kernel(**inputs):
    global _NC
    inp = {k: np.asarray(v) for k, v in inputs.items()}
    t0 = time.time()
    if _NC is None:
        _NC = build()
    K_last["tb"] = time.time() - t0
    maps = make_in_maps(inp)
    used = set(_NC._mk_declared)
    maps = [{k: v for k, v in m.items() if k in used} for m in maps]
    t1 = time.time()
    res = run_bass_kernel_spmd(_NC, maps, core_ids=list(range(8)))
    K_last["res"] = res
    out = np.zeros((2, S_, D), np.float32)
    print("timing build/run", K_last.get("tb"), time.time() - t1)
    for c in range(8):
        b, tq = c // 4, c % 4
        out[b, tq * TOWN:(tq + 1) * TOWN] = res.results[c]["out"]
    return out
```

```python
import os
import time
from contextlib import ExitStack
import numpy as np
import concourse.bass as bass
import concourse.mybir as mybir
from concourse.bass_utils import run_bass_kernel_spmd

F32 = mybir.dt.float32
BF16 = mybir.dt.bfloat16
I32 = mybir.dt.int32
U32 = mybir.dt.uint32
AF = mybir.ActivationFunctionType
ALU = mybir.AluOpType
AX = mybir.AxisListType

D = 2048
S_ = int(os.environ.get("MK_S", "4096"))
NT = S_ // 128
TOWN = S_ // 4
STG = min(2048, S_)
NTO = TOWN // 128
A_W = 1024
R_W = 1024
IN_COLS = 6592
RW_COLS = 3520
UPTO = int(os.environ.get("MK_UPTO", "99"))
DBG = os.environ.get("MK_DBG", "")
CGS = [int(v) for v in os.environ.get("MK_CGS", "").split(",") if v]


class Sched:
    def __init__(self, nc, n_dma_sems=32):
        self.nc = nc
        self.engs = {"pe": nc.tensor, "act": nc.scalar, "dve": nc.vector,
                     "pool": nc.gpsimd, "sp": nc.sync}
        self.sem = {k: nc.alloc_semaphore(name=f"s_{k}") for k in self.engs}
        self.cnt = {k: 0 for k in self.engs}
        self.seen = {k: {} for k in self.engs}
        self.dma_sems = [nc.alloc_semaphore(name=f"s_dma{i}") for i in range(n_dma_sems)]
        self.dma_cnt = [0] * n_dma_sems
        self.dma_next = 0
        self.res = {}
        self.ninst = 0
        self.nwait = 0

    def _semh(self, sk):
        return self.sem[sk] if isinstance(sk, str) else self.dma_sems[sk[1]]

    def _need(self, e, sk, val):
        if val <= 0 or self.seen[e].get(sk, 0) >= val:
            return
        self.engs[e].wait_ge(self._semh(sk), val)
        self.seen[e][sk] = val
        self.nwait += 1

    @staticmethod
    def _key(x):
        return x if isinstance(x, str) else x.tensor.name

    def _r(self, key):
        return self.res.setdefault(key, {"w": {}, "r": {}})

    def _deps(self, e, reads, writes, me):
        for k in reads:
            for sk, v in self._r(k)["w"].items():
                self._need(e, sk, v)
        for k in writes:
            rr = self._r(k)
            for sk, v in rr["w"].items():
                if sk != me:
                    self._need(e, sk, v)
            for sk, v in rr["r"].items():
                if sk != me:
                    self._need(e, sk, v)

    def _commit(self, reads, writes, sk, val):
        for k in reads:
            self._r(k)["r"][sk] = val
        for k in writes:
            self._r(k)["w"][sk] = val

    def I(self, e, method, *args, w=("out", "accum_out"), xr=(), xw=(), **kw):
        reads, writes = [], []
        for name, v in kw.items():
            if isinstance(v, bass.AP):
                if v.tensor.name.startswith("in_"):
                    continue
                (writes if name in w else reads).append(v.tensor.name)
        for v in args:
            if isinstance(v, bass.AP):
                writes.append(v.tensor.name)
        reads += [self._key(x) for x in xr]
        writes += [self._key(x) for x in xw]
        self._deps(e, reads, writes, e)
        inst = getattr(self.engs[e], method)(*args, **kw)
        self.cnt[e] += 1
        inst.then_inc(self.sem[e], 1)
        self._commit(reads, writes, e, self.cnt[e])
        self.ninst += 1
        return inst

    def dma(self, q, out, in_, method="dma_start", xr=(), xw=(), **kw):
        reads, writes = [], []
        if not in_.tensor.name.startswith("in_"):
            reads.append(in_.tensor.name)
        writes.append(out.tensor.name)
        for name, v in kw.items():
            if isinstance(v, bass.AP):
                reads.append(v.tensor.name)
        reads += [self._key(x) for x in xr]
        writes += [self._key(x) for x in xw]
        i = self.dma_next
        self.dma_next = (self.dma_next + 1) % len(self.dma_sems)
        sk = ("dma", i)
        self._need(q, sk, self.dma_cnt[i])
        self._deps(q, reads, writes, sk)
        inst = getattr(self.engs[q], method)(out=out, in_=in_, **kw)
        self.dma_cnt[i] += 16
        inst.then_inc(self.dma_sems[i], 16)
        self._commit(reads, writes, sk, self.dma_cnt[i])
        self.ninst += 1
        return inst

    def barrier(self):
        for e in self.engs:
            self.wait_all(e)

    def wait_all(self, e):
        for k in self.engs:
            if k != e:
                self._need(e, k, self.cnt[k])
        for i in range(len(self.dma_sems)):
            self._need(e, ("dma", i), self.dma_cnt[i])


class Ctx:
    pass


def build():
    nc = bass.Bass("TRN2", target_bir_lowering=False)
    S = Sched(nc)
    K = Ctx()
    K.nc, K.S = nc, S
    K.uid = 0

    K.declared = []

    def din(name, shape, dt=F32):
        K.declared.append("in_" + name)
        return nc.dram_tensor("in_" + name, list(shape), dt, kind="ExternalInput").ap()

    def dscr(name, shape, dt):
        return nc.dram_tensor(name, list(shape), dt, kind="Internal").ap()

    K.din, K.dscr = din, dscr
    I = Ctx()
    K.I = I
    I.x = din("x", [S_, D])
    I.xo = din("xo", [TOWN, D])
    I.g1 = din("g1", [128, D])
    I.win = din("win", [D, IN_COLS])
    I.wout = din("wout", [D, D])
    I.g2 = din("g2", [128, D])
    I.gf = din("gf", [128, D])
    I.ident = din("ident", [128, 128])
    I.ridx = din("ridx", [128, 16], I32)
    I.pb = din("pb", [128, 16 * 16])
    I.pm = din("pm", [128, 16 * 16])
    I.own = din("own", [128, 16 * 16])
    I.dhi = din("dhi", [128, NT * 16])
    I.dlo = din("dlo", [128, NT * 16])
    I.esel = din("esel", [32, 16 * 128])
    I.kb = din("kb", [128, 2])
    I.cm = din("cm", [128, 4 * 512])
    I.ones = din("ones", [128, 128])
    I.w2 = din("w2", [96, 1024])
    I.a2 = din("a2", [96, 1024])
    I.rg2 = din("g2r", [256, 1024])
    I.mul = din("mul", [128, 4])
    I.w0 = din("w0", [128, 8])
    I.a0 = din("a0", [128, 8])
    I.bones = din("bones", [128, 128])
    I.mask2 = din("mask2", [128, 512])
    I.masklt = din("masklt", [128, 128])
    I.rst = din("rst", [128, min(2048, S_)])
    I.prm = din("prm", [128, 80])
    I.wq = din("wq", [D, 2048])
    I.subk = din("subk", [16, 128, 128])
    I.u = din("u", [16384, D])
    I.v = din("v", [16384, D])
    I.iota16 = din("iota16", [128, 16])
    out = nc.dram_tensor("out", [TOWN, D], F32, kind="ExternalOutput").ap()
    K.out = out
    Z = Ctx()
    K.Z = Z
    Z.qT = dscr("z_qT", [8, 128, S_], BF16)
    Z.kT = dscr("z_kT", [8, 128, S_], BF16)
    Z.v = dscr("z_v", [8, S_, 128], BF16)
    Z.rw = dscr("z_rw", [RW_COLS, S_], F32)
    Z.mix = dscr("z_mix", [D * 4, TOWN], BF16)
    Z.ub = dscr("z_ub", [16384, D], BF16)
    Z.vb = dscr("z_vb", [16384, D], BF16)
    Z.lw = dscr("z_lw", [1024, S_], F32)
    Z.a = dscr("z_a", [1024, S_], F32)
    Z.g = dscr("z_g", [1024, S_], F32)

    es_all = ExitStack()

    def sb(es, name, shape, dt):
        return es.enter_context(nc.sbuf_tensor(name, list(shape), dt))

    def ps(es, name, shape, dt=F32):
        return es.enter_context(nc.psum_tensor(name, list(shape), dt))

    K.sb, K.ps = sb, ps
    C = Ctx()
    K.C = C
    C.identf = sb(es_all, "identf", [128, 128], F32)
    C.identb = sb(es_all, "identb", [128, 128], BF16)
    S.dma("sp", C.identf[:], I.ident)
    S.I("dve", "tensor_copy", out=C.identb[:], in_=C.identf[:])

    if UPTO >= 1:
        phase_norm_proj(K)
        S.barrier()
    if UPTO >= 2:
        phase_zero_mix(K)
        S.barrier()
    if UPTO >= 6:
        for r in range(16):
            rs_ = slice(r * 1024, (r + 1) * 1024)
            S.dma("pool", Z.ub[rs_, :], I.u[rs_, :])
            S.dma("pool", Z.vb[rs_, :], I.v[rs_, :])
    if UPTO >= 3 and not os.environ.get("MK_NOATT"):
        phase_attn(K)
        S.barrier()
    if UPTO >= 4:
        phase_rwkv_lora(K)
        S.barrier()
        phase_rwkv_heads(K)
        S.barrier()
    if UPTO >= 5:
        phase_tail(K)
        S.barrier()
    if DBG:
        dbg_dump(K)
    S.wait_all("sp")
    es_all.close()
    print("ninst", S.ninst, "nwait", S.nwait)
    nc._mk_declared = list(K.declared)
    return nc


def dbg_dump(K):
    nc, S, Z = K.nc, K.S, K.Z
    items = {"qT0": Z.qT[0], "kT7": Z.kT[7], "v3": Z.v[3], "rw0": Z.rw[0:128, :], "rwl": Z.rw[RW_COLS - 64:RW_COLS, :],
             "mix": Z.mix, "mixa": Z.mix[0:4 * 256, :], "mixr": Z.mix[4 * 1024:4 * 1024 + 4 * 256, :],
             "lw": Z.lw[0:128, :], "za": Z.a[0:128, :], "zg": Z.g[0:128, :]}
    for name in DBG.split(","):
        if name not in items:
            continue
        src = items[name]
        dst = nc.dram_tensor("dbg_" + name, list(src.shape), src.dtype, kind="ExternalOutput").ap()
        S.dma("sp", dst, src)


def sumsq(K, junk, src, ss_col):
    S = K.S
    S.I("act", "activation", out=junk[:], in_=src, func=AF.Square)
    S.I("dve", "tensor_reduce", out=ss_col, in_=junk[:], axis=AX.X, op=ALU.add)


def rms_rstd(K, ss_col, tmp_col, out_col):
    S = K.S
    S.I("dve", "tensor_scalar", out=tmp_col, in0=ss_col, scalar1=1.0 / D, scalar2=1e-6,
        op0=ALU.mult, op1=ALU.add)
    S.I("act", "activation", out=tmp_col, in_=tmp_col, func=AF.Sqrt)
    S.I("dve", "reciprocal", out=out_col, in_=tmp_col)


def phase_norm_proj(K):
    nc, S, I, Z, C = K.nc, K.S, K.I, K.Z, K.C
    with ExitStack() as es:
        sb, ps = (lambda *a: K.sb(es, *a)), (lambda *a: K.ps(es, *a))
        xnT = sb("xnT", [128, 16, S_], BF16)
        ss = sb("ss", [128, NT], F32)
        ms = sb("ms", [128, NT], F32)
        rstd = sb("rstd", [128, NT], F32)
        xn = [sb(f"xn{j}", [128, D], BF16) for j in range(2)]
        with ExitStack() as es1:
            sb1, ps1 = (lambda *a: K.sb(es1, *a)), (lambda *a: K.ps(es1, *a))
            g1 = sb1("g1", [128, D], F32)
            S.dma("sp", g1[:], I.g1)
            xb = [sb1(f"xb{j}", [128, D], F32) for j in range(2)]
            junk = sb1("junk", [128, D], F32)
            pT = [ps1(f"pT{j}", [128, 16, 128], BF16) for j in range(2)]
            for i in range(NT):
                j = i % 2
                S.dma("sp", xb[j][:], I.x[i * 128:(i + 1) * 128, :])
                sumsq(K, junk, xb[j][:], ss[:, i:i + 1])
                rms_rstd(K, ss[:, i:i + 1], ms[:, i:i + 1], rstd[:, i:i + 1])
                S.I("dve", "scalar_tensor_tensor", out=xn[j][:], in0=xb[j][:], scalar=rstd[:, i:i + 1],
                    in1=g1[:], op0=ALU.mult, op1=ALU.mult)
                for kc in range(16):
                    S.I("pe", "transpose", out=pT[j][:, kc, :], in_=xn[j][:, kc * 128:(kc + 1) * 128],
                        identity=C.identb[:])
                S.I("act" if i % 2 == 0 else "dve", "copy" if i % 2 == 0 else "tensor_copy",
                    out=xnT[:, :, i * 128:(i + 1) * 128], in_=pT[j][:])
        S.barrier()
        if "stat" in DBG:
            for nm, t in (("ss", ss), ("ms", ms), ("rstd", rstd)):
                dst = nc.dram_tensor("dbg_" + nm, [128, NT], F32, kind="ExternalOutput").ap()
                S.dma("sp", dst, t[:])
            dst = nc.dram_tensor("dbg_xnl", [128, D], BF16, kind="ExternalOutput").ap()
            S.dma("sp", dst, xn[(NT - 1) % 2][:])
        if "xnT" in DBG:
            dst = nc.dram_tensor("dbg_xnT", [128, 16, S_], BF16, kind="ExternalOutput").ap()
            S.dma("sp", dst, xnT[:])
        wb = [sb(f"wb{j}", [128, 16, 512], BF16) for j in range(2)]
        stg_b = [sb(f"stgb{j}", [128, STG], BF16) for j in range(2)]
        stg_f = [sb(f"stgf{j}", [128, STG], F32) for j in range(2)]
        pp = [ps(f"pp{j}", [128, 512], F32) for j in range(4)]
        K.pcnt = 0
        K.ecnt = 0
        K.scnt = 0
        win_v = I.win.rearrange("(kc p) c -> p kc c", p=128)
        ngroups = (IN_COLS + 511) // 512
        for cg in range(ngroups):
            if CGS and cg not in CGS:
                continue
            c0 = cg * 512
            ncol = min(512, IN_COLS - c0)
            w = wb[cg % 2]
            S.dma("pool", w[:, :, 0:ncol], win_v[:, :, c0:c0 + ncol])
            is_v = 2048 <= c0 < 3072
            if is_v:
                h0 = (c0 - 2048) // 128
                for tt in range(NT):
                    p = pp[K.pcnt % 4]; K.pcnt += 1
                    for kc in range(16):
                        S.I("pe", "matmul", out=p[:, :], lhsT=xnT[:, kc, tt * 128:(tt + 1) * 128],
                            rhs=w[:, kc, :], start=(kc == 0), stop=(kc == 15))
                    st = stg_b[K.scnt % 2]; K.scnt += 1
                    evac(K, st[:, 0:512], p[:, :])
                    S.dma("sp", Z.v[h0:h0 + 4, tt * 128:(tt + 1) * 128, :].rearrange("h t d -> t h d"),
                          st[:, 0:512].rearrange("t (h d) -> t h d", h=4))
                continue
            for cc in range((ncol + 127) // 128):
                m = min(128, ncol - cc * 128)
                col = c0 + cc * 128
                f32_out = col >= 3072
                for half in range(S_ // STG):
                    st = (stg_f if f32_out else stg_b)[K.scnt % 2]; K.scnt += 1
                    for tg in range(STG // 512):
                        p = pp[K.pcnt % 4]; K.pcnt += 1
                        t0 = half * STG + tg * 512
                        for kc in range(16):
                            S.I("pe", "matmul", out=p[0:m, :], lhsT=w[:, kc, cc * 128:cc * 128 + m],
                                rhs=xnT[:, kc, t0:t0 + 512], start=(kc == 0), stop=(kc == 15))
                        evac(K, st[0:m, tg * 512:(tg + 1) * 512], p[0:m, :],
                             scale=(128 ** -0.5 if col < 1024 else None))
                    hs = slice(half * STG, (half + 1) * STG)
                    if col < 1024:
                        S.dma("sp", Z.qT[col // 128, :, hs], st[:, :])
                    elif col < 2048:
                        S.dma("sp", Z.kT[(col - 1024) // 128, :, hs], st[:, :])
                    else:
                        r0 = col - 3072
                        S.dma("sp", Z.rw[r0:r0 + m, hs], st[0:m, :])


def evac(K, dst, src, scale=None):
    S = K.S
    K.ecnt += 1
    if scale is not None:
        if K.ecnt % 2 == 0:
            S.I("act", "activation", out=dst, in_=src, func=AF.Copy, scale=float(scale))
        else:
            S.I("dve", "tensor_scalar", out=dst, in0=src, scalar1=float(scale), scalar2=None, op0=ALU.mult)
    else:
        if K.ecnt % 2 == 0:
            S.I("act", "copy", out=dst, in_=src)
        else:
            S.I("dve", "tensor_copy", out=dst, in_=src)


def phase_zero_mix(K):
    nc, S, Z = K.nc, K.S, K.Z
    with ExitStack() as es:
        zt = K.sb(es, "zt", [128, TOWN], BF16)
        S.I("dve", "memset", zt[:], 0.0)
        mv = Z.mix.rearrange("(a p) t -> a p t", p=128)
        for a in range(D * 4 // 128):
            S.dma("sp", mv[a], zt[:])
        S.barrier()


def phase_attn(K):
    nc, S, I, Z, C = K.nc, K.S, K.I, K.Z, K.C
    NB = S_ // 256
    NQ = S_ // 512
    with ExitStack() as es:
        sb, ps = (lambda *a: K.sb(es, *a)), (lambda *a: K.ps(es, *a))
        pb = sb("a_pb", [128, 16, 16], F32); S.dma("sp", pb[:], I.pb.rearrange("p (a b) -> p a b", a=16))
        pm = sb("a_pm", [128, 16, 16], F32); S.dma("sp", pm[:], I.pm.rearrange("p (a b) -> p a b", a=16))
        own = sb("a_own", [128, 16, 16], F32); S.dma("sp", own[:], I.own.rearrange("p (a b) -> p a b", a=16))
        dhi = sb("a_dhi", [128, NT, 16], F32); S.dma("sp", dhi[:], I.dhi.rearrange("p (a b) -> p a b", b=16))
        dlo = sb("a_dlo", [128, NT, 16], F32); S.dma("sp", dlo[:], I.dlo.rearrange("p (a b) -> p a b", b=16))
        esel = sb("a_esel", [32, 16, 128], BF16); S.dma("pool", esel[:], I.esel.rearrange("p (a b) -> p a b", a=16))
        kb = sb("a_kb", [128, 2], F32); S.dma("sp", kb[:], I.kb)
        cm = sb("a_cm", [128, 4, 512], BF16); S.dma("pool", cm[:], I.cm.rearrange("p (a b) -> p a b", a=4))
        ones = sb("a_ones", [128, 128], BF16); S.dma("pool", ones[:], I.ones)
        qT = sb("a_qT", [128, S_], BF16)
        kT = sb("a_kT", [128, S_], BF16)
        v = sb("a_v", [128, NT, 128], BF16)
        mbt = sb("a_mbt", [32, S_], BF16)
        km32 = sb("a_km32", [128, 16], F32)
        kmb = sb("a_kmb", [128, 16], BF16)
        gm = sb("a_gm", [128, 16], F32)
        m8 = sb("a_m8", [128, 8], F32)
        sel = sb("a_sel", [128, 16], F32)
        mbv = sb("a_mbv", [128, 16], F32)
        mb = sb("a_mb", [128, 32], F32)
        kbh = sb("a_kbh", [128, 2], F32)
        PT = [sb(f"a_PT{j}", [128, 512], BF16) for j in range(2)]
        rD = sb("a_rD", [128, 512], F32)
        Ost = [sb(f"a_O{j}", [128, 512], BF16) for j in range(2)]
        pg = ps("a_pg", [128, 16], F32)
        pt = ps("a_pt", [32, 128], F32)
        pS = [ps(f"a_pS{j}", [128, 512], F32) for j in range(2)]
        pO = ps("a_pO", [128, 512], F32)
        pD = ps("a_pD", [128, 512], F32)
        mixv = Z.mix.rearrange("(f q) t -> f q t", q=4)
        S.I("dve", "memset", km32[:], 0.0)
        for h in range(8):
            slope = 2.0 ** (-(h + 1))
            S.dma("sp", qT[:], Z.qT[h])
            S.dma("sp", kT[:], Z.kT[h])
            S.dma("sp", v[:], Z.v[h].rearrange("(j p) d -> p j d", p=128))
            S.I("dve", "tensor_reduce", out=km32[:, 0:NB], in_=kT[:].rearrange("p (n l) -> p n l", l=256),
                axis=AX.X, op=ALU.add)
            S.I("dve", "tensor_scalar", out=kmb[:], in0=km32[:], scalar1=1.0 / 256, scalar2=None, op0=ALU.mult)
            S.I("dve", "tensor_scalar", out=kbh[:], in0=kb[:], scalar1=slope, scalar2=None, op0=ALU.mult)
            for i in range(NT):
                qb = i // 2
                S.I("pe", "matmul", out=pg[:, :], lhsT=qT[:, i * 128:(i + 1) * 128], rhs=kmb[:, :],
                    start=True, stop=True)
                S.I("dve", "tensor_tensor", out=gm[:], in0=pg[:, :], in1=pb[:, qb, :], op=ALU.add)
                S.I("dve", "max", out=m8[:], in_=gm[:])
                S.I("dve", "scalar_tensor_tensor", out=sel[:], in0=gm[:], scalar=m8[:, 2:3], in1=pm[:, qb, :],
                    op0=ALU.is_ge, op1=ALU.mult)
                S.I("dve", "tensor_tensor", out=sel[:], in0=sel[:], in1=own[:, qb, :], op=ALU.add)
                S.I("dve", "tensor_scalar", out=mbv[:], in0=sel[:], scalar1=-1.0, scalar2=30000.0,
                    op0=ALU.add, op1=ALU.mult)
                S.I("dve", "scalar_tensor_tensor", out=mb[:, 0:16], in0=dhi[:, i, :], scalar=slope, in1=mbv[:],
                    op0=ALU.mult, op1=ALU.add)
                S.I("dve", "tensor_scalar", out=mb[:, 16:32], in0=dlo[:, i, :], scalar1=slope, scalar2=None,
                    op0=ALU.mult)
                S.I("pe", "transpose", out=pt[:, :], in_=mb[:, :], identity=C.identf[:])
                S.I("act", "copy", out=mbt[:, i * 128:(i + 1) * 128], in_=pt[:, :])
            for Q in range(NQ):
                nkt = 4 * (Q + 1)
                qs = slice(Q * 512, (Q + 1) * 512)

                def qk(kt):
                    p = pS[kt % 2]
                    S.I("pe", "matmul", out=p[:, :], lhsT=kT[:, kt * 128:(kt + 1) * 128], rhs=qT[:, qs],
                        start=True, stop=False)
                    diag = kt >= 4 * Q
                    S.I("pe", "matmul", out=p[:, :], lhsT=esel[:, kt // 2, :], rhs=mbt[:, qs],
                        start=False, stop=not diag)
                    if diag:
                        S.I("pe", "matmul", out=p[:, :], lhsT=C.identb[:], rhs=cm[:, kt - 4 * Q, :],
                            start=False, stop=True)

                qk(0)
                for kt in range(nkt):
                    if kt + 1 < nkt:
                        qk(kt + 1)
                    P = PT[kt % 2]
                    S.I("act", "activation", out=P[:], in_=pS[kt % 2][:, :], func=AF.Exp,
                        bias=kbh[:, (kt % 2):(kt % 2) + 1], scale=1.0)
                    S.I("pe", "matmul", out=pO[:, :], lhsT=v[:, kt, :], rhs=P[:], start=(kt == 0),
                        stop=(kt == nkt - 1))
                    S.I("pe", "matmul", out=pD[:, :], lhsT=ones[:], rhs=P[:], start=(kt == 0),
                        stop=(kt == nkt - 1))
                S.I("dve", "reciprocal", out=rD[:], in_=pD[:, :])
                O = Ost[Q % 2]
                S.I("dve", "tensor_tensor", out=O[:], in0=pO[:, :], in1=rD[:], op=ALU.mult)
                piece = min(512, TOWN)
                for pc in range(512 // piece):
                    t0 = Q * 512 + pc * piece
                    S.dma("sp", mixv[h * 128:(h + 1) * 128, t0 // TOWN, (t0 % TOWN):(t0 % TOWN) + piece],
                          O[:, pc * piece:(pc + 1) * piece])


SEG = min(2048, S_)
NCS = SEG // 128


def shifted_load(K, raw, halo, mu_col, rows, r0, t0, out, tmp, act=None):
    S, Z = K.S, K.Z
    if t0 == 0:
        S.I("dve", "memset", halo[0:rows, 0:1], 0.0)
        S.dma("sp", halo[0:rows, 1:SEG + 1], Z.rw[r0:r0 + rows, 0:SEG])
    else:
        S.dma("sp", halo[0:rows, 0:SEG + 1], Z.rw[r0:r0 + rows, t0 - 1:t0 + SEG])
    S.I("dve", "tensor_tensor", out=tmp[0:rows, :], in0=halo[0:rows, 0:SEG], in1=halo[0:rows, 1:SEG + 1],
        op=ALU.subtract)
    S.I("dve", "scalar_tensor_tensor", out=out, in0=tmp[0:rows, :], scalar=mu_col, in1=halo[0:rows, 1:SEG + 1],
        op0=ALU.mult, op1=ALU.add)


def phase_rwkv_lora(K):
    nc, S, I, Z, C = K.nc, K.S, K.I, K.Z, K.C
    with ExitStack() as es:
        sb, ps = (lambda *a: K.sb(es, *a)), (lambda *a: K.ps(es, *a))
        w2b = sb("l_w2", [96, 1024], BF16); S.dma("pool", w2b[:], I.w2)
        a2b = sb("l_a2", [96, 1024], BF16); S.dma("pool", a2b[:], I.a2)
        g2b = sb("l_g2", [128, 2, 1024], BF16); S.dma("pool", g2b[:], I.rg2.rearrange("(k p) c -> p k c", p=128))
        mul = sb("l_mu", [128, 4], F32); S.dma("sp", mul[:], I.mul)
        w0 = sb("l_w0", [128, 8], F32); S.dma("sp", w0[:], I.w0)
        a0 = sb("l_a0", [128, 8], F32); S.dma("sp", a0[:], I.a0)
        halo = sb("l_halo", [128, SEG + 1], F32)
        tmp = sb("l_tmp", [128, SEG], F32)
        xs = sb("l_xs", [128, SEG], F32)
        txw = sb("l_txw", [96, SEG], BF16)
        xab = sb("l_xab", [96, SEG], BF16)
        sxg = sb("l_sxg", [128, 2, SEG], BF16)
        stg = [sb(f"l_stg{j}", [128, SEG], F32) for j in range(2)]
        pp = [ps(f"l_pp{j}", [128, 512], F32) for j in range(4)]
        n = 0
        ns = 0
        for seg in range(S_ // SEG):
            t0 = seg * SEG
            shifted_load(K, Z.rw, halo, mul[0:96, 0:1], 96, 3072, t0, xs[0:96, :], tmp)
            S.I("act", "activation", out=txw[:], in_=xs[0:96, :], func=AF.Tanh)
            shifted_load(K, Z.rw, halo, mul[0:96, 1:2], 96, 3168, t0, xs[0:96, :], tmp)
            S.I("act", "copy", out=xab[:], in_=xs[0:96, :])
            for kc in range(2):
                shifted_load(K, Z.rw, halo, mul[:, 2 + kc:3 + kc], 128, 3264 + kc * 128, t0, xs[:, :], tmp)
                S.I("act", "activation", out=sxg[:, kc, :], in_=xs[:, :], func=AF.Sigmoid)
            for cc in range(8):
                cs = slice(cc * 128, (cc + 1) * 128)
                for which in range(3):
                    st = stg[ns % 2]; ns += 1
                    for tg in range(SEG // 512):
                        ts_ = slice(tg * 512, (tg + 1) * 512)
                        p = pp[n % 4]; n += 1
                        if which == 0:
                            S.I("pe", "matmul", out=p[:, :], lhsT=w2b[:, cs], rhs=txw[:, ts_], start=True, stop=True)
                            S.I("act", "activation", out=st[:, ts_], in_=p[:, :], func=AF.Sigmoid,
                                bias=w0[:, cc:cc + 1], scale=1.0)
                        elif which == 1:
                            S.I("pe", "matmul", out=p[:, :], lhsT=a2b[:, cs], rhs=xab[:, ts_], start=True, stop=True)
                            S.I("act", "activation", out=st[:, ts_], in_=p[:, :], func=AF.Sigmoid,
                                bias=a0[:, cc:cc + 1], scale=1.0)
                        else:
                            for kc in range(2):
                                S.I("pe", "matmul", out=p[:, :], lhsT=g2b[:, kc, cs], rhs=sxg[:, kc, ts_],
                                    start=(kc == 0), stop=(kc == 1))
                            S.I("dve", "tensor_copy", out=st[:, ts_], in_=p[:, :])
                    if which == 0:
                        S.I("dve", "tensor_scalar", out=st[:, :], in0=st[:, :], scalar1=-0.6065306597, scalar2=None,
                            op0=ALU.mult)
                    dst = (Z.lw, Z.a, Z.g)[which]
                    S.dma("sp", dst[cs, t0:t0 + SEG], st[:, :])


def phase_rwkv_heads(K):
    nc, S, I, Z, C = K.nc, K.S, K.I, K.Z, K.C
    NPAIR = int(os.environ.get("MK_NPAIR", "8"))
    with ExitStack() as es:
        sb, ps = (lambda *a: K.sb(es, *a)), (lambda *a: K.ps(es, *a))
        bones = sb("r_bones", [128, 128], BF16); S.dma("pool", bones[:], I.bones)
        mask2 = sb("r_mask2", [128, 2, 256], F32); S.dma("sp", mask2[:], I.mask2.rearrange("p (a b) -> p a b", a=2))
        masklt = sb("r_masklt", [128, 128], F32); S.dma("sp", masklt[:], I.masklt)
        rst = sb("r_rst", [128, SEG], F32); S.dma("sp", rst[:], I.rst)
        prm = sb("r_prm", [128, 10, 8], F32); S.dma("sp", prm[:], I.prm.rearrange("p (a b) -> p a b", a=10))
        omka = sb("r_omka", [128, 8], F32)
        S.I("dve", "tensor_scalar", out=omka[:], in0=prm[:, 5, :], scalar1=-1.0, scalar2=1.0, op0=ALU.mult, op1=ALU.add)
        halo = sb("r_halo", [128, SEG + 1], F32)
        F = [sb(f"r_F{j}", [128, SEG], F32) for j in range(9)]
        gkeep = sb("r_g", [128, SEG], F32)
        bvkeep = sb("r_bv", [128, SEG], F32)
        AR = sb("r_AR", [128, NCS, 2, 128], BF16)
        btb = sb("r_bt", [128, SEG], BF16)
        ktb = sb("r_kt", [128, SEG], BF16)
        bhb = sb("r_bh", [128, SEG], BF16)
        khb = sb("r_kh", [128, SEG], BF16)
        vbb = sb("r_vb", [128, SEG], BF16)
        sqb = sb("r_sqb", [128, SEG], BF16)
        gamC = sb("r_gamC", [128, NCS], F32)
        ostg = sb("r_ostg", [128, SEG], BF16)
        STh = [[sb(f"r_ST{h}{j}", [128, 64], F32) for j in range(2)] for h in range(2)]
        STbh = [sb(f"r_STb{h}", [128, 64], BF16) for h in range(2)]
        NU = 4
        TRg = [sb(f"r_TR{g}", [128, 3, 128], BF16) for g in range(2)]
        MM = [sb(f"r_MM{u}", [128, 2, 256], BF16) for u in range(NU)]
        Pm = [[sb(f"r_P{u}{j}", [128, 128], BF16) for j in range(2)] for u in range(NU)]
        PTm = [[sb(f"r_PT{u}{j}", [128, 128], BF16) for j in range(2)] for u in range(NU)]
        Tm = [[sb(f"r_T{u}{j}", [128, 128], BF16) for j in range(2)] for u in range(NU)]
        ZT = [sb(f"r_ZT{h}", [128, 64], BF16) for h in range(2)]
        UT = [sb(f"r_UT{h}", [128, 64], BF16) for h in range(2)]
        ysb = sb("r_ysb", [128, 2, 64], F32)
        ysq = sb("r_ysq", [128, 2, 64], F32)
        st1 = sb("r_st1", [128, 8], F32)
        ynb = sb("r_ynb", [128, 128], BF16)
        of1 = sb("r_of1", [128, 128], F32)
        pTb = ps("r_pTb", [128, 1024], BF16)
        pM = ps("r_pM", [128, 2, 256], F32)
        pDb = [ps(f"r_pD{u}", [128, 4, 128], F32) for u in range(NU)]
        pCh = [ps(f"r_pC{h}", [128, 8, 64], F32) for h in range(2)]
        pX = pM[:].rearrange("p a b -> p (a b)")
        mixv = Z.mix.rearrange("(f q) t -> f q t", q=4)

        for hp in range(NPAIR):
            cc = hp
            c0 = hp * 128
            col = lambda a: prm[:, a, cc:cc + 1]
            for hh in range(2):
                S.I("dve", "memset", STh[hh][0][:], 0.0)
                S.I("dve", "memset", STbh[hh][:], 0.0)
            cur = 0
            for seg in range(S_ // SEG):
                t0 = seg * SEG
                rs, ks, vs, lw, aa, t1, t2, t3, t4 = F
                shifted_load(K, Z.rw, halo, col(0), 128, c0, t0, rs[:, :], t1)
                shifted_load(K, Z.rw, halo, col(1), 128, 1024 + c0, t0, ks[:, :], t1)
                shifted_load(K, Z.rw, halo, col(2), 128, 2048 + c0, t0, vs[:, :], t1)
                S.dma("sp", lw[:], Z.lw[c0:c0 + 128, t0:t0 + SEG])
                S.dma("sp", aa[:], Z.a[c0:c0 + 128, t0:t0 + SEG])
                S.dma("sp", gkeep[:], Z.g[c0:c0 + 128, t0:t0 + SEG])
                S.I("dve", "tensor_scalar", out=t1[:], in0=ks[:], scalar1=col(4), scalar2=None, op0=ALU.mult)
                S.I("act", "activation", out=sqb[:], in_=t1[:], func=AF.Square)
                for tg in range(SEG // 512):
                    ts_ = slice(tg * 512, (tg + 1) * 512)
                    S.I("pe", "matmul", out=pX, lhsT=bones[:], rhs=sqb[:, ts_], start=True, stop=True)
                    S.I("dve", "tensor_scalar", out=t2[:, ts_], in0=pX, scalar1=1e-24, scalar2=None, op0=ALU.add)
                S.I("act", "activation", out=t2[:], in_=t2[:], func=AF.Sqrt)
                S.I("dve", "reciprocal", out=t2[:], in_=t2[:])
                S.I("dve", "tensor_tensor", out=t1[:], in0=t1[:], in1=t2[:], op=ALU.mult)
                S.I("dve", "tensor_scalar", out=t2[:], in0=aa[:], scalar1=col(5), scalar2=omka[:, cc:cc + 1],
                    op0=ALU.mult, op1=ALU.add)
                S.I("dve", "tensor_tensor", out=ks[:], in0=ks[:], in1=t2[:], op=ALU.mult)
                S.I("dve", "tensor_tensor", out=aa[:], in0=t1[:], in1=aa[:], op=ALU.mult)
                S.I("dve", "tensor_tensor", out=t2[:], in0=rs[:], in1=ks[:], op=ALU.mult)
                S.I("dve", "tensor_scalar", out=sqb[:], in0=t2[:], scalar1=col(6), scalar2=None, op0=ALU.mult)
                for tg in range(SEG // 512):
                    ts_ = slice(tg * 512, (tg + 1) * 512)
                    S.I("pe", "matmul", out=pX, lhsT=bones[:], rhs=sqb[:, ts_], start=True, stop=True)
                    S.I("dve", "tensor_tensor", out=bvkeep[:, ts_], in0=pX, in1=vs[:, ts_], op=ALU.mult)
                S.I("act", "copy", out=vbb[:], in_=vs[:])
                S.I("dve", "tensor_tensor_scan", out=t2[:], data0=rst[:], data1=lw[:], initial=0.0,
                    op0=ALU.mult, op1=ALU.add)
                S.I("act", "activation", out=t3[:], in_=t2[:], func=AF.Exp)
                S.I("dve", "tensor_tensor", out=AR[:, :, 1, :], in0=rs[:].rearrange("p (c t) -> p c t", t=128),
                    in1=t3[:].rearrange("p (c t) -> p c t", t=128), op=ALU.mult)
                S.I("dve", "tensor_tensor", out=t3[:], in0=t2[:], in1=lw[:], op=ALU.subtract)
                S.I("act", "activation", out=t3[:], in_=t3[:], func=AF.Exp)
                S.I("dve", "scalar_tensor_tensor", out=AR[:, :, 0, :], in0=t1[:].rearrange("p (c t) -> p c t", t=128),
                    scalar=-1.0, in1=t3[:].rearrange("p (c t) -> p c t", t=128), op0=ALU.mult, op1=ALU.mult)
                S.I("act", "activation", out=t3[:], in_=t2[:], func=AF.Exp, scale=-1.0)
                S.I("dve", "tensor_tensor", out=btb[:], in0=aa[:], in1=t3[:], op=ALU.mult)
                S.I("dve", "tensor_tensor", out=ktb[:], in0=ks[:], in1=t3[:], op=ALU.mult)
                for c in range(NCS):
                    cs = slice(c * 128, (c + 1) * 128)
                    S.I("dve", "tensor_scalar", out=t4[:, cs], in0=t2[:, cs], scalar1=-1.0,
                        scalar2=t2[:, c * 128 + 127:c * 128 + 128], op0=ALU.mult, op1=ALU.add)
                S.I("act", "activation", out=t4[:], in_=t4[:], func=AF.Exp)
                S.I("dve", "tensor_tensor", out=bhb[:], in0=aa[:], in1=t4[:], op=ALU.mult)
                S.I("dve", "tensor_tensor", out=khb[:], in0=ks[:], in1=t4[:], op=ALU.mult)
                S.I("act", "activation", out=gamC[:], in_=t2[:].rearrange("p (c t) -> p c t", t=128)[:, :, 127],
                    func=AF.Exp)
                def stageA(u, c, hh, TR):
                    cs = slice(c * 128, (c + 1) * 128)
                    rows = slice(hh * 64, hh * 64 + 64)
                    mm, pD = MM[u], pDb[u]
                    P, PT, T = Pm[u], PTm[u], Tm[u]
                    S.I("pe", "matmul", out=pM[:, 0, :], lhsT=btb[rows, cs], rhs=AR[rows, c, :, :], start=True, stop=True)
                    S.I("pe", "matmul", out=pM[:, 1, :], lhsT=ktb[rows, cs], rhs=AR[rows, c, :, :], start=True, stop=True)
                    S.I("pe", "matmul", out=pD[:, 0, :], lhsT=AR[rows, c, 0, :], rhs=btb[rows, cs], start=True, stop=True)
                    S.I("dve", "tensor_tensor", out=mm[:], in0=pM[:], in1=mask2[:], op=ALU.mult)
                    yield
                    S.I("dve", "tensor_tensor", out=PT[0][:], in0=pD[:, 0, :], in1=masklt[:], op=ALU.mult)
                    S.I("dve", "tensor_tensor", out=T[0][:], in0=mm[:, 0, 0:128], in1=C.identb[:], op=ALU.add)
                    yield
                    Pc, PTc, Tc = mm[:, 0, 0:128], PT[0][:], T[0][:]
                    for rd in range(6):
                        j = (rd + 1) % 2
                        S.I("pe", "matmul", out=pD[:, 1, :], lhsT=Pc, rhs=PTc, start=True, stop=True)
                        if rd < 5:
                            S.I("pe", "matmul", out=pD[:, 2, :], lhsT=PTc, rhs=Pc, start=True, stop=True)
                        yield
                        S.I("act", "copy", out=PT[j][:], in_=pD[:, 1, :])
                        if rd < 5:
                            S.I("act", "copy", out=P[j][:], in_=pD[:, 2, :])
                        yield
                        S.I("pe", "matmul", out=pD[:, 3, :], lhsT=PT[j][:], rhs=Tc, start=True, stop=True)
                        yield
                        S.I("dve", "tensor_tensor", out=T[j][:], in0=pD[:, 3, :], in1=Tc, op=ALU.add)
                        Pc, PTc, Tc = P[j][:], PT[j][:], T[j][:]
                    K.Tfinal[u] = Tc

                def chain(u, c, hh, TR, cur):
                    rows = slice(hh * 64, hh * 64 + 64)
                    hc = slice(hh * 64, hh * 64 + 64)
                    mm, pC, Tc = MM[u], pCh[hh], K.Tfinal[u]
                    Sc, Sn, STb = STh[hh][cur], STh[hh][1 - cur], STbh[hh]
                    S.I("pe", "matmul", out=pC[:, 0, :], lhsT=AR[rows, c, 0, :], rhs=STb[rows, :], start=True, stop=False)
                    S.I("pe", "matmul", out=pC[:, 0, :], lhsT=mm[:, 1, 0:128], rhs=TR[:, 2, hc], start=False, stop=True)
                    yield
                    S.I("act", "copy", out=ZT[hh][:], in_=pC[:, 0, :])
                    yield
                    S.I("pe", "matmul", out=pC[:, 1, :], lhsT=Tc, rhs=ZT[hh][:], start=True, stop=True)
                    yield
                    S.I("act", "copy", out=UT[hh][:], in_=pC[:, 1, :])
                    yield
                    pY = pC[:, 3, :]
                    S.I("pe", "matmul", out=pY, lhsT=AR[rows, c, 1, :], rhs=STb[rows, :], start=True, stop=False)
                    S.I("pe", "matmul", out=pY, lhsT=mm[:, 0, 128:256], rhs=UT[hh][:], start=False, stop=False)
                    S.I("pe", "matmul", out=pY, lhsT=mm[:, 1, 128:256], rhs=TR[:, 2, hc], start=False, stop=True)
                    S.I("pe", "matmul", out=pC[rows, 2, :], lhsT=TR[:, 0, hc], rhs=UT[hh][:], start=True, stop=False)
                    S.I("pe", "matmul", out=pC[rows, 2, :], lhsT=TR[:, 1, hc], rhs=TR[:, 2, hc], start=False, stop=True)
                    yield
                    S.I("dve", "scalar_tensor_tensor", out=Sn[rows, :], in0=Sc[rows, :], scalar=gamC[rows, c:c + 1],
                        in1=pC[rows, 2, :], op0=ALU.mult, op1=ALU.add)
                    S.I("act", "copy", out=STb[rows, :], in_=Sn[rows, :])
                    S.I("act", "copy", out=ysb[:, hh, :], in_=pY)

                def run_rr(gens):
                    gens = list(gens)
                    while gens:
                        for g_ in list(gens):
                            try:
                                next(g_)
                            except StopIteration:
                                gens.remove(g_)

                K.Tfinal = [None] * NU
                for c0_ in range(0, NCS, 2):
                    grp = [c0_ + g_ for g_ in range(2) if c0_ + g_ < NCS]
                    for g_, c in enumerate(grp):
                        cs = slice(c * 128, (c + 1) * 128)
                        TR = TRg[g_]
                        S.I("pe", "transpose", out=pTb[:, g_ * 384 + 0:g_ * 384 + 128], in_=bhb[:, cs], identity=C.identb[:])
                        S.I("pe", "transpose", out=pTb[:, g_ * 384 + 128:g_ * 384 + 256], in_=khb[:, cs], identity=C.identb[:])
                        S.I("pe", "transpose", out=pTb[:, g_ * 384 + 256:g_ * 384 + 384], in_=vbb[:, cs], identity=C.identb[:])
                        S.I("act", "copy", out=TR[:].rearrange("p a b -> p (a b)"), in_=pTb[:, g_ * 384:g_ * 384 + 384])
                    run_rr([stageA(g_ * 2 + hh, c, hh, TRg[g_]) for g_, c in enumerate(grp) for hh in range(2)])
                    for g_, c in enumerate(grp):
                        cs = slice(c * 128, (c + 1) * 128)
                        run_rr([chain(g_ * 2 + hh, c, hh, TRg[g_], cur) for hh in range(2)])
                        cur = 1 - cur
                        y3 = ysb[:]
                        S.I("dve", "tensor_reduce", out=st1[:, 0:2], in_=y3, axis=AX.X, op=ALU.add)
                        S.I("act", "activation", out=ysq[:], in_=y3, func=AF.Square)
                        S.I("dve", "tensor_reduce", out=st1[:, 2:4], in_=ysq[:], axis=AX.X, op=ALU.add)
                        S.I("dve", "tensor_scalar", out=st1[:, 0:2], in0=st1[:, 0:2], scalar1=1.0 / 64, scalar2=None, op0=ALU.mult)
                        S.I("dve", "tensor_tensor", out=st1[:, 4:6], in0=st1[:, 0:2], in1=st1[:, 0:2], op=ALU.mult)
                        S.I("dve", "tensor_scalar", out=st1[:, 2:4], in0=st1[:, 2:4], scalar1=1.0 / 64, scalar2=64e-5,
                            op0=ALU.mult, op1=ALU.add)
                        S.I("dve", "tensor_tensor", out=st1[:, 2:4], in0=st1[:, 2:4], in1=st1[:, 4:6], op=ALU.subtract)
                        S.I("act", "activation", out=st1[:, 2:4], in_=st1[:, 2:4], func=AF.Sqrt)
                        S.I("dve", "reciprocal", out=st1[:, 6:8], in_=st1[:, 2:4])
                        for hh in range(2):
                            S.I("dve", "tensor_scalar", out=ynb[:, hh * 64:(hh + 1) * 64], in0=ysb[:, hh, :],
                                scalar1=st1[:, hh:hh + 1], scalar2=st1[:, 6 + hh:7 + hh], op0=ALU.subtract, op1=ALU.mult)
                        S.I("pe", "transpose", out=pTb[:, 768:896], in_=ynb[:], identity=C.identb[:])
                        S.I("dve", "tensor_scalar", out=of1[:], in0=pTb[:, 768:896], scalar1=col(7), scalar2=col(8),
                            op0=ALU.mult, op1=ALU.add)
                        S.I("dve", "tensor_tensor", out=of1[:], in0=of1[:], in1=bvkeep[:, cs], op=ALU.add)
                        S.I("dve", "tensor_tensor", out=ostg[:, cs], in0=of1[:], in1=gkeep[:, cs], op=ALU.mult)
                piece = min(SEG, TOWN)
                for pc in range(SEG // piece):
                    tt0 = t0 + pc * piece
                    S.dma("sp", mixv[1024 + c0:1024 + c0 + 128, tt0 // TOWN, (tt0 % TOWN):(tt0 % TOWN) + piece],
                          ostg[:, pc * piece:(pc + 1) * piece])


def phase_peer(K, hres):
    nc, S, I, Z, C = K.nc, K.S, K.I, K.Z, K.C
    NEG = -1e30
    with ExitStack() as es:
        sb, ps = (lambda *a: K.sb(es, *a)), (lambda *a: K.ps(es, *a))
        idx_all = sb("p_idx", [128, NTO, 128], I32)
        gate_all = sb("p_gate", [128, NTO, 128], F32)
        ss2 = sb("p_ss2", [128, 8], F32)
        ms2 = sb("p_ms2", [128, 8], F32)
        rstd2 = sb("p_rstd2", [128, 8], F32)
        g2 = sb("p_g2", [128, D], F32); S.dma("sp", g2[:], I.g2)
        junk = sb("p_junk", [128, D], F32)
        with ExitStack() as es1:
            sb1, ps1 = (lambda *a: K.sb(es1, *a)), (lambda *a: K.ps(es1, *a))
            wqb = sb1("p_wq", [128, 16, 2048], BF16)
            wq_v = I.wq.rearrange("(kc p) c -> p kc c", p=128)
            for j in range(4):
                S.dma("pool", wqb[:, :, j * 512:(j + 1) * 512], wq_v[:, :, j * 512:(j + 1) * 512])
            skn = sb1("p_skn", [128, 16, 128], BF16)
            S.dma("pool", skn[:], I.subk.rearrange("h n c -> n h c"))
            skT = sb1("p_skT", [128, 16, 128], BF16)
            iota16 = sb1("p_iota", [128, 16], F32); S.dma("sp", iota16[:], I.iota16)
            xn2b = sb1("p_xn2b", [128, D], BF16)
            xn2T = sb1("p_xn2T", [128, 16, 128], BF16)
            q2T = sb1("p_q2T", [128, 16, 128], BF16)
            s_sb = sb1("p_s", [128, 16, 128], F32)
            s2 = sb1("p_s2", [128, 128], F32)
            sv = sb1("p_sv", [128, 16, 16], F32)
            si = sb1("p_si", [128, 16, 16], U32)
            sif = sb1("p_sif", [128, 16, 16], F32)
            cand = sb1("p_cand", [128, 8, 256], F32)
            c2 = sb1("p_c2", [128, 256], F32)
            best = sb1("p_best", [128, 8, 16], F32)
            bi = sb1("p_bi", [128, 8, 16], U32)
            ai = sb1("p_ai", [128, 8, 16], U32)
            bbi = sb1("p_bbi", [128, 8, 16], U32)
            af = sb1("p_af", [128, 8, 16], F32)
            bf = sb1("p_bf", [128, 8, 16], F32)
            oh = sb1("p_oh", [128, 8, 16, 16], F32)
            i1 = sb1("p_i1", [128, 8, 16], F32)
            i2 = sb1("p_i2", [128, 8, 16], F32)
            ef = sb1("p_ef", [128, 8, 16], F32)
            gs = sb1("p_gs", [128, 8], F32)
            pT = ps1("p_pT", [128, 16, 128], BF16)
            pq = [ps1(f"p_pq{j}", [128, 4, 128], F32) for j in range(2)]
            psc = [ps1(f"p_psc{j}", [128, 4, 128], F32) for j in range(2)]
            for half in range(2):
                for j in range(8):
                    hp = half * 8 + j
                    S.I("pe", "transpose", out=pT[:, j, :], in_=skn[:, hp, :], identity=C.identb[:])
                S.I("act", "copy", out=skT[:, half * 8:(half + 1) * 8, :], in_=pT[:, 0:8, :])
            B4 = [128, 8, 16, 16]
            for tt in range(NTO):
                sumsq(K, junk, hres[:, tt, :], ss2[:, tt:tt + 1])
                rms_rstd(K, ss2[:, tt:tt + 1], ms2[:, tt:tt + 1], rstd2[:, tt:tt + 1])
                S.I("dve", "scalar_tensor_tensor", out=xn2b[:], in0=hres[:, tt, :], scalar=rstd2[:, tt:tt + 1],
                    in1=g2[:], op0=ALU.mult, op1=ALU.mult)
                for kc in range(16):
                    S.I("pe", "transpose", out=pT[:, kc, :], in_=xn2b[:, kc * 128:(kc + 1) * 128], identity=C.identb[:])
                S.I("act", "copy", out=xn2T[:], in_=pT[:])
                for g4 in range(4):
                    p = pq[g4 % 2]
                    for j in range(4):
                        hp = g4 * 4 + j
                        for kc in range(16):
                            S.I("pe", "matmul", out=p[:, j, :], lhsT=wqb[:, kc, hp * 128:(hp + 1) * 128],
                                rhs=xn2T[:, kc, :], start=(kc == 0), stop=(kc == 15))
                    S.I("act", "copy", out=q2T[:, g4 * 4:(g4 + 1) * 4, :], in_=p[:])
                for g4 in range(4):
                    p = psc[g4 % 2]
                    for j in range(4):
                        hp = g4 * 4 + j
                        S.I("pe", "matmul", out=p[:, j, :], lhsT=q2T[:, hp, :], rhs=skT[:, hp, :], start=True, stop=True)
                    S.I("dve", "tensor_copy", out=s_sb[:, g4 * 4:(g4 + 1) * 4, :], in_=p[:])
                for hp in range(16):
                    S.I("dve", "max", out=sv[:, hp, 0:8], in_=s_sb[:, hp, :])
                    S.I("dve", "max_index", out=si[:, hp, 0:8], in_max=sv[:, hp, 0:8], in_values=s_sb[:, hp, :])
                    S.I("dve", "match_replace", out=s2[:], in_to_replace=sv[:, hp, 0:8], in_values=s_sb[:, hp, :],
                        imm_value=NEG)
                    S.I("dve", "max", out=sv[:, hp, 8:16], in_=s2[:])
                    S.I("dve", "max_index", out=si[:, hp, 8:16], in_max=sv[:, hp, 8:16], in_values=s2[:])
                S.I("dve", "tensor_copy", out=sif[:], in_=si[:])
                sv4 = sv[:].rearrange("p (h two) k -> p h two k", two=2)
                sif4 = sif[:].rearrange("p (h two) k -> p h two k", two=2)
                S.I("dve", "tensor_tensor", out=cand[:].rearrange("p h (a b) -> p h a b", a=16),
                    in0=sv4[:, :, 0, :].unsqueeze(3).broadcast_to(B4), in1=sv4[:, :, 1, :].unsqueeze(2).broadcast_to(B4),
                    op=ALU.add)
                for h in range(8):
                    S.I("dve", "max", out=best[:, h, 0:8], in_=cand[:, h, :])
                    S.I("dve", "max_index", out=bi[:, h, 0:8], in_max=best[:, h, 0:8], in_values=cand[:, h, :])
                    S.I("dve", "match_replace", out=c2[:], in_to_replace=best[:, h, 0:8], in_values=cand[:, h, :],
                        imm_value=NEG)
                    S.I("dve", "max", out=best[:, h, 8:16], in_=c2[:])
                    S.I("dve", "max_index", out=bi[:, h, 8:16], in_max=best[:, h, 8:16], in_values=c2[:])
                S.I("dve", "tensor_single_scalar", out=ai[:], in_=bi[:], scalar=4, op=ALU.logical_shift_right)
                S.I("dve", "tensor_single_scalar", out=bbi[:], in_=bi[:], scalar=15, op=ALU.bitwise_and)
                S.I("dve", "tensor_copy", out=af[:], in_=ai[:])
                S.I("dve", "tensor_copy", out=bf[:], in_=bbi[:])
                io4 = iota16[:].unsqueeze(1).unsqueeze(1).broadcast_to(B4)
                for (xf, which, dst) in ((af, 0, i1), (bf, 1, i2)):
                    S.I("dve", "tensor_tensor", out=oh[:], in0=xf[:].unsqueeze(3).broadcast_to(B4), in1=io4, op=ALU.is_equal)
                    S.I("dve", "tensor_tensor", out=oh[:], in0=oh[:], in1=sif4[:, :, which, :].unsqueeze(2).broadcast_to(B4),
                        op=ALU.mult)
                    S.I("dve", "tensor_reduce", out=dst[:], in_=oh[:], axis=AX.X, op=ALU.add)
                S.I("dve", "scalar_tensor_tensor", out=ef[:], in0=i1[:], scalar=128.0, in1=i2[:], op0=ALU.mult, op1=ALU.add)
                S.I("dve", "tensor_copy", out=idx_all[:, tt, :].rearrange("p (h k) -> p h k", h=8), in_=ef[:])
                S.I("dve", "tensor_tensor", out=ef[:], in0=best[:], in1=best[:, :, 0:1].broadcast_to([128, 8, 16]),
                    op=ALU.subtract)
                S.I("act", "activation", out=ef[:], in_=ef[:], func=AF.Exp)
                S.I("dve", "tensor_reduce", out=gs[:], in_=ef[:], axis=AX.X, op=ALU.add)
                S.I("dve", "reciprocal", out=gs[:], in_=gs[:])
                S.I("dve", "tensor_tensor", out=gate_all[:, tt, :].rearrange("p (h k) -> p h k", h=8), in0=ef[:],
                    in1=gs[:].unsqueeze(2).broadcast_to([128, 8, 16]), op=ALU.mult)
        S.barrier()
        if "peer" in DBG:
            for nm, t in (("pidx", idx_all), ("pgate", gate_all)):
                dst = nc.dram_tensor("dbg_" + nm, [128, NTO, 128], t.dtype, kind="ExternalOutput").ap()
                S.dma("sp", dst, t[:])
        with ExitStack() as es1:
            sb1, ps1 = (lambda *a: K.sb(es1, *a)), (lambda *a: K.ps(es1, *a))
            xn2p = [ps1(f"p_xn2p{j}", [128, 512], F32) for j in range(4)]
            accp = [ps1(f"p_accp{j}", [128, 512], F32) for j in range(4)]
            ring = [sb1(f"p_ring{j}", [128, D], BF16) for j in range(8)]
            hpre = sb1("p_hpre", [128, 128], F32)
            hp4 = sb1("p_hp4", [128, 4, 128], F32)
            actv = sb1("p_act", [128, 128], F32)
            nr = 0
            for tt in range(NTO):
                for j in range(4):
                    js = slice(j * 512, (j + 1) * 512)
                    S.I("dve", "scalar_tensor_tensor", out=xn2p[j][:, :], in0=hres[:, tt, js], scalar=rstd2[:, tt:tt + 1],
                        in1=g2[:, js], op0=ALU.mult, op1=ALU.mult)
                for slot in range(128):
                    ug = ring[nr % 8]; nr += 1
                    S.dma("pool", ug[:], Z.ub, method="indirect_dma_start", out_offset=None,
                          in_offset=bass.IndirectOffsetOnAxis(ap=idx_all[:, tt, slot:slot + 1], axis=0), xr=[idx_all[:]])
                    for j in range(4):
                        js = slice(j * 512, (j + 1) * 512)
                        S.I("dve", "scalar_tensor_tensor", out=junk[:, js], in0=ug[:, js], scalar=1.0, in1=xn2p[j][:, :],
                            op0=ALU.mult, op1=ALU.mult, accum_out=hp4[:, j, slot:slot + 1])
                S.I("dve", "tensor_tensor", out=hp4[:, 0:2, :], in0=hp4[:, 0:2, :], in1=hp4[:, 2:4, :], op=ALU.add)
                S.I("dve", "tensor_tensor", out=hpre[:], in0=hp4[:, 0, :], in1=hp4[:, 1, :], op=ALU.add)
                if "peer" in DBG and tt == 0:
                    dst = nc.dram_tensor("dbg_hpre", [128, 128], F32, kind="ExternalOutput").ap()
                    S.dma("sp", dst, hpre[:])
                S.I("act", "activation", out=actv[:], in_=hpre[:], func=AF.Gelu)
                S.I("dve", "tensor_tensor", out=actv[:], in0=actv[:], in1=gate_all[:, tt, :], op=ALU.mult)
                for slot in range(128):
                    vg = ring[nr % 8]; nr += 1
                    S.dma("pool", vg[:], Z.vb, method="indirect_dma_start", out_offset=None,
                          in_offset=bass.IndirectOffsetOnAxis(ap=idx_all[:, tt, slot:slot + 1], axis=0), xr=[idx_all[:]])
                    for j in range(4):
                        js = slice(j * 512, (j + 1) * 512)
                        if slot == 0:
                            S.I("dve", "tensor_scalar", out=accp[j][:, :], in0=vg[:, js], scalar1=actv[:, 0:1], scalar2=None,
                                op0=ALU.mult)
                        else:
                            S.I("dve", "scalar_tensor_tensor", out=accp[j][:, :], in0=vg[:, js], scalar=actv[:, slot:slot + 1],
                                in1=accp[j][:, :], op0=ALU.mult, op1=ALU.add)
                for j in range(4):
                    js = slice(j * 512, (j + 1) * 512)
                    S.I("dve", "tensor_tensor", out=hres[:, tt, js], in0=accp[j][:, :], in1=hres[:, tt, js], op=ALU.add)
        S.barrier()


def phase_tail(K):
    nc, S, I, Z, C = K.nc, K.S, K.I, K.Z, K.C
    with ExitStack() as es:
        sb, ps = (lambda *a: K.sb(es, *a)), (lambda *a: K.ps(es, *a))
        hres = sb("hres", [128, NTO, D], F32)
        S.dma("sp", hres[:], I.xo.rearrange("(tt p) d -> p tt d", p=128))
        with ExitStack() as es1:
            sb1, ps1 = (lambda *a: K.sb(es1, *a)), (lambda *a: K.ps(es1, *a))
            ridx = sb1("ridx", [128, 16], I32)
            S.dma("sp", ridx[:], I.ridx)
            mixT = sb1("mixT", [128, 16, TOWN], BF16)
            for fc in range(16):
                if os.environ.get("MK_NOIND"):
                    S.dma("sp", mixT[:, fc, :], Z.mix[fc * 128:(fc + 1) * 128, :])
                    continue
                S.dma("pool", mixT[:, fc, :], Z.mix, method="indirect_dma_start", out_offset=None,
                      in_offset=bass.IndirectOffsetOnAxis(ap=ridx[:, fc:fc + 1], axis=0), xr=[ridx[:]])
            wb = [sb1(f"wo{j}", [128, 16, 512], BF16) for j in range(2)]
            pp = [ps1(f"po{j}", [128, 512], F32) for j in range(4)]
            wo_v = I.wout.rearrange("(kc p) c -> p kc c", p=128)
            n = 0
            for cg in range(4):
                w = wb[cg % 2]
                S.dma("pool", w[:], wo_v[:, :, cg * 512:(cg + 1) * 512])
                for tt in range(NTO):
                    p = pp[n % 4]; n += 1
                    for kc in range(16):
                        S.I("pe", "matmul", out=p[:, :], lhsT=mixT[:, kc, tt * 128:(tt + 1) * 128],
                            rhs=w[:, kc, :], start=(kc == 0), stop=(kc == 15))
                    S.I("dve", "tensor_tensor", out=hres[:, tt, cg * 512:(cg + 1) * 512], in0=p[:, :],
                        in1=hres[:, tt, cg * 512:(cg + 1) * 512], op=ALU.add)
        S.barrier()
        if UPTO >= 6:
            phase_peer(K, hres)
        if os.environ.get("MK_NOFIN"):
            for tt in range(NTO):
                S.dma("sp", K.out[tt * 128:(tt + 1) * 128, :], hres[:, tt, :])
            return
        with ExitStack() as es1:
            sb1 = (lambda *a: K.sb(es1, *a))
            gf = sb1("gf", [128, D], F32)
            S.dma("sp", gf[:], I.gf)
            junk = sb1("junk2", [128, D], F32)
            ss = sb1("ss2", [128, max(NTO, 8)], F32)
            ms = sb1("ms2", [128, max(NTO, 8)], F32)
            rstd = sb1("rstd2", [128, max(NTO, 8)], F32)
            ot = [sb1(f"ot{j}", [128, D], F32) for j in range(2)]
            for tt in range(NTO):
                sumsq(K, junk, hres[:, tt, :], ss[:, tt:tt + 1])
                rms_rstd(K, ss[:, tt:tt + 1], ms[:, tt:tt + 1], rstd[:, tt:tt + 1])
                S.I("dve", "scalar_tensor_tensor", out=ot[tt % 2][:], in0=hres[:, tt, :],
                    scalar=rstd[:, tt:tt + 1], in1=gf[:], op0=ALU.mult, op1=ALU.mult)
                S.dma("sp", K.out[tt * 128:(tt + 1) * 128, :], ot[tt % 2][:])


def rep(v, n=128):
    v = np.asarray(v, np.float32).reshape(1, -1)
    return np.ascontiguousarray(np.broadcast_to(v, (n, v.shape[1])))


def make_in_maps(inp):
    maps = []
    for c in range(8):
        b, tq = c // 4, c % 4
        m = {}
        m["in_x"] = np.ascontiguousarray(inp["x"][b, :S_])
        m["in_xo"] = np.ascontiguousarray(inp["x"][b, tq * TOWN:(tq + 1) * TOWN])
        m["in_g1"] = rep(inp["norm1_g"][0])
        m["in_win"] = np.ascontiguousarray(inp["w_in"][0])
        m["in_wout"] = np.ascontiguousarray(inp["w_out"][0])
        m["in_g2"] = rep(inp["norm2_g"][0])
        m["in_gf"] = rep(inp["final_g"])
        m["in_ident"] = np.eye(128, dtype=np.float32)
        m.update(attn_consts())
        m.update(rwkv_consts(inp))
        m["in_wq"] = np.ascontiguousarray(inp["peer_wq"][0])
        m["in_subk"] = np.ascontiguousarray(inp["peer_sub_keys"][0].reshape(16, 128, 128))
        m["in_u"] = np.ascontiguousarray(inp["peer_u"][0])
        m["in_v"] = np.ascontiguousarray(inp["peer_v"][0])
        m["in_iota16"] = rep(np.arange(16))
        f = np.arange(128)[:, None] + 128 * np.arange(16)[None, :]
        m["in_ridx"] = (f * 4 + tq).astype(np.int32)
        maps.append(m)
    return maps


_AC = None


def attn_consts():
    global _AC
    if _AC is not None:
        return _AC
    NB = S_ // 256
    n = np.arange(16)[None, :]
    qb = np.arange(16)[:, None]
    past = (n < qb) & (n < NB)
    pb = np.where(past, 0.0, -1e30).astype(np.float32)
    pm = past.astype(np.float32)
    own = (n == qb).astype(np.float32)
    p = np.arange(128)[:, None, None]
    i = np.arange(NT)[None, :, None]
    nn = np.arange(16)[None, None, :]
    Dm = (256 * nn - (128 * i + p)).astype(np.float64)
    dhi = 64.0 * np.floor(Dm / 64.0)
    dlo = Dm - dhi
    esel = np.zeros((32, 16, 128), np.float32)
    for nb in range(16):
        esel[nb, nb, :] = 1.0
        esel[16 + nb, nb, :] = 1.0
    kb = (np.arange(2)[None, :] * 128 + np.arange(128)[:, None]).astype(np.float32)
    k = np.arange(128)[:, None, None]
    r = np.arange(4)[None, :, None]
    q = np.arange(512)[None, None, :]
    cm = np.where((r * 128 + k) <= q, 0.0, -30000.0).astype(np.float32)
    _AC = {
        "in_pb": rep(pb.reshape(-1)), "in_pm": rep(pm.reshape(-1)), "in_own": rep(own.reshape(-1)),
        "in_dhi": np.ascontiguousarray(dhi.reshape(128, -1).astype(np.float32)),
        "in_dlo": np.ascontiguousarray(dlo.reshape(128, -1).astype(np.float32)),
        "in_esel": esel.reshape(32, -1), "in_kb": kb, "in_cm": np.ascontiguousarray(cm.reshape(128, -1)),
        "in_ones": np.ones((128, 128), np.float32),
    }
    return _AC


def rwkv_consts(inp):
    cols = lambda v: np.ascontiguousarray(np.asarray(v, np.float32).reshape(8, 128).T)
    mu = inp["rwkv_mu"][0]
    mul = np.zeros((128, 4), np.float32)
    mul[:96, 0] = mu[3072:3168]
    mul[:96, 1] = mu[3168:3264]
    mul[:, 2] = mu[3264:3392]
    mul[:, 3] = mu[3392:3520]
    prm = np.zeros((128, 10, 8), np.float32)
    prm[:, 0] = cols(mu[0:1024]); prm[:, 1] = cols(mu[1024:2048]); prm[:, 2] = cols(mu[2048:3072])
    prm[:, 4] = cols(inp["rwkv_k_k"][0]); prm[:, 5] = cols(inp["rwkv_k_a"][0]); prm[:, 6] = cols(inp["rwkv_r_k"][0].reshape(-1))
    prm[:, 7] = cols(inp["rwkv_ln_g"][0]); prm[:, 8] = cols(inp["rwkv_ln_b"][0])
    s_ = np.arange(128)[:, None]; t_ = np.arange(128)[None, :]
    strict = (s_ < t_).astype(np.float32); incl = (s_ <= t_).astype(np.float32)
    m2 = np.concatenate([strict, incl, strict, incl], axis=1)
    bones = np.kron(np.eye(2, dtype=np.float32), np.ones((64, 64), np.float32))
    seg = min(2048, S_)
    rst = np.ones((128, seg), np.float32); rst[:, ::128] = 0.0
    return {"in_w2": np.ascontiguousarray(inp["rwkv_w2"][0]), "in_a2": np.ascontiguousarray(inp["rwkv_a2"][0]),
            "in_g2r": np.ascontiguousarray(inp["rwkv_g2"][0]), "in_mul": mul, "in_w0": cols(inp["rwkv_w0"][0]),
            "in_a0": cols(inp["rwkv_a0"][0]), "in_bones": bones, "in_mask2": m2,
            "in_masklt": (s_ > t_).astype(np.float32), "in_rst": rst, "in_prm": prm.reshape(128, 80)}


_NC = None
K_last = {}


def kernel(**inputs):
    global _NC
    inp = {k: np.asarray(v) for k, v in inputs.items()}
    t0 = time.time()
    if _NC is None:
        _NC = build()
    K_last["tb"] = time.time() - t0
    maps = make_in_maps(inp)
    used = set(_NC._mk_declared)
    maps = [{k: v for k, v in m.items() if k in used} for m in maps]
    t1 = time.time()
    res = run_bass_kernel_spmd(_NC, maps, core_ids=list(range(8)))
    K_last["res"] = res
    out = np.zeros((2, S_, D), np.float32)
    print("timing build/run", K_last.get("tb"), time.time() - t1)
    for c in range(8):
        b, tq = c // 4, c % 4
        out[b, tq * TOWN:(tq + 1) * TOWN] = res.results[c]["out"]
    return out
```

```python
import os
import time
from contextlib import ExitStack
import numpy as np
import concourse.bass as bass
import concourse.mybir as mybir
from concourse.bass_utils import run_bass_kernel_spmd

F32 = mybir.dt.float32
BF16 = mybir.dt.bfloat16
I32 = mybir.dt.int32
U32 = mybir.dt.uint32
AF = mybir.ActivationFunctionType
ALU = mybir.AluOpType
AX = mybir.AxisListType

D = 2048
S_ = int(os.environ.get("MK_S", "4096"))
NT = S_ // 128
TOWN = S_ // 4
STG = min(2048, S_)
NTO = TOWN // 128
A_W = 1024
R_W = 1024
IN_COLS = 6592
RW_COLS = 3520
UPTO = int(os.environ.get("MK_UPTO", "99"))
DBG = os.environ.get("MK_DBG", "")
CGS = [int(v) for v in os.environ.get("MK_CGS", "").split(",") if v]


class Sched:
    def __init__(self, nc, n_dma_sems=32):
        self.nc = nc
        self.engs = {"pe": nc.tensor, "act": nc.scalar, "dve": nc.vector,
                     "pool": nc.gpsimd, "sp": nc.sync}
        self.sem = {k: nc.alloc_semaphore(name=f"s_{k}") for k in self.engs}
        self.cnt = {k: 0 for k in self.engs}
        self.seen = {k: {} for k in self.engs}
        self.dma_sems = [nc.alloc_semaphore(name=f"s_dma{i}") for i in range(n_dma_sems)]
        self.dma_cnt = [0] * n_dma_sems
        self.dma_next = 0
        self.res = {}
        self.ninst = 0
        self.nwait = 0

    def _semh(self, sk):
        return self.sem[sk] if isinstance(sk, str) else self.dma_sems[sk[1]]

    def _need(self, e, sk, val):
        if val <= 0 or self.seen[e].get(sk, 0) >= val:
            return
        self.engs[e].wait_ge(self._semh(sk), val)
        self.seen[e][sk] = val
        self.nwait += 1

    @staticmethod
    def _key(x):
        return x if isinstance(x, str) else x.tensor.name

    def _r(self, key):
        return self.res.setdefault(key, {"w": {}, "r": {}})

    def _deps(self, e, reads, writes, me):
        for k in reads:
            for sk, v in self._r(k)["w"].items():
                self._need(e, sk, v)
        for k in writes:
            rr = self._r(k)
            for sk, v in rr["w"].items():
                if sk != me:
                    self._need(e, sk, v)
            for sk, v in rr["r"].items():
                if sk != me:
                    self._need(e, sk, v)

    def _commit(self, reads, writes, sk, val):
        for k in reads:
            self._r(k)["r"][sk] = val
        for k in writes:
            self._r(k)["w"][sk] = val

    def I(self, e, method, *args, w=("out", "accum_out"), xr=(), xw=(), **kw):
        reads, writes = [], []
        for name, v in kw.items():
            if isinstance(v, bass.AP):
                if v.tensor.name.startswith("in_"):
                    continue
                (writes if name in w else reads).append(v.tensor.name)
        for v in args:
            if isinstance(v, bass.AP):
                writes.append(v.tensor.name)
        reads += [self._key(x) for x in xr]
        writes += [self._key(x) for x in xw]
        self._deps(e, reads, writes, e)
        inst = getattr(self.engs[e], method)(*args, **kw)
        self.cnt[e] += 1
        inst.then_inc(self.sem[e], 1)
        self._commit(reads, writes, e, self.cnt[e])
        self.ninst += 1
        return inst

    def dma(self, q, out, in_, method="dma_start", xr=(), xw=(), **kw):
        reads, writes = [], []
        if not in_.tensor.name.startswith("in_"):
            reads.append(in_.tensor.name)
        writes.append(out.tensor.name)
        for name, v in kw.items():
            if isinstance(v, bass.AP):
                reads.append(v.tensor.name)
        reads += [self._key(x) for x in xr]
        writes += [self._key(x) for x in xw]
        i = self.dma_next
        self.dma_next = (self.dma_next + 1) % len(self.dma_sems)
        sk = ("dma", i)
        self._need(q, sk, self.dma_cnt[i])
        self._deps(q, reads, writes, sk)
        inst = getattr(self.engs[q], method)(out=out, in_=in_, **kw)
        self.dma_cnt[i] += 16
        inst.then_inc(self.dma_sems[i], 16)
        self._commit(reads, writes, sk, self.dma_cnt[i])
        self.ninst += 1
        return inst

    def barrier(self):
        for e in self.engs:
            self.wait_all(e)

    def wait_all(self, e):
        for k in self.engs:
            if k != e:
                self._need(e, k, self.cnt[k])
        for i in range(len(self.dma_sems)):
            self._need(e, ("dma", i), self.dma_cnt[i])


class Ctx:
    pass


def build():
    nc = bass.Bass("TRN2", target_bir_lowering=False)
    S = Sched(nc)
    K = Ctx()
    K.nc, K.S = nc, S
    K.uid = 0

    K.declared = []

    def din(name, shape, dt=F32):
        K.declared.append("in_" + name)
        return nc.dram_tensor("in_" + name, list(shape), dt, kind="ExternalInput").ap()

    def dscr(name, shape, dt):
        return nc.dram_tensor(name, list(shape), dt, kind="Internal").ap()

    K.din, K.dscr = din, dscr
    I = Ctx()
    K.I = I
    I.x = din("x", [S_, D])
    I.xo = din("xo", [TOWN, D])
    I.g1 = din("g1", [128, D])
    I.win = din("win", [D, IN_COLS])
    I.wout = din("wout", [D, D])
    I.g2 = din("g2", [128, D])
    I.gf = din("gf", [128, D])
    I.ident = din("ident", [128, 128])
    I.ridx = din("ridx", [128, 16], I32)
    I.pb = din("pb", [128, 16 * 16])
    I.pm = din("pm", [128, 16 * 16])
    I.own = din("own", [128, 16 * 16])
    I.dhi = din("dhi", [128, NT * 16])
    I.dlo = din("dlo", [128, NT * 16])
    I.esel = din("esel", [32, 16 * 128])
    I.kb = din("kb", [128, 2])
    I.cm = din("cm", [128, 4 * 512])
    I.ones = din("ones", [128, 128])
    I.w2 = din("w2", [96, 1024])
    I.a2 = din("a2", [96, 1024])
    I.rg2 = din("g2r", [256, 1024])
    I.mul = din("mul", [128, 4])
    I.w0 = din("w0", [128, 8])
    I.a0 = din("a0", [128, 8])
    I.bones = din("bones", [128, 128])
    I.mask2 = din("mask2", [128, 512])
    I.masklt = din("masklt", [128, 128])
    I.rst = din("rst", [128, min(2048, S_)])
    I.prm = din("prm", [128, 80])
    I.wq = din("wq", [D, 2048])
    I.subk = din("subk", [16, 128, 128])
    I.u = din("u", [16384, D])
    I.v = din("v", [16384, D])
    I.iota16 = din("iota16", [128, 16])
    out = nc.dram_tensor("out", [TOWN, D], F32, kind="ExternalOutput").ap()
    K.out = out
    Z = Ctx()
    K.Z = Z
    Z.qT = dscr("z_qT", [8, 128, S_], BF16)
    Z.kT = dscr("z_kT", [8, 128, S_], BF16)
    Z.v = dscr("z_v", [8, S_, 128], BF16)
    Z.rw = dscr("z_rw", [RW_COLS, S_], F32)
    Z.mix = dscr("z_mix", [D * 4, TOWN], BF16)
    Z.ub = dscr("z_ub", [16384, D], BF16)
    Z.vb = dscr("z_vb", [16384, D], BF16)
    Z.lw = dscr("z_lw", [1024, S_], F32)
    Z.a = dscr("z_a", [1024, S_], F32)
    Z.g = dscr("z_g", [1024, S_], F32)

    es_all = ExitStack()

    def sb(es, name, shape, dt):
        return es.enter_context(nc.sbuf_tensor(name, list(shape), dt))

    def ps(es, name, shape, dt=F32):
        return es.enter_context(nc.psum_tensor(name, list(shape), dt))

    K.sb, K.ps = sb, ps
    C = Ctx()
    K.C = C
    C.identf = sb(es_all, "identf", [128, 128], F32)
    C.identb = sb(es_all, "identb", [128, 128], BF16)
    S.dma("sp", C.identf[:], I.ident)
    S.I("dve", "tensor_copy", out=C.identb[:], in_=C.identf[:])

    if UPTO >= 1:
        phase_norm_proj(K)
        S.barrier()
    if UPTO >= 2:
        phase_zero_mix(K)
        S.barrier()
    if UPTO >= 6:
        for r in range(16):
            rs_ = slice(r * 1024, (r + 1) * 1024)
            S.dma("pool", Z.ub[rs_, :], I.u[rs_, :])
            S.dma("pool", Z.vb[rs_, :], I.v[rs_, :])
    if UPTO >= 3 and not os.environ.get("MK_NOATT"):
        phase_attn(K)
        S.barrier()
    if UPTO >= 4:
        phase_rwkv_lora(K)
        S.barrier()
        phase_rwkv_heads(K)
        S.barrier()
    if UPTO >= 5:
        phase_tail(K)
        S.barrier()
    if DBG:
        dbg_dump(K)
    S.wait_all("sp")
    es_all.close()
    print("ninst", S.ninst, "nwait", S.nwait)
    nc._mk_declared = list(K.declared)
    return nc


def dbg_dump(K):
    nc, S, Z = K.nc, K.S, K.Z
    items = {"qT0": Z.qT[0], "kT7": Z.kT[7], "v3": Z.v[3], "rw0": Z.rw[0:128, :], "rwl": Z.rw[RW_COLS - 64:RW_COLS, :],
             "mix": Z.mix, "mixa": Z.mix[0:4 * 256, :], "mixr": Z.mix[4 * 1024:4 * 1024 + 4 * 256, :],
             "lw": Z.lw[0:128, :], "za": Z.a[0:128, :], "zg": Z.g[0:128, :]}
    for name in DBG.split(","):
        if name not in items:
            continue
        src = items[name]
        dst = nc.dram_tensor("dbg_" + name, list(src.shape), src.dtype, kind="ExternalOutput").ap()
        S.dma("sp", dst, src)


def sumsq(K, junk, src, ss_col):
    S = K.S
    S.I("act", "activation", out=junk[:], in_=src, func=AF.Square)
    S.I("dve", "tensor_reduce", out=ss_col, in_=junk[:], axis=AX.X, op=ALU.add)


def rms_rstd(K, ss_col, tmp_col, out_col):
    S = K.S
    S.I("dve", "tensor_scalar", out=tmp_col, in0=ss_col, scalar1=1.0 / D, scalar2=1e-6,
        op0=ALU.mult, op1=ALU.add)
    S.I("act", "activation", out=tmp_col, in_=tmp_col, func=AF.Sqrt)
    S.I("dve", "reciprocal", out=out_col, in_=tmp_col)


def phase_norm_proj(K):
    nc, S, I, Z, C = K.nc, K.S, K.I, K.Z, K.C
    with ExitStack() as es:
        sb, ps = (lambda *a: K.sb(es, *a)), (lambda *a: K.ps(es, *a))
        xnT = sb("xnT", [128, 16, S_], BF16)
        ss = sb("ss", [128, NT], F32)
        ms = sb("ms", [128, NT], F32)
        rstd = sb("rstd", [128, NT], F32)
        xn = [sb(f"xn{j}", [128, D], BF16) for j in range(2)]
        with ExitStack() as es1:
            sb1, ps1 = (lambda *a: K.sb(es1, *a)), (lambda *a: K.ps(es1, *a))
            g1 = sb1("g1", [128, D], F32)
            S.dma("sp", g1[:], I.g1)
            xb = [sb1(f"xb{j}", [128, D], F32) for j in range(2)]
            junk = sb1("junk", [128, D], F32)
            pT = [ps1(f"pT{j}", [128, 16, 128], BF16) for j in range(2)]
            for i in range(NT):
                j = i % 2
                S.dma("sp", xb[j][:], I.x[i * 128:(i + 1) * 128, :])
                sumsq(K, junk, xb[j][:], ss[:, i:i + 1])
                rms_rstd(K, ss[:, i:i + 1], ms[:, i:i + 1], rstd[:, i:i + 1])
                S.I("dve", "scalar_tensor_tensor", out=xn[j][:], in0=xb[j][:], scalar=rstd[:, i:i + 1],
                    in1=g1[:], op0=ALU.mult, op1=ALU.mult)
                for kc in range(16):
                    S.I("pe", "transpose", out=pT[j][:, kc, :], in_=xn[j][:, kc * 128:(kc + 1) * 128],
                        identity=C.identb[:])
                S.I("act" if i % 2 == 0 else "dve", "copy" if i % 2 == 0 else "tensor_copy",
                    out=xnT[:, :, i * 128:(i + 1) * 128], in_=pT[j][:])
        S.barrier()
        if "stat" in DBG:
            for nm, t in (("ss", ss), ("ms", ms), ("rstd", rstd)):
                dst = nc.dram_tensor("dbg_" + nm, [128, NT], F32, kind="ExternalOutput").ap()
                S.dma("sp", dst, t[:])
            dst = nc.dram_tensor("dbg_xnl", [128, D], BF16, kind="ExternalOutput").ap()
            S.dma("sp", dst, xn[(NT - 1) % 2][:])
        if "xnT" in DBG:
            dst = nc.dram_tensor("dbg_xnT", [128, 16, S_], BF16, kind="ExternalOutput").ap()
            S.dma("sp", dst, xnT[:])
        wb = [sb(f"wb{j}", [128, 16, 512], BF16) for j in range(2)]
        stg_b = [sb(f"stgb{j}", [128, STG], BF16) for j in range(2)]
        stg_f = [sb(f"stgf{j}", [128, STG], F32) for j in range(2)]
        pp = [ps(f"pp{j}", [128, 512], F32) for j in range(4)]
        K.pcnt = 0
        K.ecnt = 0
        K.scnt = 0
        win_v = I.win.rearrange("(kc p) c -> p kc c", p=128)
        ngroups = (IN_COLS + 511) // 512
        for cg in range(ngroups):
            if CGS and cg not in CGS:
                continue
            c0 = cg * 512
            ncol = min(512, IN_COLS - c0)
            w = wb[cg % 2]
            S.dma("pool", w[:, :, 0:ncol], win_v[:, :, c0:c0 + ncol])
            is_v = 2048 <= c0 < 3072
            if is_v:
                h0 = (c0 - 2048) // 128
                for tt in range(NT):
                    p = pp[K.pcnt % 4]; K.pcnt += 1
                    for kc in range(16):
                        S.I("pe", "matmul", out=p[:, :], lhsT=xnT[:, kc, tt * 128:(tt + 1) * 128],
                            rhs=w[:, kc, :], start=(kc == 0), stop=(kc == 15))
                    st = stg_b[K.scnt % 2]; K.scnt += 1
                    evac(K, st[:, 0:512], p[:, :])
                    S.dma("sp", Z.v[h0:h0 + 4, tt * 128:(tt + 1) * 128, :].rearrange("h t d -> t h d"),
                          st[:, 0:512].rearrange("t (h d) -> t h d", h=4))
                continue
            for cc in range((ncol + 127) // 128):
                m = min(128, ncol - cc * 128)
                col = c0 + cc * 128
                f32_out = col >= 3072
                for half in range(S_ // STG):
                    st = (stg_f if f32_out else stg_b)[K.scnt % 2]; K.scnt += 1
                    for tg in range(STG // 512):
                        p = pp[K.pcnt % 4]; K.pcnt += 1
                        t0 = half * STG + tg * 512
                        for kc in range(16):
                            S.I("pe", "matmul", out=p[0:m, :], lhsT=w[:, kc, cc * 128:cc * 128 + m],
                                rhs=xnT[:, kc, t0:t0 + 512], start=(kc == 0), stop=(kc == 15))
                        evac(K, st[0:m, tg * 512:(tg + 1) * 512], p[0:m, :],
                             scale=(128 ** -0.5 if col < 1024 else None))
                    hs = slice(half * STG, (half + 1) * STG)
                    if col < 1024:
                        S.dma("sp", Z.qT[col // 128, :, hs], st[:, :])
                    elif col < 2048:
                        S.dma("sp", Z.kT[(col - 1024) // 128, :, hs], st[:, :])
                    else:
                        r0 = col - 3072
                        S.dma("sp", Z.rw[r0:r0 + m, hs], st[0:m, :])


def evac(K, dst, src, scale=None):
    S = K.S
    K.ecnt += 1
    if scale is not None:
        if K.ecnt % 2 == 0:
            S.I("act", "activation", out=dst, in_=src, func=AF.Copy, scale=float(scale))
        else:
            S.I("dve", "tensor_scalar", out=dst, in0=src, scalar1=float(scale), scalar2=None, op0=ALU.mult)
    else:
        if K.ecnt % 2 == 0:
            S.I("act", "copy", out=dst, in_=src)
        else:
            S.I("dve", "tensor_copy", out=dst, in_=src)


def phase_zero_mix(K):
    nc, S, Z = K.nc, K.S, K.Z
    with ExitStack() as es:
        zt = K.sb(es, "zt", [128, TOWN], BF16)
        S.I("dve", "memset", zt[:], 0.0)
        mv = Z.mix.rearrange("(a p) t -> a p t", p=128)
        for a in range(D * 4 // 128):
            S.dma("sp", mv[a], zt[:])
        S.barrier()


def phase_attn(K):
    nc, S, I, Z, C = K.nc, K.S, K.I, K.Z, K.C
    NB = S_ // 256
    NQ = S_ // 512
    with ExitStack() as es:
        sb, ps = (lambda *a: K.sb(es, *a)), (lambda *a: K.ps(es, *a))
        pb = sb("a_pb", [128, 16, 16], F32); S.dma("sp", pb[:], I.pb.rearrange("p (a b) -> p a b", a=16))
        pm = sb("a_pm", [128, 16, 16], F32); S.dma("sp", pm[:], I.pm.rearrange("p (a b) -> p a b", a=16))
        own = sb("a_own", [128, 16, 16], F32); S.dma("sp", own[:], I.own.rearrange("p (a b) -> p a b", a=16))
        dhi = sb("a_dhi", [128, NT, 16], F32); S.dma("sp", dhi[:], I.dhi.rearrange("p (a b) -> p a b", b=16))
        dlo = sb("a_dlo", [128, NT, 16], F32); S.dma("sp", dlo[:], I.dlo.rearrange("p (a b) -> p a b", b=16))
        esel = sb("a_esel", [32, 16, 128], BF16); S.dma("pool", esel[:], I.esel.rearrange("p (a b) -> p a b", a=16))
        kb = sb("a_kb", [128, 2], F32); S.dma("sp", kb[:], I.kb)
        cm = sb("a_cm", [128, 4, 512], BF16); S.dma("pool", cm[:], I.cm.rearrange("p (a b) -> p a b", a=4))
        ones = sb("a_ones", [128, 128], BF16); S.dma("pool", ones[:], I.ones)
        qT = sb("a_qT", [128, S_], BF16)
        kT = sb("a_kT", [128, S_], BF16)
        v = sb("a_v", [128, NT, 128], BF16)
        mbt = sb("a_mbt", [32, S_], BF16)
        km32 = sb("a_km32", [128, 16], F32)
        kmb = sb("a_kmb", [128, 16], BF16)
        gm = sb("a_gm", [128, 16], F32)
        m8 = sb("a_m8", [128, 8], F32)
        sel = sb("a_sel", [128, 16], F32)
        mbv = sb("a_mbv", [128, 16], F32)
        mb = sb("a_mb", [128, 32], F32)
        kbh = sb("a_kbh", [128, 2], F32)
        PT = [sb(f"a_PT{j}", [128, 512], BF16) for j in range(2)]
        rD = sb("a_rD", [128, 512], F32)
        Ost = [sb(f"a_O{j}", [128, 512], BF16) for j in range(2)]
        pg = ps("a_pg", [128, 16], F32)
        pt = ps("a_pt", [32, 128], F32)
        pS = [ps(f"a_pS{j}", [128, 512], F32) for j in range(2)]
        pO = ps("a_pO", [128, 512], F32)
        pD = ps("a_pD", [128, 512], F32)
        mixv = Z.mix.rearrange("(f q) t -> f q t", q=4)
        S.I("dve", "memset", km32[:], 0.0)
        for h in range(8):
            slope = 2.0 ** (-(h + 1))
            S.dma("sp", qT[:], Z.qT[h])
            S.dma("sp", kT[:], Z.kT[h])
            S.dma("sp", v[:], Z.v[h].rearrange("(j p) d -> p j d", p=128))
            S.I("dve", "tensor_reduce", out=km32[:, 0:NB], in_=kT[:].rearrange("p (n l) -> p n l", l=256),
                axis=AX.X, op=ALU.add)
            S.I("dve", "tensor_scalar", out=kmb[:], in0=km32[:], scalar1=1.0 / 256, scalar2=None, op0=ALU.mult)
            S.I("dve", "tensor_scalar", out=kbh[:], in0=kb[:], scalar1=slope, scalar2=None, op0=ALU.mult)
            for i in range(NT):
                qb = i // 2
                S.I("pe", "matmul", out=pg[:, :], lhsT=qT[:, i * 128:(i + 1) * 128], rhs=kmb[:, :],
                    start=True, stop=True)
                S.I("dve", "tensor_tensor", out=gm[:], in0=pg[:, :], in1=pb[:, qb, :], op=ALU.add)
                S.I("dve", "max", out=m8[:], in_=gm[:])
                S.I("dve", "scalar_tensor_tensor", out=sel[:], in0=gm[:], scalar=m8[:, 2:3], in1=pm[:, qb, :],
                    op0=ALU.is_ge, op1=ALU.mult)
                S.I("dve", "tensor_tensor", out=sel[:], in0=sel[:], in1=own[:, qb, :], op=ALU.add)
                S.I("dve", "tensor_scalar", out=mbv[:], in0=sel[:], scalar1=-1.0, scalar2=30000.0,
                    op0=ALU.add, op1=ALU.mult)
                S.I("dve", "scalar_tensor_tensor", out=mb[:, 0:16], in0=dhi[:, i, :], scalar=slope, in1=mbv[:],
                    op0=ALU.mult, op1=ALU.add)
                S.I("dve", "tensor_scalar", out=mb[:, 16:32], in0=dlo[:, i, :], scalar1=slope, scalar2=None,
                    op0=ALU.mult)
                S.I("pe", "transpose", out=pt[:, :], in_=mb[:, :], identity=C.identf[:])
                S.I("act", "copy", out=mbt[:, i * 128:(i + 1) * 128], in_=pt[:, :])
            for Q in range(NQ):
                nkt = 4 * (Q + 1)
                qs = slice(Q * 512, (Q + 1) * 512)

                def qk(kt):
                    p = pS[kt % 2]
                    S.I("pe", "matmul", out=p[:, :], lhsT=kT[:, kt * 128:(kt + 1) * 128], rhs=qT[:, qs],
                        start=True, stop=False)
                    diag = kt >= 4 * Q
                    S.I("pe", "matmul", out=p[:, :], lhsT=esel[:, kt // 2, :], rhs=mbt[:, qs],
                        start=False, stop=not diag)
                    if diag:
                        S.I("pe", "matmul", out=p[:, :], lhsT=C.identb[:], rhs=cm[:, kt - 4 * Q, :],
                            start=False, stop=True)

                qk(0)
                for kt in range(nkt):
                    if kt + 1 < nkt:
                        qk(kt + 1)
                    P = PT[kt % 2]
                    S.I("act", "activation", out=P[:], in_=pS[kt % 2][:, :], func=AF.Exp,
                        bias=kbh[:, (kt % 2):(kt % 2) + 1], scale=1.0)
                    S.I("pe", "matmul", out=pO[:, :], lhsT=v[:, kt, :], rhs=P[:], start=(kt == 0),
                        stop=(kt == nkt - 1))
                    S.I("pe", "matmul", out=pD[:, :], lhsT=ones[:], rhs=P[:], start=(kt == 0),
                        stop=(kt == nkt - 1))
                S.I("dve", "reciprocal", out=rD[:], in_=pD[:, :])
                O = Ost[Q % 2]
                S.I("dve", "tensor_tensor", out=O[:], in0=pO[:, :], in1=rD[:], op=ALU.mult)
                piece = min(512, TOWN)
                for pc in range(512 // piece):
                    t0 = Q * 512 + pc * piece
                    S.dma("sp", mixv[h * 128:(h + 1) * 128, t0 // TOWN, (t0 % TOWN):(t0 % TOWN) + piece],
                          O[:, pc * piece:(pc + 1) * piece])


SEG = min(2048, S_)
NCS = SEG // 128


def shifted_load(K, raw, halo, mu_col, rows, r0, t0, out, tmp, act=None):
    S, Z = K.S, K.Z
    if t0 == 0:
        S.I("dve", "memset", halo[0:rows, 0:1], 0.0)
        S.dma("sp", halo[0:rows, 1:SEG + 1], Z.rw[r0:r0 + rows, 0:SEG])
    else:
        S.dma("sp", halo[0:rows, 0:SEG + 1], Z.rw[r0:r0 + rows, t0 - 1:t0 + SEG])
    S.I("dve", "tensor_tensor", out=tmp[0:rows, :], in0=halo[0:rows, 0:SEG], in1=halo[0:rows, 1:SEG + 1],
        op=ALU.subtract)
    S.I("dve", "scalar_tensor_tensor", out=out, in0=tmp[0:rows, :], scalar=mu_col, in1=halo[0:rows, 1:SEG + 1],
        op0=ALU.mult, op1=ALU.add)


def phase_rwkv_lora(K):
    nc, S, I, Z, C = K.nc, K.S, K.I, K.Z, K.C
    with ExitStack() as es:
        sb, ps = (lambda *a: K.sb(es, *a)), (lambda *a: K.ps(es, *a))
        w2b = sb("l_w2", [96, 1024], BF16); S.dma("pool", w2b[:], I.w2)
        a2b = sb("l_a2", [96, 1024], BF16); S.dma("pool", a2b[:], I.a2)
        g2b = sb("l_g2", [128, 2, 1024], BF16); S.dma("pool", g2b[:], I.rg2.rearrange("(k p) c -> p k c", p=128))
        mul = sb("l_mu", [128, 4], F32); S.dma("sp", mul[:], I.mul)
        w0 = sb("l_w0", [128, 8], F32); S.dma("sp", w0[:], I.w0)
        a0 = sb("l_a0", [128, 8], F32); S.dma("sp", a0[:], I.a0)
        halo = sb("l_halo", [128, SEG + 1], F32)
        tmp = sb("l_tmp", [128, SEG], F32)
        xs = sb("l_xs", [128, SEG], F32)
        txw = sb("l_txw", [96, SEG], BF16)
        xab = sb("l_xab", [96, SEG], BF16)
        sxg = sb("l_sxg", [128, 2, SEG], BF16)
        stg = [sb(f"l_stg{j}", [128, SEG], F32) for j in range(2)]
        pp = [ps(f"l_pp{j}", [128, 512], F32) for j in range(4)]
        n = 0
        ns = 0
        for seg in range(S_ // SEG):
            t0 = seg * SEG
            shifted_load(K, Z.rw, halo, mul[0:96, 0:1], 96, 3072, t0, xs[0:96, :], tmp)
            S.I("act", "activation", out=txw[:], in_=xs[0:96, :], func=AF.Tanh)
            shifted_load(K, Z.rw, halo, mul[0:96, 1:2], 96, 3168, t0, xs[0:96, :], tmp)
            S.I("act", "copy", out=xab[:], in_=xs[0:96, :])
            for kc in range(2):
                shifted_load(K, Z.rw, halo, mul[:, 2 + kc:3 + kc], 128, 3264 + kc * 128, t0, xs[:, :], tmp)
                S.I("act", "activation", out=sxg[:, kc, :], in_=xs[:, :], func=AF.Sigmoid)
            for cc in range(8):
                cs = slice(cc * 128, (cc + 1) * 128)
                for which in range(3):
                    st = stg[ns % 2]; ns += 1
                    for tg in range(SEG // 512):
                        ts_ = slice(tg * 512, (tg + 1) * 512)
                        p = pp[n % 4]; n += 1
                        if which == 0:
                            S.I("pe", "matmul", out=p[:, :], lhsT=w2b[:, cs], rhs=txw[:, ts_], start=True, stop=True)
                            S.I("act", "activation", out=st[:, ts_], in_=p[:, :], func=AF.Sigmoid,
                                bias=w0[:, cc:cc + 1], scale=1.0)
                        elif which == 1:
                            S.I("pe", "matmul", out=p[:, :], lhsT=a2b[:, cs], rhs=xab[:, ts_], start=True, stop=True)
                            S.I("act", "activation", out=st[:, ts_], in_=p[:, :], func=AF.Sigmoid,
                                bias=a0[:, cc:cc + 1], scale=1.0)
                        else:
                            for kc in range(2):
                                S.I("pe", "matmul", out=p[:, :], lhsT=g2b[:, kc, cs], rhs=sxg[:, kc, ts_],
                                    start=(kc == 0), stop=(kc == 1))
                            S.I("dve", "tensor_copy", out=st[:, ts_], in_=p[:, :])
                    if which == 0:
                        S.I("dve", "tensor_scalar", out=st[:, :], in0=st[:, :], scalar1=-0.6065306597, scalar2=None,
                            op0=ALU.mult)
                    dst = (Z.lw, Z.a, Z.g)[which]
                    S.dma("sp", dst[cs, t0:t0 + SEG], st[:, :])


def phase_rwkv_heads(K):
    nc, S, I, Z, C = K.nc, K.S, K.I, K.Z, K.C
    NPAIR = int(os.environ.get("MK_NPAIR", "8"))
    with ExitStack() as es:
        sb, ps = (lambda *a: K.sb(es, *a)), (lambda *a: K.ps(es, *a))
        bones = sb("r_bones", [128, 128], BF16); S.dma("pool", bones[:], I.bones)
        mask2 = sb("r_mask2", [128, 2, 256], F32); S.dma("sp", mask2[:], I.mask2.rearrange("p (a b) -> p a b", a=2))
        masklt = sb("r_masklt", [128, 128], F32); S.dma("sp", masklt[:], I.masklt)
        rst = sb("r_rst", [128, SEG], F32); S.dma("sp", rst[:], I.rst)
        prm = sb("r_prm", [128, 10, 8], F32); S.dma("sp", prm[:], I.prm.rearrange("p (a b) -> p a b", a=10))
        omka = sb("r_omka", [128, 8], F32)
        S.I("dve", "tensor_scalar", out=omka[:], in0=prm[:, 5, :], scalar1=-1.0, scalar2=1.0, op0=ALU.mult, op1=ALU.add)
        halo = sb("r_halo", [128, SEG + 1], F32)
        F = [sb(f"r_F{j}", [128, SEG], F32) for j in range(9)]
        gkeep = sb("r_g", [128, SEG], F32)
        bvkeep = sb("r_bv", [128, SEG], F32)
        AR = sb("r_AR", [128, NCS, 2, 128], BF16)
        btb = sb("r_bt", [128, SEG], BF16)
        ktb = sb("r_kt", [128, SEG], BF16)
        bhb = sb("r_bh", [128, SEG], BF16)
        khb = sb("r_kh", [128, SEG], BF16)
        vbb = sb("r_vb", [128, SEG], BF16)
        sqb = sb("r_sqb", [128, SEG], BF16)
        gamC = sb("r_gamC", [128, NCS], F32)
        ostg = sb("r_ostg", [128, SEG], BF16)
        STh = [[sb(f"r_ST{h}{j}", [128, 64], F32) for j in range(2)] for h in range(2)]
        STbh = [sb(f"r_STb{h}", [128, 64], BF16) for h in range(2)]
        NU = 8
        TRg = [sb(f"r_TR{g}", [128, 3, 128], BF16) for g in range(4)]
        MM = [sb(f"r_MM{u}", [128, 2, 256], BF16) for u in range(NU)]
        Pm = [[sb(f"r_P{u}{j}", [128, 128], BF16) for j in range(2)] for u in range(NU)]
        PTm = [[sb(f"r_PT{u}{j}", [128, 128], BF16) for j in range(2)] for u in range(NU)]
        Tm = [[sb(f"r_T{u}{j}", [128, 128], BF16) for j in range(2)] for u in range(NU)]
        ZT = [sb(f"r_ZT{h}", [128, 64], BF16) for h in range(2)]
        UT = [sb(f"r_UT{h}", [128, 64], BF16) for h in range(2)]
        ysb = sb("r_ysb", [128, 2, 64], F32)
        ysq = sb("r_ysq", [128, 2, 64], F32)
        st1 = sb("r_st1", [128, 8], F32)
        ynb = sb("r_ynb", [128, 128], BF16)
        of1 = sb("r_of1", [128, 128], F32)
        pTb = ps("r_pTb", [128, 1024], BF16)
        pM = ps("r_pM", [128, 2, 256], F32)
        pDb = [ps(f"r_pD{u}", [128, 4, 128], F32) for u in range(4)]
        pCh = [ps(f"r_pC{h}", [128, 8, 64], F32) for h in range(2)]
        pX = pM[:].rearrange("p a b -> p (a b)")
        mixv = Z.mix.rearrange("(f q) t -> f q t", q=4)

        for hp in range(NPAIR):
            cc = hp
            c0 = hp * 128
            col = lambda a: prm[:, a, cc:cc + 1]
            for hh in range(2):
                S.I("dve", "memset", STh[hh][0][:], 0.0)
                S.I("dve", "memset", STbh[hh][:], 0.0)
            cur = 0
            for seg in range(S_ // SEG):
                t0 = seg * SEG
                rs, ks, vs, lw, aa, t1, t2, t3, t4 = F
                shifted_load(K, Z.rw, halo, col(0), 128, c0, t0, rs[:, :], t1)
                shifted_load(K, Z.rw, halo, col(1), 128, 1024 + c0, t0, ks[:, :], t1)
                shifted_load(K, Z.rw, halo, col(2), 128, 2048 + c0, t0, vs[:, :], t1)
                S.dma("sp", lw[:], Z.lw[c0:c0 + 128, t0:t0 + SEG])
                S.dma("sp", aa[:], Z.a[c0:c0 + 128, t0:t0 + SEG])
                S.dma("sp", gkeep[:], Z.g[c0:c0 + 128, t0:t0 + SEG])
                S.I("dve", "tensor_scalar", out=t1[:], in0=ks[:], scalar1=col(4), scalar2=None, op0=ALU.mult)
                S.I("act", "activation", out=sqb[:], in_=t1[:], func=AF.Square)
                for tg in range(SEG // 512):
                    ts_ = slice(tg * 512, (tg + 1) * 512)
                    S.I("pe", "matmul", out=pX, lhsT=bones[:], rhs=sqb[:, ts_], start=True, stop=True)
                    S.I("dve", "tensor_scalar", out=t2[:, ts_], in0=pX, scalar1=1e-24, scalar2=None, op0=ALU.add)
                S.I("act", "activation", out=t2[:], in_=t2[:], func=AF.Sqrt)
                S.I("dve", "reciprocal", out=t2[:], in_=t2[:])
                S.I("dve", "tensor_tensor", out=t1[:], in0=t1[:], in1=t2[:], op=ALU.mult)
                S.I("dve", "tensor_scalar", out=t2[:], in0=aa[:], scalar1=col(5), scalar2=omka[:, cc:cc + 1],
                    op0=ALU.mult, op1=ALU.add)
                S.I("dve", "tensor_tensor", out=ks[:], in0=ks[:], in1=t2[:], op=ALU.mult)
                S.I("dve", "tensor_tensor", out=aa[:], in0=t1[:], in1=aa[:], op=ALU.mult)
                S.I("dve", "tensor_tensor", out=t2[:], in0=rs[:], in1=ks[:], op=ALU.mult)
                S.I("dve", "tensor_scalar", out=sqb[:], in0=t2[:], scalar1=col(6), scalar2=None, op0=ALU.mult)
                for tg in range(SEG // 512):
                    ts_ = slice(tg * 512, (tg + 1) * 512)
                    S.I("pe", "matmul", out=pX, lhsT=bones[:], rhs=sqb[:, ts_], start=True, stop=True)
                    S.I("dve", "tensor_tensor", out=bvkeep[:, ts_], in0=pX, in1=vs[:, ts_], op=ALU.mult)
                S.I("act", "copy", out=vbb[:], in_=vs[:])
                S.I("dve", "tensor_tensor_scan", out=t2[:], data0=rst[:], data1=lw[:], initial=0.0,
                    op0=ALU.mult, op1=ALU.add)
                S.I("act", "activation", out=t3[:], in_=t2[:], func=AF.Exp)
                S.I("dve", "tensor_tensor", out=AR[:, :, 1, :], in0=rs[:].rearrange("p (c t) -> p c t", t=128),
                    in1=t3[:].rearrange("p (c t) -> p c t", t=128), op=ALU.mult)
                S.I("dve", "tensor_tensor", out=t3[:], in0=t2[:], in1=lw[:], op=ALU.subtract)
                S.I("act", "activation", out=t3[:], in_=t3[:], func=AF.Exp)
                S.I("dve", "scalar_tensor_tensor", out=AR[:, :, 0, :], in0=t1[:].rearrange("p (c t) -> p c t", t=128),
                    scalar=-1.0, in1=t3[:].rearrange("p (c t) -> p c t", t=128), op0=ALU.mult, op1=ALU.mult)
                S.I("act", "activation", out=t3[:], in_=t2[:], func=AF.Exp, scale=-1.0)
                S.I("dve", "tensor_tensor", out=btb[:], in0=aa[:], in1=t3[:], op=ALU.mult)
                S.I("dve", "tensor_tensor", out=ktb[:], in0=ks[:], in1=t3[:], op=ALU.mult)
                for c in range(NCS):
                    cs = slice(c * 128, (c + 1) * 128)
                    S.I("dve", "tensor_scalar", out=t4[:, cs], in0=t2[:, cs], scalar1=-1.0,
                        scalar2=t2[:, c * 128 + 127:c * 128 + 128], op0=ALU.mult, op1=ALU.add)
                S.I("act", "activation", out=t4[:], in_=t4[:], func=AF.Exp)
                S.I("dve", "tensor_tensor", out=bhb[:], in0=aa[:], in1=t4[:], op=ALU.mult)
                S.I("dve", "tensor_tensor", out=khb[:], in0=ks[:], in1=t4[:], op=ALU.mult)
                S.I("act", "activation", out=gamC[:], in_=t2[:].rearrange("p (c t) -> p c t", t=128)[:, :, 127],
                    func=AF.Exp)
                def stageA(u, c, hh, TR):
                    cs = slice(c * 128, (c + 1) * 128)
                    rows = slice(hh * 64, hh * 64 + 64)
                    mm, pD = MM[u], pDb[u % 4]
                    P, PT, T = Pm[u], PTm[u], Tm[u]
                    S.I("pe", "matmul", out=pM[:, 0, :], lhsT=btb[rows, cs], rhs=AR[rows, c, :, :], start=True, stop=True)
                    S.I("pe", "matmul", out=pM[:, 1, :], lhsT=ktb[rows, cs], rhs=AR[rows, c, :, :], start=True, stop=True)
                    S.I("pe", "matmul", out=pD[:, 0, :], lhsT=AR[rows, c, 0, :], rhs=btb[rows, cs], start=True, stop=True)
                    S.I("dve", "tensor_tensor", out=mm[:], in0=pM[:], in1=mask2[:], op=ALU.mult)
                    yield
                    S.I("dve", "tensor_tensor", out=PT[0][:], in0=pD[:, 0, :], in1=masklt[:], op=ALU.mult)
                    S.I("dve", "tensor_tensor", out=T[0][:], in0=mm[:, 0, 0:128], in1=C.identb[:], op=ALU.add)
                    yield
                    Pc, PTc, Tc = mm[:, 0, 0:128], PT[0][:], T[0][:]
                    for rd in range(6):
                        j = (rd + 1) % 2
                        S.I("pe", "matmul", out=pD[:, 1, :], lhsT=Pc, rhs=PTc, start=True, stop=True)
                        if rd < 5:
                            S.I("pe", "matmul", out=pD[:, 2, :], lhsT=PTc, rhs=Pc, start=True, stop=True)
                        yield
                        S.I("act", "copy", out=PT[j][:], in_=pD[:, 1, :])
                        if rd < 5:
                            S.I("act", "copy", out=P[j][:], in_=pD[:, 2, :])
                        yield
                        S.I("pe", "matmul", out=pD[:, 3, :], lhsT=PT[j][:], rhs=Tc, start=True, stop=True)
                        yield
                        S.I("dve", "tensor_tensor", out=T[j][:], in0=pD[:, 3, :], in1=Tc, op=ALU.add)
                        Pc, PTc, Tc = P[j][:], PT[j][:], T[j][:]
                    K.Tfinal[u] = Tc

                def chain(u, c, hh, TR):
                    cur = c % 2
                    rows = slice(hh * 64, hh * 64 + 64)
                    hc = slice(hh * 64, hh * 64 + 64)
                    mm, pC, Tc = MM[u], pCh[hh], K.Tfinal[u]
                    Sc, Sn, STb = STh[hh][cur], STh[hh][1 - cur], STbh[hh]
                    S.I("pe", "matmul", out=pC[:, 0, :], lhsT=AR[rows, c, 0, :], rhs=STb[rows, :], start=True, stop=False)
                    S.I("pe", "matmul", out=pC[:, 0, :], lhsT=mm[:, 1, 0:128], rhs=TR[:, 2, hc], start=False, stop=True)
                    yield
                    S.I("act", "copy", out=ZT[hh][:], in_=pC[:, 0, :])
                    yield
                    S.I("pe", "matmul", out=pC[:, 1, :], lhsT=Tc, rhs=ZT[hh][:], start=True, stop=True)
                    yield
                    S.I("act", "copy", out=UT[hh][:], in_=pC[:, 1, :])
                    yield
                    pY = pC[:, 3, :]
                    S.I("pe", "matmul", out=pY, lhsT=AR[rows, c, 1, :], rhs=STb[rows, :], start=True, stop=False)
                    S.I("pe", "matmul", out=pY, lhsT=mm[:, 0, 128:256], rhs=UT[hh][:], start=False, stop=False)
                    S.I("pe", "matmul", out=pY, lhsT=mm[:, 1, 128:256], rhs=TR[:, 2, hc], start=False, stop=True)
                    S.I("pe", "matmul", out=pC[rows, 2, :], lhsT=TR[:, 0, hc], rhs=UT[hh][:], start=True, stop=False)
                    S.I("pe", "matmul", out=pC[rows, 2, :], lhsT=TR[:, 1, hc], rhs=TR[:, 2, hc], start=False, stop=True)
                    yield
                    S.I("dve", "scalar_tensor_tensor", out=Sn[rows, :], in0=Sc[rows, :], scalar=gamC[rows, c:c + 1],
                        in1=pC[rows, 2, :], op0=ALU.mult, op1=ALU.add)
                    S.I("act", "copy", out=STb[rows, :], in_=Sn[rows, :])
                    S.I("act", "copy", out=ysb[:, hh, :], in_=pY)

                def run_rr(gens):
                    gens = list(gens)
                    while gens:
                        for g_ in list(gens):
                            try:
                                next(g_)
                            except StopIteration:
                                gens.remove(g_)

                def gn_out(c):
                    cs = slice(c * 128, (c + 1) * 128)
                    y3 = ysb[:]
                    S.I("dve", "tensor_reduce", out=st1[:, 0:2], in_=y3, axis=AX.X, op=ALU.add)
                    S.I("act", "activation", out=ysq[:], in_=y3, func=AF.Square)
                    yield
                    S.I("dve", "tensor_reduce", out=st1[:, 2:4], in_=ysq[:], axis=AX.X, op=ALU.add)
                    S.I("dve", "tensor_scalar", out=st1[:, 0:2], in0=st1[:, 0:2], scalar1=1.0 / 64, scalar2=None, op0=ALU.mult)
                    S.I("dve", "tensor_tensor", out=st1[:, 4:6], in0=st1[:, 0:2], in1=st1[:, 0:2], op=ALU.mult)
                    S.I("dve", "tensor_scalar", out=st1[:, 2:4], in0=st1[:, 2:4], scalar1=1.0 / 64, scalar2=64e-5,
                        op0=ALU.mult, op1=ALU.add)
                    S.I("dve", "tensor_tensor", out=st1[:, 2:4], in0=st1[:, 2:4], in1=st1[:, 4:6], op=ALU.subtract)
                    yield
                    S.I("act", "activation", out=st1[:, 2:4], in_=st1[:, 2:4], func=AF.Sqrt)
                    yield
                    S.I("dve", "reciprocal", out=st1[:, 6:8], in_=st1[:, 2:4])
                    for hh in range(2):
                        S.I("dve", "tensor_scalar", out=ynb[:, hh * 64:(hh + 1) * 64], in0=ysb[:, hh, :],
                            scalar1=st1[:, hh:hh + 1], scalar2=st1[:, 6 + hh:7 + hh], op0=ALU.subtract, op1=ALU.mult)
                    yield
                    S.I("pe", "transpose", out=pTb[:, 768:896], in_=ynb[:], identity=C.identb[:])
                    yield
                    S.I("dve", "tensor_scalar", out=of1[:], in0=pTb[:, 768:896], scalar1=col(7), scalar2=col(8),
                        op0=ALU.mult, op1=ALU.add)
                    S.I("dve", "tensor_tensor", out=of1[:], in0=of1[:], in1=bvkeep[:, cs], op=ALU.add)
                    S.I("dve", "tensor_tensor", out=ostg[:, cs], in0=of1[:], in1=gkeep[:, cs], op=ALU.mult)

                def groups_of(gi):
                    return [gi * 2 + g_ for g_ in range(2) if gi * 2 + g_ < NCS]

                def emit_tr(gi):
                    par = gi % 2
                    for g_, c in enumerate(groups_of(gi)):
                        cs = slice(c * 128, (c + 1) * 128)
                        TR = TRg[par * 2 + g_]
                        S.I("pe", "transpose", out=pTb[:, g_ * 384 + 0:g_ * 384 + 128], in_=bhb[:, cs], identity=C.identb[:])
                        S.I("pe", "transpose", out=pTb[:, g_ * 384 + 128:g_ * 384 + 256], in_=khb[:, cs], identity=C.identb[:])
                        S.I("pe", "transpose", out=pTb[:, g_ * 384 + 256:g_ * 384 + 384], in_=vbb[:, cs], identity=C.identb[:])
                        S.I("act", "copy", out=TR[:].rearrange("p a b -> p (a b)"), in_=pTb[:, g_ * 384:g_ * 384 + 384])

                def stageA_gens(gi):
                    par = gi % 2
                    return [stageA(par * 4 + g_ * 2 + hh, c, hh, TRg[par * 2 + g_])
                            for g_, c in enumerate(groups_of(gi)) for hh in range(2)]

                def chains_gen(gi):
                    par = gi % 2
                    for g_, c in enumerate(groups_of(gi)):
                        gens = [chain(par * 4 + g_ * 2 + hh, c, hh, TRg[par * 2 + g_]) for hh in range(2)]
                        while gens:
                            for g2 in list(gens):
                                try:
                                    next(g2)
                                except StopIteration:
                                    gens.remove(g2)
                            yield
                        yield from gn_out(c)

                K.Tfinal = [None] * NU
                NG = (NCS + 1) // 2
                emit_tr(0)
                run_rr(stageA_gens(0))
                for gi in range(NG):
                    gens = [chains_gen(gi)]
                    if gi + 1 < NG:
                        emit_tr(gi + 1)
                        gens += stageA_gens(gi + 1)
                    run_rr(gens)
                piece = min(SEG, TOWN)
                for pc in range(SEG // piece):
                    tt0 = t0 + pc * piece
                    S.dma("sp", mixv[1024 + c0:1024 + c0 + 128, tt0 // TOWN, (tt0 % TOWN):(tt0 % TOWN) + piece],
                          ostg[:, pc * piece:(pc + 1) * piece])


def phase_peer(K, hres):
    nc, S, I, Z, C = K.nc, K.S, K.I, K.Z, K.C
    NEG = -1e30
    with ExitStack() as es:
        sb, ps = (lambda *a: K.sb(es, *a)), (lambda *a: K.ps(es, *a))
        idx_all = sb("p_idx", [128, NTO, 128], I32)
        gate_all = sb("p_gate", [128, NTO, 128], F32)
        ss2 = sb("p_ss2", [128, 8], F32)
        ms2 = sb("p_ms2", [128, 8], F32)
        rstd2 = sb("p_rstd2", [128, 8], F32)
        g2 = sb("p_g2", [128, D], F32); S.dma("sp", g2[:], I.g2)
        junk = sb("p_junk", [128, D], F32)
        with ExitStack() as es1:
            sb1, ps1 = (lambda *a: K.sb(es1, *a)), (lambda *a: K.ps(es1, *a))
            wqb = sb1("p_wq", [128, 16, 2048], BF16)
            wq_v = I.wq.rearrange("(kc p) c -> p kc c", p=128)
            for j in range(4):
                S.dma("pool", wqb[:, :, j * 512:(j + 1) * 512], wq_v[:, :, j * 512:(j + 1) * 512])
            skn = sb1("p_skn", [128, 16, 128], BF16)
            S.dma("pool", skn[:], I.subk.rearrange("h n c -> n h c"))
            skT = sb1("p_skT", [128, 16, 128], BF16)
            iota16 = sb1("p_iota", [128, 16], F32); S.dma("sp", iota16[:], I.iota16)
            xn2b = sb1("p_xn2b", [128, D], BF16)
            xn2T = sb1("p_xn2T", [128, 16, 128], BF16)
            q2T = sb1("p_q2T", [128, 16, 128], BF16)
            s_sb = sb1("p_s", [128, 16, 128], F32)
            s2 = sb1("p_s2", [128, 128], F32)
            sv = sb1("p_sv", [128, 16, 16], F32)
            si = sb1("p_si", [128, 16, 16], U32)
            sif = sb1("p_sif", [128, 16, 16], F32)
            cand = sb1("p_cand", [128, 8, 256], F32)
            c2 = sb1("p_c2", [128, 256], F32)
            best = sb1("p_best", [128, 8, 16], F32)
            bi = sb1("p_bi", [128, 8, 16], U32)
            ai = sb1("p_ai", [128, 8, 16], U32)
            bbi = sb1("p_bbi", [128, 8, 16], U32)
            af = sb1("p_af", [128, 8, 16], F32)
            bf = sb1("p_bf", [128, 8, 16], F32)
            oh = sb1("p_oh", [128, 8, 16, 16], F32)
            i1 = sb1("p_i1", [128, 8, 16], F32)
            i2 = sb1("p_i2", [128, 8, 16], F32)
            ef = sb1("p_ef", [128, 8, 16], F32)
            gs = sb1("p_gs", [128, 8], F32)
            pT = ps1("p_pT", [128, 16, 128], BF16)
            pq = [ps1(f"p_pq{j}", [128, 4, 128], F32) for j in range(2)]
            psc = [ps1(f"p_psc{j}", [128, 4, 128], F32) for j in range(2)]
            for half in range(2):
                for j in range(8):
                    hp = half * 8 + j
                    S.I("pe", "transpose", out=pT[:, j, :], in_=skn[:, hp, :], identity=C.identb[:])
                S.I("act", "copy", out=skT[:, half * 8:(half + 1) * 8, :], in_=pT[:, 0:8, :])
            B4 = [128, 8, 16, 16]
            for tt in range(NTO):
                sumsq(K, junk, hres[:, tt, :], ss2[:, tt:tt + 1])
                rms_rstd(K, ss2[:, tt:tt + 1], ms2[:, tt:tt + 1], rstd2[:, tt:tt + 1])
                S.I("dve", "scalar_tensor_tensor", out=xn2b[:], in0=hres[:, tt, :], scalar=rstd2[:, tt:tt + 1],
                    in1=g2[:], op0=ALU.mult, op1=ALU.mult)
                for kc in range(16):
                    S.I("pe", "transpose", out=pT[:, kc, :], in_=xn2b[:, kc * 128:(kc + 1) * 128], identity=C.identb[:])
                S.I("act", "copy", out=xn2T[:], in_=pT[:])
                for g4 in range(4):
                    p = pq[g4 % 2]
                    for j in range(4):
                        hp = g4 * 4 + j
                        for kc in range(16):
                            S.I("pe", "matmul", out=p[:, j, :], lhsT=wqb[:, kc, hp * 128:(hp + 1) * 128],
                                rhs=xn2T[:, kc, :], start=(kc == 0), stop=(kc == 15))
                    S.I("act", "copy", out=q2T[:, g4 * 4:(g4 + 1) * 4, :], in_=p[:])
                for g4 in range(4):
                    p = psc[g4 % 2]
                    for j in range(4):
                        hp = g4 * 4 + j
                        S.I("pe", "matmul", out=p[:, j, :], lhsT=q2T[:, hp, :], rhs=skT[:, hp, :], start=True, stop=True)
                    S.I("dve", "tensor_copy", out=s_sb[:, g4 * 4:(g4 + 1) * 4, :], in_=p[:])
                for hp in range(16):
                    S.I("dve", "max", out=sv[:, hp, 0:8], in_=s_sb[:, hp, :])
                    S.I("dve", "max_index", out=si[:, hp, 0:8], in_max=sv[:, hp, 0:8], in_values=s_sb[:, hp, :])
                    S.I("dve", "match_replace", out=s2[:], in_to_replace=sv[:, hp, 0:8], in_values=s_sb[:, hp, :],
                        imm_value=NEG)
                    S.I("dve", "max", out=sv[:, hp, 8:16], in_=s2[:])
                    S.I("dve", "max_index", out=si[:, hp, 8:16], in_max=sv[:, hp, 8:16], in_values=s2[:])
                S.I("dve", "tensor_copy", out=sif[:], in_=si[:])
                sv4 = sv[:].rearrange("p (h two) k -> p h two k", two=2)
                sif4 = sif[:].rearrange("p (h two) k -> p h two k", two=2)
                S.I("dve", "tensor_tensor", out=cand[:].rearrange("p h (a b) -> p h a b", a=16),
                    in0=sv4[:, :, 0, :].unsqueeze(3).broadcast_to(B4), in1=sv4[:, :, 1, :].unsqueeze(2).broadcast_to(B4),
                    op=ALU.add)
                for h in range(8):
                    S.I("dve", "max", out=best[:, h, 0:8], in_=cand[:, h, :])
                    S.I("dve", "max_index", out=bi[:, h, 0:8], in_max=best[:, h, 0:8], in_values=cand[:, h, :])
                    S.I("dve", "match_replace", out=c2[:], in_to_replace=best[:, h, 0:8], in_values=cand[:, h, :],
                        imm_value=NEG)
                    S.I("dve", "max", out=best[:, h, 8:16], in_=c2[:])
                    S.I("dve", "max_index", out=bi[:, h, 8:16], in_max=best[:, h, 8:16], in_values=c2[:])
                S.I("dve", "tensor_single_scalar", out=ai[:], in_=bi[:], scalar=4, op=ALU.logical_shift_right)
                S.I("dve", "tensor_single_scalar", out=bbi[:], in_=bi[:], scalar=15, op=ALU.bitwise_and)
                S.I("dve", "tensor_copy", out=af[:], in_=ai[:])
                S.I("dve", "tensor_copy", out=bf[:], in_=bbi[:])
                io4 = iota16[:].unsqueeze(1).unsqueeze(1).broadcast_to(B4)
                for (xf, which, dst) in ((af, 0, i1), (bf, 1, i2)):
                    S.I("dve", "tensor_tensor", out=oh[:], in0=xf[:].unsqueeze(3).broadcast_to(B4), in1=io4, op=ALU.is_equal)
                    S.I("dve", "tensor_tensor", out=oh[:], in0=oh[:], in1=sif4[:, :, which, :].unsqueeze(2).broadcast_to(B4),
                        op=ALU.mult)
                    S.I("dve", "tensor_reduce", out=dst[:], in_=oh[:], axis=AX.X, op=ALU.add)
                S.I("dve", "scalar_tensor_tensor", out=ef[:], in0=i1[:], scalar=128.0, in1=i2[:], op0=ALU.mult, op1=ALU.add)
                S.I("dve", "tensor_copy", out=idx_all[:, tt, :].rearrange("p (h k) -> p h k", h=8), in_=ef[:])
                S.I("dve", "tensor_tensor", out=ef[:], in0=best[:], in1=best[:, :, 0:1].broadcast_to([128, 8, 16]),
                    op=ALU.subtract)
                S.I("act", "activation", out=ef[:], in_=ef[:], func=AF.Exp)
                S.I("dve", "tensor_reduce", out=gs[:], in_=ef[:], axis=AX.X, op=ALU.add)
                S.I("dve", "reciprocal", out=gs[:], in_=gs[:])
                S.I("dve", "tensor_tensor", out=gate_all[:, tt, :].rearrange("p (h k) -> p h k", h=8), in0=ef[:],
                    in1=gs[:].unsqueeze(2).broadcast_to([128, 8, 16]), op=ALU.mult)
        S.barrier()
        if "peer" in DBG:
            for nm, t in (("pidx", idx_all), ("pgate", gate_all)):
                dst = nc.dram_tensor("dbg_" + nm, [128, NTO, 128], t.dtype, kind="ExternalOutput").ap()
                S.dma("sp", dst, t[:])
        with ExitStack() as es1:
            sb1, ps1 = (lambda *a: K.sb(es1, *a)), (lambda *a: K.ps(es1, *a))
            xn2p = [ps1(f"p_xn2p{j}", [128, 512], F32) for j in range(4)]
            accp = [ps1(f"p_accp{j}", [128, 512], F32) for j in range(4)]
            ring = [sb1(f"p_ring{j}", [128, D], BF16) for j in range(8)]
            hpre = sb1("p_hpre", [128, 128], F32)
            hp4 = sb1("p_hp4", [128, 4, 128], F32)
            actv = sb1("p_act", [128, 128], F32)
            nr = 0
            for tt in range(NTO):
                for j in range(4):
                    js = slice(j * 512, (j + 1) * 512)
                    S.I("dve", "scalar_tensor_tensor", out=xn2p[j][:, :], in0=hres[:, tt, js], scalar=rstd2[:, tt:tt + 1],
                        in1=g2[:, js], op0=ALU.mult, op1=ALU.mult)
                for slot in range(128):
                    ug = ring[nr % 8]; nr += 1
                    S.dma("pool", ug[:], Z.ub, method="indirect_dma_start", out_offset=None,
                          in_offset=bass.IndirectOffsetOnAxis(ap=idx_all[:, tt, slot:slot + 1], axis=0), xr=[idx_all[:]])
                    for j in range(4):
                        js = slice(j * 512, (j + 1) * 512)
                        S.I("dve", "scalar_tensor_tensor", out=junk[:, js], in0=ug[:, js], scalar=1.0, in1=xn2p[j][:, :],
                            op0=ALU.mult, op1=ALU.mult, accum_out=hp4[:, j, slot:slot + 1])
                S.I("dve", "tensor_tensor", out=hp4[:, 0:2, :], in0=hp4[:, 0:2, :], in1=hp4[:, 2:4, :], op=ALU.add)
                S.I("dve", "tensor_tensor", out=hpre[:], in0=hp4[:, 0, :], in1=hp4[:, 1, :], op=ALU.add)
                if "peer" in DBG and tt == 0:
                    dst = nc.dram_tensor("dbg_hpre", [128, 128], F32, kind="ExternalOutput").ap()
                    S.dma("sp", dst, hpre[:])
                S.I("act", "activation", out=actv[:], in_=hpre[:], func=AF.Gelu)
                S.I("dve", "tensor_tensor", out=actv[:], in0=actv[:], in1=gate_all[:, tt, :], op=ALU.mult)
                for slot in range(128):
                    vg = ring[nr % 8]; nr += 1
                    S.dma("pool", vg[:], Z.vb, method="indirect_dma_start", out_offset=None,
                          in_offset=bass.IndirectOffsetOnAxis(ap=idx_all[:, tt, slot:slot + 1], axis=0), xr=[idx_all[:]])
                    for j in range(4):
                        js = slice(j * 512, (j + 1) * 512)
                        if slot == 0:
                            S.I("dve", "tensor_scalar", out=accp[j][:, :], in0=vg[:, js], scalar1=actv[:, 0:1], scalar2=None,
                                op0=ALU.mult)
                        else:
                            S.I("dve", "scalar_tensor_tensor", out=accp[j][:, :], in0=vg[:, js], scalar=actv[:, slot:slot + 1],
                                in1=accp[j][:, :], op0=ALU.mult, op1=ALU.add)
                for j in range(4):
                    js = slice(j * 512, (j + 1) * 512)
                    S.I("dve", "tensor_tensor", out=hres[:, tt, js], in0=accp[j][:, :], in1=hres[:, tt, js], op=ALU.add)
        S.barrier()


def phase_tail(K):
    nc, S, I, Z, C = K.nc, K.S, K.I, K.Z, K.C
    with ExitStack() as es:
        sb, ps = (lambda *a: K.sb(es, *a)), (lambda *a: K.ps(es, *a))
        hres = sb("hres", [128, NTO, D], F32)
        S.dma("sp", hres[:], I.xo.rearrange("(tt p) d -> p tt d", p=128))
        with ExitStack() as es1:
            sb1, ps1 = (lambda *a: K.sb(es1, *a)), (lambda *a: K.ps(es1, *a))
            ridx = sb1("ridx", [128, 16], I32)
            S.dma("sp", ridx[:], I.ridx)
            mixT = sb1("mixT", [128, 16, TOWN], BF16)
            for fc in range(16):
                if os.environ.get("MK_NOIND"):
                    S.dma("sp", mixT[:, fc, :], Z.mix[fc * 128:(fc + 1) * 128, :])
                    continue
                S.dma("pool", mixT[:, fc, :], Z.mix, method="indirect_dma_start", out_offset=None,
                      in_offset=bass.IndirectOffsetOnAxis(ap=ridx[:, fc:fc + 1], axis=0), xr=[ridx[:]])
            wb = [sb1(f"wo{j}", [128, 16, 512], BF16) for j in range(2)]
            pp = [ps1(f"po{j}", [128, 512], F32) for j in range(4)]
            wo_v = I.wout.rearrange("(kc p) c -> p kc c", p=128)
            n = 0
            for cg in range(4):
                w = wb[cg % 2]
                S.dma("pool", w[:], wo_v[:, :, cg * 512:(cg + 1) * 512])
                for tt in range(NTO):
                    p = pp[n % 4]; n += 1
                    for kc in range(16):
                        S.I("pe", "matmul", out=p[:, :], lhsT=mixT[:, kc, tt * 128:(tt + 1) * 128],
                            rhs=w[:, kc, :], start=(kc == 0), stop=(kc == 15))
                    S.I("dve", "tensor_tensor", out=hres[:, tt, cg * 512:(cg + 1) * 512], in0=p[:, :],
                        in1=hres[:, tt, cg * 512:(cg + 1) * 512], op=ALU.add)
        S.barrier()
        if UPTO >= 6:
            phase_peer(K, hres)
        if os.environ.get("MK_NOFIN"):
            for tt in range(NTO):
                S.dma("sp", K.out[tt * 128:(tt + 1) * 128, :], hres[:, tt, :])
            return
        with ExitStack() as es1:
            sb1 = (lambda *a: K.sb(es1, *a))
            gf = sb1("gf", [128, D], F32)
            S.dma("sp", gf[:], I.gf)
            junk = sb1("junk2", [128, D], F32)
            ss = sb1("ss2", [128, max(NTO, 8)], F32)
            ms = sb1("ms2", [128, max(NTO, 8)], F32)
            rstd = sb1("rstd2", [128, max(NTO, 8)], F32)
            ot = [sb1(f"ot{j}", [128, D], F32) for j in range(2)]
            for tt in range(NTO):
                sumsq(K, junk, hres[:, tt, :], ss[:, tt:tt + 1])
                rms_rstd(K, ss[:, tt:tt + 1], ms[:, tt:tt + 1], rstd[:, tt:tt + 1])
                S.I("dve", "scalar_tensor_tensor", out=ot[tt % 2][:], in0=hres[:, tt, :],
                    scalar=rstd[:, tt:tt + 1], in1=gf[:], op0=ALU.mult, op1=ALU.mult)
                S.dma("sp", K.out[tt * 128:(tt + 1) * 128, :], ot[tt % 2][:])


def rep(v, n=128):
    v = np.asarray(v, np.float32).reshape(1, -1)
    return np.ascontiguousarray(np.broadcast_to(v, (n, v.shape[1])))


def make_in_maps(inp):
    maps = []
    for c in range(8):
        b, tq = c // 4, c % 4
        m = {}
        m["in_x"] = np.ascontiguousarray(inp["x"][b, :S_])
        m["in_xo"] = np.ascontiguousarray(inp["x"][b, tq * TOWN:(tq + 1) * TOWN])
        m["in_g1"] = rep(inp["norm1_g"][0])
        m["in_win"] = np.ascontiguousarray(inp["w_in"][0])
        m["in_wout"] = np.ascontiguousarray(inp["w_out"][0])
        m["in_g2"] = rep(inp["norm2_g"][0])
        m["in_gf"] = rep(inp["final_g"])
        m["in_ident"] = np.eye(128, dtype=np.float32)
        m.update(attn_consts())
        m.update(rwkv_consts(inp))
        m["in_wq"] = np.ascontiguousarray(inp["peer_wq"][0])
        m["in_subk"] = np.ascontiguousarray(inp["peer_sub_keys"][0].reshape(16, 128, 128))
        m["in_u"] = np.ascontiguousarray(inp["peer_u"][0])
        m["in_v"] = np.ascontiguousarray(inp["peer_v"][0])
        m["in_iota16"] = rep(np.arange(16))
        f = np.arange(128)[:, None] + 128 * np.arange(16)[None, :]
        m["in_ridx"] = (f * 4 + tq).astype(np.int32)
        maps.append(m)
    return maps


_AC = None


def attn_consts():
    global _AC
    if _AC is not None:
        return _AC
    NB = S_ // 256
    n = np.arange(16)[None, :]
    qb = np.arange(16)[:, None]
    past = (n < qb) & (n < NB)
    pb = np.where(past, 0.0, -1e30).astype(np.float32)
    pm = past.astype(np.float32)
    own = (n == qb).astype(np.float32)
    p = np.arange(128)[:, None, None]
    i = np.arange(NT)[None, :, None]
    nn = np.arange(16)[None, None, :]
    Dm = (256 * nn - (128 * i + p)).astype(np.float64)
    dhi = 64.0 * np.floor(Dm / 64.0)
    dlo = Dm - dhi
    esel = np.zeros((32, 16, 128), np.float32)
    for nb in range(16):
        esel[nb, nb, :] = 1.0
        esel[16 + nb, nb, :] = 1.0
    kb = (np.arange(2)[None, :] * 128 + np.arange(128)[:, None]).astype(np.float32)
    k = np.arange(128)[:, None, None]
    r = np.arange(4)[None, :, None]
    q = np.arange(512)[None, None, :]
    cm = np.where((r * 128 + k) <= q, 0.0, -30000.0).astype(np.float32)
    _AC = {
        "in_pb": rep(pb.reshape(-1)), "in_pm": rep(pm.reshape(-1)), "in_own": rep(own.reshape(-1)),
        "in_dhi": np.ascontiguousarray(dhi.reshape(128, -1).astype(np.float32)),
        "in_dlo": np.ascontiguousarray(dlo.reshape(128, -1).astype(np.float32)),
        "in_esel": esel.reshape(32, -1), "in_kb": kb, "in_cm": np.ascontiguousarray(cm.reshape(128, -1)),
        "in_ones": np.ones((128, 128), np.float32),
    }
    return _AC


def rwkv_consts(inp):
    cols = lambda v: np.ascontiguousarray(np.asarray(v, np.float32).reshape(8, 128).T)
    mu = inp["rwkv_mu"][0]
    mul = np.zeros((128, 4), np.float32)
    mul[:96, 0] = mu[3072:3168]
    mul[:96, 1] = mu[3168:3264]
    mul[:, 2] = mu[3264:3392]
    mul[:, 3] = mu[3392:3520]
    prm = np.zeros((128, 10, 8), np.float32)
    prm[:, 0] = cols(mu[0:1024]); prm[:, 1] = cols(mu[1024:2048]); prm[:, 2] = cols(mu[2048:3072])
    prm[:, 4] = cols(inp["rwkv_k_k"][0]); prm[:, 5] = cols(inp["rwkv_k_a"][0]); prm[:, 6] = cols(inp["rwkv_r_k"][0].reshape(-1))
    prm[:, 7] = cols(inp["rwkv_ln_g"][0]); prm[:, 8] = cols(inp["rwkv_ln_b"][0])
    s_ = np.arange(128)[:, None]; t_ = np.arange(128)[None, :]
    strict = (s_ < t_).astype(np.float32); incl = (s_ <= t_).astype(np.float32)
    m2 = np.concatenate([strict, incl, strict, incl], axis=1)
    bones = np.kron(np.eye(2, dtype=np.float32), np.ones((64, 64), np.float32))
    seg = min(2048, S_)
    rst = np.ones((128, seg), np.float32); rst[:, ::128] = 0.0
    return {"in_w2": np.ascontiguousarray(inp["rwkv_w2"][0]), "in_a2": np.ascontiguousarray(inp["rwkv_a2"][0]),
            "in_g2r": np.ascontiguousarray(inp["rwkv_g2"][0]), "in_mul": mul, "in_w0": cols(inp["rwkv_w0"][0]),
            "in_a0": cols(inp["rwkv_a0"][0]), "in_bones": bones, "in_mask2": m2,
            "in_masklt": (s_ > t_).astype(np.float32), "in_rst": rst, "in_prm": prm.reshape(128, 80)}


_NC = None
K_last = {}


def kernel(**inputs):
    global _NC
    inp = {k: np.asarray(v) for k, v in inputs.items()}
    t0 = time.time()
    if _NC is None:
        _NC = build()
    K_last["tb"] = time.time() - t0
    maps = make_in_maps(inp)
    used = set(_NC._mk_declared)
    maps = [{k: v for k, v in m.items() if k in used} for m in maps]
    t1 = time.time()
    res = run_bass_kernel_spmd(_NC, maps, core_ids=list(range(8)))
    K_last["res"] = res
    out = np.zeros((2, S_, D), np.float32)
    print("timing build/run", K_last.get("tb"), time.time() - t1)
    for c in range(8):
        b, tq = c // 4, c % 4
        out[b, tq * TOWN:(tq + 1) * TOWN] = res.results[c]["out"]
    return out
```
